# Optimizing a Trainium2 kernel written in Bass

```python
import math
import jax, jax.numpy as jnp
from jax import lax
import numpy as np


D_MODEL = 2048
BATCH = 4
SEQ = 8192
DEPTH = 1
DEC_BATCH = 1
DEC_SEQ = 16384
PAST_LEN = 128

HEAD_DIM = 64
D_H = D_MODEL * 5 // 8
D_A = D_MODEL * 3 // 8
N_HEADS_A = D_A // HEAD_DIM
D_IN = 3 * D_H + 3 * D_A
D_MIX = D_H + D_A
HYENA_ORDER = 2
SHORT_CONV = 3
N_BANDS = 16
POS_EMB_DIM = 1 + 2 * N_BANDS
FILTER_WIDTH = 64
DECAY_TARGET = 1e-2
FAST_DECAY_PCT = 0.3
SLOW_DECAY_PCT = 1.5
MIN_DECAY = math.log(DECAY_TARGET) / SLOW_DECAY_PCT
MAX_DECAY = math.log(DECAY_TARGET) / FAST_DECAY_PCT
ATTN_PATTERNS = ((128, 1), (512, 4), (2048, 16))
NUM_BUCKETS = 32
MAX_DISTANCE = 1024
NEG_INF = -1e30
N_GROUPS = 4
EXPERTS_PER_GROUP = 8
N_EXPERTS = N_GROUPS * EXPERTS_PER_GROUP
TOP_K = 2
D_EXPERT = 1024
MOE_BLOCK = 128
RMS_EPS = 1e-6

kernel_name = 'hybrid_hyena_dilated_attn_hmoe_encoder'


def rms_norm(x, g):
    xf = x.astype(jnp.float32)
    y = xf * lax.rsqrt(jnp.mean(xf * xf, axis=-1, keepdims=True) + RMS_EPS)
    return (y * g.astype(jnp.float32)).astype(x.dtype)


def short_conv(u, w, b):
    up = jnp.pad(u, ((0, 0), (1, 1), (0, 0)))
    return up[:, :-2] * w[0] + up[:, 1:-1] * w[1] + up[:, 2:] * w[2] + b


def hyena_filter_spectra(L, w1, b1, w2, b2, w3, b3, freq, w_out):
    f32 = jnp.float32
    pos = jnp.arange(L, dtype=f32)
    t = jnp.linspace(0.0, 1.0, L, dtype=f32)
    bands = jnp.linspace(1e-4, N_BANDS - 1, N_BANDS, dtype=f32)
    ang = (2.0 * math.pi / L) * pos[:, None] * bands[None, :]
    feats = jnp.concatenate([t[:, None], jnp.cos(ang), -jnp.sin(ang)], axis=-1)
    fr = freq.astype(f32)
    a = jnp.sin(fr * (feats @ w1.astype(f32) + b1.astype(f32)))
    a = jnp.sin(fr * (a @ w2.astype(f32) + b2.astype(f32)))
    a = jnp.sin(fr * (a @ w3.astype(f32) + b3.astype(f32)))
    hr = (a @ w_out.astype(f32)).reshape(L, HYENA_ORDER, 2, D_H)
    deltas = jnp.abs(jnp.linspace(MIN_DECAY, MAX_DECAY, D_H, dtype=f32))
    hr = hr * jnp.exp(-t[:, None] * deltas[None, :])[:, None, None, :]
    g = jnp.concatenate([hr[:, :, 0], jnp.zeros((1, HYENA_ORDER, D_H), f32), hr[:0:-1, :, 1]], axis=0)
    g = g / jnp.sum(jnp.abs(g), axis=0, keepdims=True)
    return jnp.fft.rfft(g, axis=0)


def long_conv(z, spec, skip):
    L = z.shape[1]
    zf = z.astype(jnp.float32)
    zs = jnp.fft.rfft(zf, n=2 * L, axis=1)
    y = jnp.fft.irfft(zs * spec[None], n=2 * L, axis=1)[:, :L]
    return (y + zf * skip.astype(jnp.float32)).astype(z.dtype)


def t5_bucket(rel):
    half = NUM_BUCKETS // 2
    max_exact = half // 2
    ret = jnp.where(rel > 0, half, 0)
    n = jnp.abs(rel)
    nf = jnp.maximum(n, 1).astype(jnp.float32)
    large = max_exact + (jnp.log(nf / max_exact) / math.log(MAX_DISTANCE / max_exact)
                         * (half - max_exact)).astype(jnp.int32)
    large = jnp.minimum(large, half - 1)
    return ret + jnp.where(n < max_exact, n, large)


def dilated_window_attention(q, k, v, rel_bias, window, dilation):
    B, L, H, C = q.shape
    d = dilation
    R = window // (2 * dilation)
    Ls = L // d
    nb = -(-Ls // R)
    P = nb * R

    def to_sub(t):
        return t.reshape(B, Ls, d, H, C).transpose(0, 2, 1, 3, 4)

    def key_windows(t):
        tp = jnp.pad(to_sub(t), ((0, 0), (0, 0), (R, P - Ls + R), (0, 0), (0, 0)))
        tp = tp.reshape(B, d, nb + 2, R, H, C)
        return jnp.concatenate([tp[:, :, :-2], tp[:, :, 1:-1], tp[:, :, 2:]], axis=3)

    qs = jnp.pad(to_sub(q), ((0, 0), (0, 0), (0, P - Ls), (0, 0), (0, 0))).reshape(B, d, nb, R, H, C)
    kw = key_windows(k)
    vw = key_windows(v)
    qi = jnp.arange(R)
    kj = jnp.arange(3 * R)
    rel = kj[None, :] - R - qi[:, None]
    key_sub = jnp.arange(nb)[:, None] * R - R + kj[None, :]
    mask = (jnp.abs(rel) <= R)[None] & ((key_sub >= 0) & (key_sub < Ls))[:, None, :]
    bias = jnp.transpose(rel_bias.astype(jnp.float32)[t5_bucket(rel * d)], (2, 0, 1))
    logits = jnp.einsum('brnihc,brnjhc->brnhij', qs, kw) * (C ** -0.5) + bias
    logits = jnp.where(mask[:, None], logits, NEG_INF)
    m = jnp.max(logits, axis=-1, keepdims=True)
    p = jnp.exp(logits - m)
    s = jnp.sum(p, axis=-1)
    o = jnp.einsum('brnhij,brnjhc->brnihc', p, vw) / jnp.transpose(s, (0, 1, 2, 4, 3))[..., None]
    lse = jnp.transpose(m[..., 0] + jnp.log(s), (0, 1, 2, 4, 3))
    o = o.reshape(B, d, P, H, C)[:, :, :Ls].transpose(0, 2, 1, 3, 4).reshape(B, L, H, C)
    lse = lse.reshape(B, d, P, H)[:, :, :Ls].transpose(0, 2, 1, 3).reshape(B, L, H)
    return o, lse


def hierarchical_moe(h, group_router_w, group_router_b, expert_router_w, expert_router_b,
                     w_gate, w_up, w_down):
    T, D = h.shape
    f32 = jnp.float32
    hf = h.astype(f32)
    g_logits = hf @ group_router_w.astype(f32) + group_router_b.astype(f32)
    g_sel = jnp.argmax(g_logits, axis=-1)
    p_group = jnp.take_along_axis(jax.nn.softmax(g_logits, axis=-1), g_sel[:, None], axis=-1)
    e_logits = jnp.einsum('td,gde->tge', hf, expert_router_w.astype(f32)) + expert_router_b.astype(f32)
    e_logits = jnp.take_along_axis(e_logits, g_sel[:, None, None], axis=1)[:, 0]
    top_v, top_i = lax.top_k(e_logits, TOP_K)
    gates = p_group * jax.nn.softmax(top_v, axis=-1)
    experts = g_sel[:, None] * EXPERTS_PER_GROUP + top_i
    A = T * TOP_K
    flat_e = experts.reshape(A)
    flat_g = gates.reshape(A)
    flat_t = jnp.repeat(jnp.arange(T, dtype=jnp.int32), TOP_K)
    order = jnp.argsort(flat_e)
    se, st, sg = flat_e[order], flat_t[order], flat_g[order]
    counts = jax.ops.segment_sum(jnp.ones((A,), jnp.int32), flat_e, num_segments=N_EXPERTS)
    starts = jnp.cumsum(counts) - counts
    padded = (counts + MOE_BLOCK - 1) // MOE_BLOCK * MOE_BLOCK
    pends = jnp.cumsum(padded)
    pstarts = pends - padded
    dest = pstarts[se] + jnp.arange(A, dtype=jnp.int32) - starts[se]
    NB = -(-A // MOE_BLOCK) + N_EXPERTS
    S = NB * MOE_BLOCK
    slot_tok = jnp.full((S,), T, jnp.int32).at[dest].set(st)
    slot_gate = jnp.zeros((S,), f32).at[dest].set(sg)
    block_expert = jnp.minimum(
        jnp.searchsorted(pends, jnp.arange(NB, dtype=jnp.int32) * MOE_BLOCK, side='right'), N_EXPERTS - 1)
    h_pad = jnp.concatenate([h, jnp.zeros((1, D), h.dtype)], axis=0)
    xb = h_pad[slot_tok].reshape(NB, MOE_BLOCK, D)

    def expert_block(args):
        xblk, e = args
        a = jax.nn.silu(xblk @ w_gate[e]) * (xblk @ w_up[e])
        return a @ w_down[e]

    yb = lax.map(expert_block, (xb, block_expert))
    y = jnp.zeros((T + 1, D), f32).at[slot_tok].add(yb.reshape(S, D).astype(f32) * slot_gate[:, None])
    return y[:T].astype(h.dtype)


def encoder_layer(x, rel_bias, mix_norm_g, w_in, sconv_w, sconv_b, filt_w1, filt_b1, filt_w2, filt_b2,
                  filt_w3, filt_b3, filt_freq, filt_w_out, hyena_skip, q_norm_g, k_norm_g,
                  out_norm_h, out_norm_a, w_out, ffn_norm_g, group_router_w, group_router_b,
                  expert_router_w, expert_router_b, w_gate, w_up, w_down):
    B, L, D = x.shape
    h = rms_norm(x, mix_norm_g)
    u = jnp.einsum('bld,de->ble', h, w_in)
    u_h, u_a = u[..., :3 * D_H], u[..., 3 * D_H:]
    u_h = short_conv(u_h, sconv_w, sconv_b)
    v_h, gate1, gate2 = jnp.split(u_h, 3, axis=-1)
    spec = hyena_filter_spectra(L, filt_w1, filt_b1, filt_w2, filt_b2, filt_w3, filt_b3, filt_freq, filt_w_out)
    z = gate1 * long_conv(v_h, spec[:, 0], hyena_skip[0])
    y_h = gate2 * long_conv(z, spec[:, 1], hyena_skip[1])
    qkv = u_a.astype(jnp.float32).reshape(B, L, 3, N_HEADS_A, HEAD_DIM)
    q = rms_norm(qkv[:, :, 0], q_norm_g)
    k = rms_norm(qkv[:, :, 1], k_norm_g)
    v = qkv[:, :, 2]
    outs, lses = [], []
    for window, dilation in ATTN_PATTERNS:
        o_i, lse_i = dilated_window_attention(q, k, v, rel_bias, window, dilation)
        outs.append(o_i)
        lses.append(lse_i)
    alpha = jax.nn.softmax(jnp.stack(lses, axis=0), axis=0)
    y_a = jnp.sum(alpha[..., None] * jnp.stack(outs, axis=0), axis=0).reshape(B, L, D_A).astype(x.dtype)
    mixed = jnp.concatenate([rms_norm(y_h, out_norm_h), rms_norm(y_a, out_norm_a)], axis=-1)
    x = x + jnp.einsum('ble,ed->bld', mixed, w_out)
    h2 = rms_norm(x, ffn_norm_g).reshape(B * L, D)
    y_moe = hierarchical_moe(h2, group_router_w, group_router_b, expert_router_w, expert_router_b,
                             w_gate, w_up, w_down)
    return x + y_moe.reshape(B, L, D)


def setup_inputs(seed: int = 0) -> dict:
    key = jax.random.key(seed)
    ks = jax.random.split(key, 32)
    f32 = jnp.float32

    def nrm(k, shape, scale):
        return jax.random.normal(k, shape, f32) * scale

    def gain(k, shape):
        return 1.0 + 0.02 * jax.random.normal(k, shape, f32)

    return {
        'x_prompt': nrm(ks[0], (BATCH, SEQ, D_MODEL), 1.0),
        'x_sample': nrm(ks[1], (DEC_BATCH, DEC_SEQ, D_MODEL), 1.0),
        'rel_bias': nrm(ks[2], (NUM_BUCKETS, N_HEADS_A), 0.2),
        'mix_norm_g': gain(ks[3], (DEPTH, D_MODEL)),
        'w_in': nrm(ks[4], (DEPTH, D_MODEL, D_IN), D_MODEL ** -0.5),
        'sconv_w': nrm(ks[5], (DEPTH, SHORT_CONV, 3 * D_H), SHORT_CONV ** -0.5),
        'sconv_b': nrm(ks[6], (DEPTH, 3 * D_H), 0.02),
        'filt_w1': nrm(ks[7], (DEPTH, POS_EMB_DIM, FILTER_WIDTH), POS_EMB_DIM ** -0.5),
        'filt_b1': nrm(ks[8], (DEPTH, FILTER_WIDTH), 0.1),
        'filt_w2': nrm(ks[9], (DEPTH, FILTER_WIDTH, FILTER_WIDTH), FILTER_WIDTH ** -0.5),
        'filt_b2': nrm(ks[10], (DEPTH, FILTER_WIDTH), 0.1),
        'filt_w3': nrm(ks[11], (DEPTH, FILTER_WIDTH, FILTER_WIDTH), FILTER_WIDTH ** -0.5),
        'filt_b3': nrm(ks[12], (DEPTH, FILTER_WIDTH), 0.1),
        'filt_freq': 1.0 + 0.05 * jax.random.normal(ks[13], (DEPTH, FILTER_WIDTH), f32),
        'filt_w_out': nrm(ks[14], (DEPTH, FILTER_WIDTH, HYENA_ORDER * 2 * D_H), FILTER_WIDTH ** -0.5),
        'hyena_skip': 1.0 + 0.1 * jax.random.normal(ks[15], (DEPTH, HYENA_ORDER, D_H), f32),
        'q_norm_g': gain(ks[16], (DEPTH, HEAD_DIM)),
        'k_norm_g': gain(ks[17], (DEPTH, HEAD_DIM)),
        'out_norm_h': gain(ks[18], (DEPTH, D_H)),
        'out_norm_a': gain(ks[19], (DEPTH, D_A)),
        'w_out': nrm(ks[20], (DEPTH, D_MIX, D_MODEL), D_MIX ** -0.5),
        'ffn_norm_g': gain(ks[21], (DEPTH, D_MODEL)),
        'group_router_w': nrm(ks[22], (DEPTH, D_MODEL, N_GROUPS), D_MODEL ** -0.5),
        'group_router_b': nrm(ks[23], (DEPTH, N_GROUPS), 0.01),
        'expert_router_w': nrm(ks[24], (DEPTH, N_GROUPS, D_MODEL, EXPERTS_PER_GROUP), D_MODEL ** -0.5),
        'expert_router_b': nrm(ks[25], (DEPTH, N_GROUPS, EXPERTS_PER_GROUP), 0.01),
        'w_gate': nrm(ks[26], (DEPTH, N_EXPERTS, D_MODEL, D_EXPERT), D_MODEL ** -0.5),
        'w_up': nrm(ks[27], (DEPTH, N_EXPERTS, D_MODEL, D_EXPERT), D_MODEL ** -0.5),
        'w_down': nrm(ks[28], (DEPTH, N_EXPERTS, D_EXPERT, D_MODEL), D_EXPERT ** -0.5),
    }


def reference(x_prompt, x_sample, rel_bias, mix_norm_g, w_in, sconv_w, sconv_b, filt_w1, filt_b1,
              filt_w2, filt_b2, filt_w3, filt_b3, filt_freq, filt_w_out, hyena_skip, q_norm_g, k_norm_g,
              out_norm_h, out_norm_a, w_out, ffn_norm_g, group_router_w, group_router_b,
              expert_router_w, expert_router_b, w_gate, w_up, w_down):
    h_p = x_prompt
    h_s = x_sample
    for l in range(DEPTH):
        lp = (mix_norm_g[l], w_in[l], sconv_w[l], sconv_b[l], filt_w1[l], filt_b1[l], filt_w2[l],
              filt_b2[l], filt_w3[l], filt_b3[l], filt_freq[l], filt_w_out[l], hyena_skip[l],
              q_norm_g[l], k_norm_g[l], out_norm_h[l], out_norm_a[l], w_out[l], ffn_norm_g[l],
              group_router_w[l], group_router_b[l], expert_router_w[l], expert_router_b[l],
              w_gate[l], w_up[l], w_down[l])
        h_p = encoder_layer(h_p, rel_bias, *lp)
        h_s = encoder_layer(h_s, rel_bias, *lp)
    y_prompt = h_p
    y_sample = h_s
    return (y_prompt, y_sample)
```

```python
import numpy as np
import concourse.bass as bass
import concourse.mybir as mybir
from concourse.bass_utils import run_bass_kernel_spmd

F32 = mybir.dt.float32
BF16 = mybir.dt.bfloat16
I32 = mybir.dt.int32
U32 = mybir.dt.uint32
ALU = mybir.AluOpType
AF = mybir.ActivationFunctionType
AX = mybir.AxisListType


def _key(item):
    if isinstance(item, (tuple, str)):
        return item
    t = getattr(item, "tensor", None)
    if t is not None:
        return t.name
    return item.name


class Prog:
    NDMA = 6

    def __init__(self, nc):
        self.nc = nc
        self.E = {"pe": nc.tensor, "dve": nc.vector, "act": nc.scalar, "pool": nc.gpsimd, "sp": nc.sync}
        self.sem = {e: nc.alloc_semaphore(name=f"c_{e}") for e in ("pe", "dve", "act", "pool")}
        self.cnt = {e: 0 for e in self.sem}
        self.known = {e: {} for e in self.E}
        self.res = {}
        self.dsem = {q: [nc.alloc_semaphore(name=f"d_{q}{i}") for i in range(self.NDMA)] for q in ("sp", "act", "pool")}
        self.dcnt = {q: [0] * self.NDMA for q in self.dsem}
        self.drr = {q: 0 for q in self.dsem}
        self.nops = 0

    def _wait(self, eng, sem, val):
        k = self.known[eng]
        name = sem.name if hasattr(sem, "name") else id(sem)
        if k.get(name, 0) >= val:
            return
        self.E[eng].wait_ge(sem, val)
        k[name] = val

    def _deps(self, eng, reads, writes):
        for r in reads:
            st = self.res.get(_key(r))
            if st is None:
                continue
            w = st["w"]
            if w is not None and not (w[2] == "pe" and eng == "pe"):
                self._wait(eng, w[0], w[1])
        for wr in writes:
            st = self.res.get(_key(wr))
            if st is None:
                continue
            w = st["w"]
            if w is not None and not (w[2] == "pe" and eng == "pe"):
                self._wait(eng, w[0], w[1])
            for (s, v, e2) in st["r"].values():
                if e2 == "pe" and eng == "pe":
                    continue
                self._wait(eng, s, v)

    def _commit(self, tok, reads, writes):
        for r in reads:
            st = self.res.setdefault(_key(r), {"w": None, "r": {}})
            nm = tok[0].name
            st["r"][nm] = tok
        for wr in writes:
            self.res[_key(wr)] = {"w": tok, "r": {}}

    def op(self, eng, fn, reads=(), writes=()):
        self._deps(eng, reads, writes)
        inst = fn(self.E[eng])
        self.cnt[eng] += 1
        inst.then_inc(self.sem[eng], 1)
        tok = (self.sem[eng], self.cnt[eng], eng)
        self._commit(tok, reads, writes)
        self.nops += 1
        return inst

    def dma(self, q, out, in_, reads=None, writes=None, fn=None, **kw):
        if reads is None:
            reads = [in_]
        if writes is None:
            writes = [out]
        i = self.drr[q]
        self.drr[q] = (i + 1) % self.NDMA
        s = self.dsem[q][i]
        if self.dcnt[q][i] > 0:
            self._wait(q, s, 16 * self.dcnt[q][i])
        self._deps(q, reads, writes)
        if fn is None:
            inst = self.E[q].dma_start(out=out, in_=in_, **kw)
        else:
            inst = fn(self.E[q])
        inst.then_inc(s, 16)
        self.dcnt[q][i] += 1
        tok = (s, 16 * self.dcnt[q][i], "dma")
        self._commit(tok, reads, writes)
        self.nops += 1
        return inst

    def barrier(self):
        for eng in self.E:
            for q in self.dsem:
                for i, sm in enumerate(self.dsem[q]):
                    if self.dcnt[q][i]:
                        self._wait(eng, sm, 16 * self.dcnt[q][i])
            for e, sm in self.sem.items():
                if self.cnt[e] and e != eng:
                    self._wait(eng, sm, self.cnt[e])
        self.res = {}

    def finish(self):
        for q in self.dsem:
            for i, s in enumerate(self.dsem[q]):
                if self.dcnt[q][i]:
                    self._wait("sp", s, 16 * self.dcnt[q][i])
        for e, s in self.sem.items():
            if self.cnt[e]:
                self._wait("sp", s, self.cnt[e])


D = 2048
DK = D // 128
EPS = 1e-6


def bcast_rows(ap_row, nparts):
    t = ap_row.tensor
    n = ap_row.shape[-1]
    return bass.AP(tensor=t, offset=ap_row.offset, ap=[[0, nparts], [1, n]])


class XT:
    def __init__(self, P, nc, es, tag):
        self.P, self.nc = P, nc
        self.xin = [es.enter_context(nc.sbuf_tensor(f"xin{tag}0", [128, 4, D], F32))] * 2
        self.xbf = [es.enter_context(nc.sbuf_tensor(f"xbf{tag}{i}", [128, 4, D], BF16)) for i in range(2)]
        self.xT = [es.enter_context(nc.sbuf_tensor(f"xT{tag}{i}", [128, DK, 512], BF16)) for i in range(2)]
        self.pt = [es.enter_context(nc.psum_tensor(f"ptr{tag}{i}", [128, 2, 512], BF16)) for i in range(2)]
        self.ident = es.enter_context(nc.sbuf_tensor(f"ident{tag}", [128, 128], BF16))
        self.n = 0

    def setup(self, ident_dram):
        P = self.P
        tmp = self.xin[0]
        P.dma("sp", tmp[:, 0, 0:128], ident_dram)
        P.op("dve", lambda e: e.tensor_copy(out=self.ident[:], in_=tmp[:, 0, 0:128]), [tmp], [self.ident])
        self.identf = None

    def load(self, x_rows_ap):
        P = self.P
        i = self.n % 2
        self.n += 1
        xin, xbf, xT = self.xin[i], self.xbf[i], self.xT[i]
        src = x_rows_ap.rearrange("(s p) d -> p s d", p=128)
        P.dma("sp", xin[:], src, [x_rows_ap], [xin])
        for s in range(4):
            eng = "act" if s % 2 == 0 else "pool"
            if eng == "act":
                P.op("act", lambda e, s=s: e.copy(out=xbf[:, s, :], in_=xin[:, s, :]), [xin], [(xbf.name, s)])
            else:
                P.op("pool", lambda e, s=s: e.tensor_copy(out=xbf[:, s, :], in_=xin[:, s, :]), [xin], [(xbf.name, s)])
        for jj in range(DK // 2):
            pt = self.pt[jj % 2]
            for j2 in range(2):
                j = jj * 2 + j2
                for s in range(4):
                    P.op("pe", lambda e, s=s, j=j, j2=j2, pt=pt: e.transpose(
                        out=pt[:, j2, s * 128:(s + 1) * 128], in_=xbf[:, s, j * 128:(j + 1) * 128], identity=self.ident[:]),
                        [(xbf.name, s), self.ident], [pt])
            eng = "dve" if jj % 2 == 0 else "act"
            if eng == "dve":
                P.op("dve", lambda e, jj=jj, pt=pt: e.tensor_copy(out=xT[:, 2 * jj:2 * jj + 2, :], in_=pt[:, :, :]), [pt], [(xT.name, jj)])
            else:
                P.op("act", lambda e, jj=jj, pt=pt: e.copy(out=xT[:, 2 * jj:2 * jj + 2, :], in_=pt[:, :, :]), [pt], [(xT.name, jj)])
        return xin, xbf, xT


import math
PI = math.pi


class ShortConv:
    def __init__(self, P, nc, es, nchunks, RC, tag):
        self.P, self.RC = P, RC
        sb = lambda n, shp, dt=F32: es.enter_context(nc.sbuf_tensor(f"{n}{tag}", shp, dt))
        self.carry = sb("carry", [RC, nchunks, 2])
        self.U = [sb(f"U{i}", [RC, 516]) for i in range(2)]
        self.ta = [sb(f"ta{i}", [RC, 512]) for i in range(2)]
        self.tb = [sb(f"tb{i}", [RC, 512]) for i in range(2)]
        self.ob = [sb(f"ob{i}", [RC, 512], BF16) for i in range(2)]
        self.n = 0

    def reset(self):
        self.P.op("pool", lambda e: e.memset(self.carry[:], 0.0), [], [self.carry])

    def step(self, m, w, fill, emit):
        P, RC = self.P, self.RC
        k = self.n % 2
        self.n += 1
        U, ta, tb, ob = self.U[k], self.ta[k], self.tb[k], self.ob[k]
        P.op("pool", lambda e: e.tensor_copy(out=U[:, 0:2], in_=self.carry[:, m, :]), [(self.carry.name, m)], [U])
        fill(U)
        P.op("pool", lambda e: e.tensor_copy(out=self.carry[:, m, :], in_=U[:, 512:514]), [U], [(self.carry.name, m)])
        if emit is None:
            return
        lo, hi = emit[1], emit[2]
        n = hi - lo
        P.op("act", lambda e: e.activation(out=ta[:, 0:n], in_=U[:, 1 + lo:1 + hi], func=AF.Identity, bias=w[:, m, 3:4], scale=w[:, m, 1:2]), [U, w], [ta])
        P.op("dve", lambda e: e.scalar_tensor_tensor(out=tb[:, 0:n], in0=U[:, lo:hi], scalar=w[:, m, 0:1], in1=ta[:, 0:n], op0=ALU.mult, op1=ALU.add), [U, w, ta], [tb])
        P.op("dve", lambda e: e.scalar_tensor_tensor(out=ob[:, lo:hi], in0=U[:, 2 + lo:2 + hi], scalar=w[:, m, 2:3], in1=tb[:, 0:n], op0=ALU.mult, op1=ALU.add), [U, w, tb], [ob])
        emit[0](ob)

    def flush(self, m, w, emit):
        P = self.P
        k = self.n % 2
        self.n += 1
        ta, ob = self.ta[k], self.ob[k]
        c = self.carry
        P.op("dve", lambda e: e.tensor_scalar(out=ta[:, 0:1], in0=c[:, m, 1:2], scalar1=w[:, m, 1:2], scalar2=w[:, m, 3:4], op0=ALU.mult, op1=ALU.add),
             [(c.name, m), w], [ta])
        P.op("dve", lambda e: e.scalar_tensor_tensor(out=ob[:, 0:1], in0=c[:, m, 0:1], scalar=w[:, m, 0:1], in1=ta[:, 0:1], op0=ALU.mult, op1=ALU.add),
             [(c.name, m), w, ta], [ob])
        emit(ob)


def phase_h(P, nc, seqs, NCOL, x_seq, w_h, gmix, scw, ident_d, UH, tag="h"):
    import contextlib
    RC = min(128, NCOL)
    NCH = NCOL // RC
    with contextlib.ExitStack() as es:
        sb = lambda n, shp, dt=F32: es.enter_context(nc.sbuf_tensor(n, shp, dt))
        xt = XT(P, nc, es, tag)
        xt.setup(ident_d)
        Wh = sb(f"Wh{tag}", [128, DK, NCOL], BF16)
        g_sb, scw_sb, epsT = sb(f"g_sb{tag}", [128, DK]), sb(f"scw_sb{tag}", [RC, NCH, 4]), sb(f"epsT{tag}", [128, 1])
        identf = sb(f"identf_h{tag}", [128, 128])
        ssq, e2c = [sb(f"ssq{tag}{i}", [128, 4]) for i in range(2)], [sb(f"rc{tag}{i}", [128, 4]) for i in range(2)]
        rbc = [sb(f"rbc{tag}{i}", [128, 4, 128]) for i in range(2)]
        rstd = [sb(f"rstd{tag}{i}", [128, 512]) for i in range(2)]
        junk = sb(f"junkh{tag}", [128, D], BF16)
        sc = ShortConv(P, nc, es, NCH, RC, tag)
        pss = es.enter_context(nc.psum_tensor(f"pss{tag}", [128, 512], F32))
        pu = [es.enter_context(nc.psum_tensor(f"pu{tag}{i}", [128, 512], F32)) for i in range(3)]
        P.op("pool", lambda e: e.memset(epsT[:], EPS), [], [epsT])
        for i in range(2):
            P.op("pool", lambda e, i=i: e.memset(ssq[i][:], 0.0), [], [ssq[i]])
        P.dma("sp", g_sb[:], gmix)
        P.dma("sp", scw_sb[:], scw)
        P.dma("sp", identf[:], ident_d)
        stage = xt.xin[0]
        for j in range(DK):
            for c0 in range(0, NCOL, 2048):
                cn = min(2048, NCOL - c0)
                P.dma("sp", stage[:, 0, 0:cn], w_h[j * 128:(j + 1) * 128, c0:c0 + cn], None, [stage])
                P.op("dve", lambda e, j=j, c0=c0, cn=cn: e.tensor_scalar(out=Wh[:, j, c0:c0 + cn], in0=stage[:, 0, 0:cn], scalar1=g_sb[:, j:j + 1],
                                                                       scalar2=None, op0=ALU.mult), [stage, g_sb], [Wh])
        ntile = 0
        for (s0, L, elo, ehi, oc0) in seqs:
            sc.reset()
            nt = L // 512
            for it in range(nt):
                t0 = s0 + it * 512
                lo_i = max(0, elo - (t0 - 1))
                hi_i = min(512, ehi - (t0 - 1))
                xin, xbf, xT = xt.load(x_seq[t0:t0 + 512, :])
                b = ntile % 2
                ntile += 1
                for s in range(4):
                    P.op("act", lambda e, s=s: e.activation(out=junk[:], in_=xin[:, s, :], func=AF.Square, accum_out=ssq[b][:, s:s + 1]), [xin], [junk, ssq[b]])
                P.op("act", lambda e: e.activation(out=e2c[b][:], in_=ssq[b][:], func=AF.Sqrt, bias=epsT[:], scale=1.0 / D), [ssq[b], epsT], [e2c[b]])
                P.op("dve", lambda e: e.reciprocal(out=e2c[b][:], in_=e2c[b][:]), [e2c[b]], [e2c[b]])
                P.op("pool", lambda e: e.memset(ssq[b][:], 0.0), [], [ssq[b]])
                P.op("dve", lambda e: e.tensor_copy(out=rbc[b][:], in_=bc_last(e2c[b][:, 0:4], 128)), [e2c[b]], [rbc[b]])
                for s4 in range(4):
                    P.op("pe", lambda e, s4=s4: e.matmul(pss[:, s4 * 128:(s4 + 1) * 128], lhsT=rbc[b][:, s4, :], rhs=identf[:], start=True, stop=True),
                         [rbc[b], identf], [pss])
                P.op("act", lambda e: e.copy(out=rstd[b][:], in_=pss[:]), [pss], [rstd[b]])
                for m in range(NCH):
                    pum = pu[m % 3]
                    for j in range(DK):
                        P.op("pe", lambda e, j=j, m=m, pum=pum: e.matmul(pum[0:RC, :], lhsT=Wh[:, j, m * RC:(m + 1) * RC], rhs=xT[:, j, :],
                                                                         start=(j == 0), stop=(j == DK - 1)), [Wh, (xT.name, j // 2)], [pum])

                    def fill(U, pum=pum):
                        P.op("dve", lambda e: e.tensor_tensor(out=U[:, 2:514], in0=pum[0:RC, :], in1=rstd[b][0:RC, :], op=ALU.mult), [pum, rstd[b]], [U])

                    def emit(ob, m=m, t0=t0, lo_i=lo_i, hi_i=hi_i):
                        c0 = oc0 + (t0 - 1 + lo_i) - elo
                        P.dma("pool", UH[m * RC:(m + 1) * RC, c0:c0 + hi_i - lo_i], ob[:, lo_i:hi_i], [ob], [(UH.tensor.name, m)],
                              allow_slow_non_contiguous=(hi_i - lo_i == 1))
                    sc.step(m, scw_sb, fill, (emit, lo_i, hi_i) if hi_i > lo_i else None)
                    if it == nt - 1 and ehi == s0 + L:
                        def emit2(ob, m=m):
                            c0 = oc0 + (ehi - 1) - elo
                            P.dma("pool", UH[m * RC:(m + 1) * RC, c0:c0 + 1], ob[:, 0:1], [ob], [(UH.tensor.name, m)], allow_slow_non_contiguous=True)
                        sc.flush(m, scw_sb, emit2)
        P.barrier()


def flat2(ap):
    nd = len(ap.shape)
    if nd == 2:
        return ap
    names = "abcdef"[:nd - 1]
    return ap.rearrange("p " + " ".join(names) + " -> p (" + " ".join(names) + ")")


def bc_col(ap, n):
    a = [list(x) for x in ap.ap]
    return bass.AP(tensor=ap.tensor, offset=ap.offset, ap=[a[0], [0, n]])


def bc_mid(ap, n):
    a = [list(x) for x in ap.ap]
    return bass.AP(tensor=ap.tensor, offset=ap.offset, ap=[a[0], [0, n]] + a[1:])


def bc_last(ap, n):
    a = [list(x) for x in ap.ap]
    return bass.AP(tensor=ap.tensor, offset=ap.offset, ap=a + [[0, n]])


def fft_consts(L):
    N = 2 * L
    N1 = N // 128
    H = N1 // 2
    c = {}
    n1 = np.arange(N1)[:, None]
    k1 = np.arange(N1)[None, :]
    th = 2 * np.pi * n1 * k1 / N1
    f1 = np.concatenate([np.cos(th), -np.sin(th)], 1)
    c["F1"] = f1.reshape(N1 // 128, 128, 2 * N1).transpose(1, 0, 2)
    n2 = np.arange(128)[:, None]
    th = 2 * np.pi * n2 * k1 / N
    c["TW1"] = np.stack([np.cos(th), -np.sin(th)], 1)
    k2 = np.arange(128)[None, :]
    th = 2 * np.pi * n2 * k2 / 128
    c["F2"] = np.stack([np.cos(th), -np.sin(th), np.sin(th)], 1)
    th = 2 * np.pi * np.arange(128)[:, None] * np.arange(128)[None, :] / 128
    c["F3"] = np.stack([np.concatenate([np.cos(th), np.sin(th)], 1),
                        np.concatenate([-np.sin(th), np.cos(th)], 1)], 1)
    k1c = np.arange(N1)[:, None]
    n1p = np.arange(128)[None, :]
    th = 2 * np.pi * k1c * n1p / N
    tw2 = np.stack([np.cos(th), np.sin(th)], 1)
    c["TW2"] = tw2.reshape(N1 // 128, 128, 2, 128).transpose(1, 0, 2, 3)
    n2p = np.arange(H)[None, :]
    th = 2 * np.pi * k1c * n2p / N1
    f4 = np.stack([np.cos(th) / N, -np.sin(th) / N], 1)
    c["F4"] = f4.reshape(N1 // 128, 128, 2, H).transpose(1, 0, 2, 3)
    return {k: np.ascontiguousarray(v, dtype=np.float32) for k, v in c.items()}


class FFT:
    def __init__(self, P, nc, es, L, cd, tag, Hown=None):
        self.P, self.nc, self.L = P, nc, L
        N1 = self.N1 = 2 * L // 128
        H = self.H = N1 // 2
        self.nch = N1 // 128
        self.Cb = 512 // N1
        Cb = self.Cb
        sb = lambda n, shp, dt: es.enter_context(nc.sbuf_tensor(f"{n}{tag}", shp, dt))
        self.F1 = sb("F1", [128, self.nch, 2 * N1], BF16)
        self.TW1 = sb("TW1", [128, 2, N1], F32)
        self.F2 = sb("F2", [128, 3, 128], BF16)
        self.F3 = sb("F3", [128, 2, 256], BF16)
        self.TW2 = sb("TW2", [128, self.nch, 2, 128], F32)
        self.F4 = sb("F4", [128, self.nch, 2, H], BF16)
        lst = [("F1", self.F1), ("F2", self.F2), ("F3", self.F3), ("F4", self.F4)]
        if Hown is not None:
            self.Hown = Hown
            self.F4o = sb("F4o", [128, self.nch, 2, Hown], BF16)
            self.SEL = sb("SELo", [H, Hown], BF16)
            lst += [("F4own", self.F4o), ("SEL", self.SEL)]
        stg = sb("fstage", [128, 1024], F32)
        for nm, dst in lst:
            n = int(np.prod(dst.shape[1:]))
            p = dst.shape[0]
            dflat = flat2(dst[:])
            sflat = flat2(cd[nm])
            for c0 in range(0, n, 1024):
                cn = min(1024, n - c0)
                P.dma("sp", stg[0:p, 0:cn], sflat[:, c0:c0 + cn], None, [stg])
                P.op("dve", lambda e, dflat=dflat, p=p, c0=c0, cn=cn: e.tensor_copy(out=dflat[:, c0:c0 + cn], in_=stg[0:p, 0:cn]), [stg], [dst])
        P.dma("sp", self.TW1[:], cd["TW1"])
        P.dma("sp", self.TW2[:], cd["TW2"])
        self.t1 = [sb(f"ft1_{i}", [128, 1024], F32) for i in range(2)]
        self.t2 = [sb(f"ft2_{i}", [128, 1024], F32) for i in range(2)]
        self.Ar = [sb(f"Ar{i}", [128, Cb, N1], BF16) for i in range(2)]
        self.Ai = [sb(f"Ai{i}", [128, Cb, N1], BF16) for i in range(2)]
        self.Yr = [sb(f"Yr{i}", [128, Cb, N1], BF16) for i in range(2)]
        self.Yi = [sb(f"Yi{i}", [128, Cb, N1], BF16) for i in range(2)]
        self.Cr = [sb(f"Cr{i}", [128, Cb, self.nch, 128], BF16) for i in range(2)]
        self.Ci = [sb(f"Ci{i}", [128, Cb, self.nch, 128], BF16) for i in range(2)]
        self.g = 0

    def fwd(self, zl, zkey, pa, pb):
        P, N1, Cb = self.P, self.N1, self.Cb
        i = self.g % 2
        t1, t2, Ar, Ai = self.t1[i], self.t2[i], self.Ar[i], self.Ai[i]
        for c in range(Cb):
            for n, (kc, z) in enumerate(zl):
                K = z.shape[0]
                P.op("pe", lambda e, c=c, n=n, kc=kc, z=z, K=K: e.matmul(pa[:, c * 2 * N1:(c + 1) * 2 * N1], lhsT=z[:, c, :], rhs=self.F1[0:K, kc, :],
                                                                        start=(n == 0), stop=(n == len(zl) - 1)), [zkey, self.F1], [pa])
        pav = pa[:].rearrange("p (c r k) -> p c r k", c=Cb, r=2)
        t1v = t1[:].rearrange("p (c r k) -> p c r k", c=Cb, r=2)
        t2v = t2[:].rearrange("p (c r k) -> p c r k", c=Cb, r=2)
        twr = bc_mid(bc_mid(self.TW1[:, 0, :], 2), Cb)
        twi = bc_mid(bc_mid(self.TW1[:, 1, :], 2), Cb)
        P.op("dve", lambda e: e.tensor_tensor(out=t1v, in0=pav, in1=twr, op=ALU.mult), [pa, self.TW1], [t1])
        P.op("dve", lambda e: e.tensor_tensor(out=t2v, in0=pav, in1=twi, op=ALU.mult), [pa, self.TW1], [t2])
        P.op("pool", lambda e: e.tensor_tensor(out=Ar[:], in0=t1v[:, :, 0, :], in1=t2v[:, :, 1, :], op=ALU.subtract), [t1, t2], [Ar])
        P.op("pool", lambda e: e.tensor_tensor(out=Ai[:], in0=t2v[:, :, 0, :], in1=t1v[:, :, 1, :], op=ALU.add), [t1, t2], [Ai])
        Arf = Ar[:].rearrange("p c k -> p (c k)")
        Aif = Ai[:].rearrange("p c k -> p (c k)")
        F2 = self.F2
        P.op("pe", lambda e: e.matmul(pb[:, 0:512], lhsT=F2[:, 0, :], rhs=Arf, start=True, stop=False), [F2, Ar], [pb])
        P.op("pe", lambda e: e.matmul(pb[:, 0:512], lhsT=F2[:, 2, :], rhs=Aif, start=False, stop=True), [F2, Ai], [pb])
        P.op("pe", lambda e: e.matmul(pb[:, 512:1024], lhsT=F2[:, 0, :], rhs=Aif, start=True, stop=False), [F2, Ai], [pb])
        P.op("pe", lambda e: e.matmul(pb[:, 512:1024], lhsT=F2[:, 1, :], rhs=Arf, start=False, stop=True), [F2, Ar], [pb])

    def inv(self, G, pa, pb, own=False, extra=None):
        P, N1, Cb, nch, H = self.P, self.N1, self.Cb, self.nch, self.H
        i = self.g % 2
        t1, t2, Yr, Yi, Cr, Ci = self.t1[i], self.t2[i], self.Yr[i], self.Yi[i], self.Cr[i], self.Ci[i]
        Xr = pb[:, 0:512].rearrange("p (c k) -> p c k", c=Cb)
        Xi = pb[:, 512:1024].rearrange("p (c k) -> p c k", c=Cb)
        Gr, Gi = G[:, :, 0:N1], G[:, :, N1:2 * N1]
        q = lambda t, j: t[:, j * 512:(j + 1) * 512].rearrange("p (c k) -> p c k", c=Cb)
        P.op("dve", lambda e: e.tensor_tensor(out=q(t1, 0), in0=Xr, in1=Gr, op=ALU.mult), [pb, G], [t1])
        P.op("dve", lambda e: e.tensor_tensor(out=q(t1, 1), in0=Xi, in1=Gi, op=ALU.mult), [pb, G], [t1])
        P.op("dve", lambda e: e.tensor_tensor(out=q(t2, 0), in0=Xr, in1=Gi, op=ALU.mult), [pb, G], [t2])
        P.op("dve", lambda e: e.tensor_tensor(out=q(t2, 1), in0=Xi, in1=Gr, op=ALU.mult), [pb, G], [t2])
        P.op("pool", lambda e: e.tensor_tensor(out=Yr[:], in0=q(t1, 0), in1=q(t1, 1), op=ALU.subtract), [t1], [Yr])
        P.op("pool", lambda e: e.tensor_tensor(out=Yi[:], in0=q(t2, 0), in1=q(t2, 1), op=ALU.add), [t2], [Yi])
        F3 = self.F3
        for c in range(Cb):
            for ch in range(nch):
                o = (c * nch + ch) * 256
                P.op("pe", lambda e, c=c, ch=ch, o=o: e.matmul(pa[:, o:o + 256], lhsT=Yr[:, c, ch * 128:(ch + 1) * 128], rhs=F3[:, 0, :], start=True, stop=False),
                     [Yr, F3], [pa])
                P.op("pe", lambda e, c=c, ch=ch, o=o: e.matmul(pa[:, o:o + 256], lhsT=Yi[:, c, ch * 128:(ch + 1) * 128], rhs=F3[:, 1, :], start=False, stop=True),
                     [Yi, F3], [pa])
        pav = pa[:].rearrange("p (c h r k) -> p c h r k", c=Cb, h=nch, r=2)
        t1v = t1[:].rearrange("p (c h r k) -> p c h r k", c=Cb, h=nch, r=2)
        t2v = t2[:].rearrange("p (c h r k) -> p c h r k", c=Cb, h=nch, r=2)
        for ch in range(nch):
            twr = bc_mid(bc_mid(self.TW2[:, ch, 0, :], 2), Cb)
            twi = bc_mid(bc_mid(self.TW2[:, ch, 1, :], 2), Cb)
            P.op("dve", lambda e, ch=ch, twr=twr: e.tensor_tensor(out=t1v[:, :, ch], in0=pav[:, :, ch], in1=twr, op=ALU.mult), [pa, self.TW2], [t1])
            P.op("dve", lambda e, ch=ch, twi=twi: e.tensor_tensor(out=t2v[:, :, ch], in0=pav[:, :, ch], in1=twi, op=ALU.mult), [pa, self.TW2], [t2])
        P.op("pool", lambda e: e.tensor_tensor(out=Cr[:], in0=t1v[:, :, :, 0, :], in1=t2v[:, :, :, 1, :], op=ALU.subtract), [t1, t2], [Cr])
        P.op("pool", lambda e: e.tensor_tensor(out=Ci[:], in0=t2v[:, :, :, 0, :], in1=t1v[:, :, :, 1, :], op=ALU.add), [t1, t2], [Ci])
        F4 = self.F4o if own else self.F4
        Hout = self.Hown if own else H
        k = 0
        tot = 2 * nch + (1 if extra is not None else 0)
        outv = pb[0:Hout, 0:Cb * 128].rearrange("p (c k) -> p c k", c=Cb)
        for ch in range(nch):
            for r, Cx in ((0, Cr), (1, Ci)):
                P.op("pe", lambda e, ch=ch, r=r, Cx=Cx, k=k: e.matmul(outv, lhsT=F4[:, ch, r, :], rhs=Cx[:, :, ch, :],
                                                                      start=(k == 0), stop=(k == tot - 1)), [F4, Cx], [pb])
                k += 1
        if extra is not None:
            P.op("pe", lambda e: e.matmul(outv, lhsT=extra[0], rhs=extra[1], start=False, stop=True), list(extra[2]), [pb])
        self.g += 1


def hyena_filters(P, nc, L, fd, FILT, CHN, tag):
    import contextlib
    NR = 2 * CHN
    RC = min(128, NR)
    NCH = NR // RC
    nt = 2 * L // 512
    nth = nt // 2
    with contextlib.ExitStack() as es:
        sb = lambda n, shp, dt=F32: es.enter_context(nc.sbuf_tensor(f"{n}{tag}", shp, dt))
        w1s, w2s, w3s = sb("w1s", [33, 64]), sb("w2s", [64, 64]), sb("w3s", [64, 64])
        fv, fb, wouts, nd = sb("fv", [64, 4]), sb("fb", [64, 3]), sb("wouts", [64, 2, NR]), sb("nd", [RC, NCH])
        ft = [sb(f"ft{i}", [33, 512]) for i in range(2)]
        tbc = [sb(f"tbc{i}", [RC, 512]) for i in range(2)]
        arg, arg2 = sb("arg", [64, 512]), sb("arg2", [64, 512])
        aa = [sb(f"aa{i}", [64, 512]) for i in range(3)]
        dec = [sb(f"dec{i}", [RC, 512]) for i in range(2)]
        gt = [sb(f"gt{i}", [RC, 512]) for i in range(2)]
        junk = sb("junk", [RC, 512])
        gob = [sb(f"gob{i}", [RC, 512], BF16) for i in range(2)]
        acc = sb("acc", [RC, NCH, nt])
        accs, inv = sb("accs", [RC, NCH]), sb("inv", [RC, NCH])
        pz = [es.enter_context(nc.psum_tensor(f"pz{tag}{i}", [64, 512], F32)) for i in range(2)]
        phr = [es.enter_context(nc.psum_tensor(f"phr{tag}{i}", [128, 512], F32)) for i in range(2)]
        for dst, src in ((w1s, fd["w1"]), (w2s, fd["w2"]), (w3s, fd["w3"]), (fv, fd["fvec"]), (wouts, fd["woutd"]), (nd, fd["ndelta"])):
            P.dma("sp", dst[:], src)
        P.op("pool", lambda e: e.memset(acc[:], 0.0), [], [acc])
        P.op("dve", lambda e: e.tensor_scalar(out=fb[:], in0=fv[:, 1:4], scalar1=fv[:, 0:1], scalar2=None, op0=ALU.mult), [fv], [fb])
        ws = [w1s, w2s, w3s]
        cnt = [0]
        MAGIC = 12582912.0

        def mlp(it):
            k = cnt[0] % 2
            cnt[0] += 1
            P.dma("sp", ft[k][:], fd["featsT2"][:, it * 512:(it + 1) * 512])
            P.dma("sp", tbc[k][:], bcast_rows(fd["trow2"][:, it * 512:(it + 1) * 512], RC))
            src = ft[k]
            for l in range(3):
                p = pz[l % 2]
                P.op("pe", lambda e, l=l, p=p, src=src: e.matmul(p[:], lhsT=ws[l][:], rhs=src[:], start=True, stop=True), [ws[l], src], [p])
                P.op("dve", lambda e, l=l, p=p: e.tensor_scalar(out=arg[:], in0=p[:], scalar1=fv[:, 0:1], scalar2=fb[:, l:l + 1],
                                                                op0=ALU.mult, op1=ALU.add), [p, fv, fb], [arg])
                P.op("dve", lambda e: e.tensor_scalar(out=arg2[:], in0=arg[:], scalar1=1.0 / (2 * PI), scalar2=MAGIC, op0=ALU.mult, op1=ALU.add), [arg], [arg2])
                P.op("dve", lambda e: e.tensor_scalar(out=arg2[:], in0=arg2[:], scalar1=-MAGIC, scalar2=None, op0=ALU.add), [arg2], [arg2])
                P.op("dve", lambda e: e.scalar_tensor_tensor(out=arg[:], in0=arg2[:], scalar=-2 * PI, in1=arg[:], op0=ALU.mult, op1=ALU.add), [arg2, arg], [arg])
                P.op("dve", lambda e: e.tensor_scalar(out=arg[:], in0=arg[:], scalar1=-PI, scalar2=PI, op0=ALU.max, op1=ALU.min), [arg], [arg])
                P.op("act", lambda e, l=l: e.activation(out=aa[l][:], in_=arg[:], func=AF.Sin), [arg], [aa[l]])
                src = aa[l]
            return aa[2], tbc[k]

        def hr_chunk(a3, tb, r, it):
            j = r % 2
            p = phr[j]
            d = it // nth
            P.op("pe", lambda e: e.matmul(p[0:RC, :], lhsT=wouts[:, d, r * RC:(r + 1) * RC], rhs=a3[:], start=True, stop=True), [wouts, a3], [p])
            P.op("act", lambda e: e.activation(out=dec[j][:], in_=tb[:], func=AF.Exp, scale=nd[:, r:r + 1]), [tb, nd], [dec[j]])
            P.op("dve", lambda e: e.tensor_tensor(out=gt[j][:], in0=p[0:RC, :], in1=dec[j][:], op=ALU.mult), [p, dec[j]], [gt[j]])
            if it == nth:
                P.op("pool", lambda e: e.memset(gt[j][:, 0:1], 0.0), [], [gt[j]])
            return gt[j]

        for it in range(nt):
            a3, tb = mlp(it)
            for r in range(NCH):
                g = hr_chunk(a3, tb, r, it)
                P.op("act", lambda e, g=g, r=r, it=it: e.activation(out=junk[:], in_=g[:], func=AF.Abs, accum_out=acc[:, r, it:it + 1]), [g], [junk, acc])
        P.op("dve", lambda e: e.reduce_sum(out=accs[:], in_=acc[:], axis=AX.X), [acc], [accs])
        P.op("dve", lambda e: e.reciprocal(out=inv[:], in_=accs[:]), [accs], [inv])
        for it in range(nt):
            a3, tb = mlp(it)
            for r in range(NCH):
                g = hr_chunk(a3, tb, r, it)
                ob = gob[r % 2]
                P.op("pool", lambda e, g=g, ob=ob, r=r: e.tensor_scalar(out=ob[:], in0=g[:], scalar1=inv[:, r:r + 1], scalar2=None, op0=ALU.mult), [g, inv], [ob])
                P.dma("pool", FILT[r * RC:(r + 1) * RC, it * 512:(it + 1) * 512], ob[:], [ob], [FILT])
        P.barrier()


def hyena_conv(P, nc, L, s0, cd, FILT, GS, UH, skip_d, G2, own_off, Hown, YHo, CHN, tag):
    import contextlib
    with contextlib.ExitStack() as es:
        fft = FFT(P, nc, es, L, cd, tag, Hown=Hown)
        N1, H, Cb, nch = fft.N1, fft.H, fft.Cb, fft.nch
        ncg = CHN // Cb
        sb = lambda n, shp, dt=F32: es.enter_context(nc.sbuf_tensor(f"{n}{tag}", shp, dt))
        pa = [es.enter_context(nc.psum_tensor(f"pa{tag}{i}", [128, 1024], F32)) for i in range(2)]
        pb = [es.enter_context(nc.psum_tensor(f"pb{tag}{i}", [128, 1024], F32)) for i in range(2)]
        zf = [sb(f"zf{i}", [128, nch, Cb, 128], BF16) for i in range(2)]
        Gt = [sb(f"Gt{i}", [128, Cb, 2 * N1], BF16) for i in range(2)]
        skipbc = sb("skipbc", [128, 2, CHN])
        P.dma("sp", skipbc[:].rearrange("p o c -> p (o c)"), bcast_rows(skip_d, 128))
        n = 0
        for o in range(2):
            for cg in range(ncg):
                z = zf[n % 2]
                G = Gt[n % 2]
                row0 = o * CHN + cg * Cb
                for kc in range(nch):
                    P.dma("sp", z[:, kc], FILT[row0:row0 + Cb, kc * 16384:(kc + 1) * 16384].rearrange("c (a b) -> a c b", b=128), [FILT], [z])
                k = fft.g % 2
                fft.fwd([(kc, z[:, kc]) for kc in range(nch)], z, pa[k], pb[k])
                P.op("act", lambda e, k=k, G=G: e.copy(out=G[:, :, 0:N1], in_=pb[k][:, 0:512].rearrange("p (c k) -> p c k", c=Cb)), [pb[k]], [G])
                P.op("act", lambda e, k=k, G=G: e.copy(out=G[:, :, N1:2 * N1], in_=pb[k][:, 512:1024].rearrange("p (c k) -> p c k", c=Cb)), [pb[k]], [G])
                P.dma("pool", GS[o][:, cg * Cb:(cg + 1) * Cb, :], G[:], [G], [GS[o]])
                fft.g += 1
                n += 1
        vt = [sb(f"vt{i}", [H, Cb, 128], BF16) for i in range(2)]
        g1t = [sb(f"g1t{i}", [H, Cb, 128], BF16) for i in range(2)]
        g2t = [sb(f"g2t{i}", [Hown, Cb, 128], BF16) for i in range(2)]
        zt = [sb(f"zt{i}", [H, Cb, 128], BF16) for i in range(2)]
        zs = [sb(f"zs{i}", [H, Cb, 128], BF16) for i in range(2)]
        yt = [sb(f"yt{i}", [Hown, Cb, 128], BF16) for i in range(2)]
        tm = [sb(f"tm{i}", [H, Cb, 128]) for i in range(2)]
        G0 = [sb(f"G0{i}", [128, Cb, 2 * N1], BF16) for i in range(2)]
        G1 = [sb(f"G1{i}", [128, Cb, 2 * N1], BF16) for i in range(2)]
        for cg in range(ncg):
            c0 = cg * Cb
            i = cg % 2
            g0, g1, v, ga, gb, z, zz, y, t = G0[i], G1[i], vt[i], g1t[i], g2t[i], zt[i], zs[i], yt[i], tm[i]
            P.dma("sp", g0[:], GS[0][:, c0:c0 + Cb, :], [GS[0]], [g0])
            P.dma("sp", g1[:], GS[1][:, c0:c0 + Cb, :], [GS[1]], [g1])
            P.dma("sp", v[:], UH[c0:c0 + Cb, 1 + s0:1 + s0 + L].rearrange("c (a b) -> a c b", b=128), [UH], [v])
            P.dma("sp", ga[:], UH[CHN + c0:CHN + c0 + Cb, 1 + s0:1 + s0 + L].rearrange("c (a b) -> a c b", b=128), [UH], [ga])
            P.dma("sp", gb[:], G2[c0:c0 + Cb, own_off:own_off + Hown * 128].rearrange("c (a b) -> a c b", b=128), [G2], [gb])
            k = fft.g % 2
            fft.fwd([(0, v[:])], v, pa[k], pb[k])
            fft.inv(g0, pa[k], pb[k])
            sk = bc_last(skipbc[0:H, 0, c0:c0 + Cb], 128)
            P.op("pool", lambda e, v=v, sk=sk, t=t: e.tensor_tensor(out=t[:], in0=v[:], in1=sk, op=ALU.mult), [v, skipbc], [t])
            P.op("dve", lambda e, t=t, k=k: e.tensor_tensor(out=t[:], in0=t[:], in1=pb[k][0:H, 0:Cb * 128].rearrange("p (c k) -> p c k", c=Cb), op=ALU.add),
                 [t, pb[k]], [t])
            P.op("pool", lambda e, t=t, ga=ga, z=z: e.tensor_tensor(out=z[:], in0=t[:], in1=ga[:], op=ALU.mult), [t, ga], [z])
            sk1 = bc_last(skipbc[0:H, 1, c0:c0 + Cb], 128)
            P.op("pool", lambda e, z=z, sk1=sk1, zz=zz: e.tensor_tensor(out=zz[:], in0=z[:], in1=sk1, op=ALU.mult), [z, skipbc], [zz])
            k = fft.g % 2
            fft.fwd([(0, z[:])], z, pa[k], pb[k])
            fft.inv(g1, pa[k], pb[k], own=True, extra=(fft.SEL[:], zz[:], (fft.SEL, zz)))
            P.op("dve", lambda e, k=k, gb=gb, y=y: e.tensor_tensor(out=y[:], in0=pb[k][0:Hown, 0:Cb * 128].rearrange("p (c k) -> p c k", c=Cb), in1=gb[:], op=ALU.mult),
                 [pb[k], gb], [y])
            P.dma("pool", YHo[c0:c0 + Cb, own_off:own_off + Hown * 128].rearrange("c (a b) -> a c b", b=128), y[:], [y], [YHo])
        P.barrier()


NH = 12
HD = 64
DA = NH * HD
KB = 17
NEGV = -30000.0


def attn_sel_table():
    sel = np.zeros((32, 2432), np.float32)
    mult = {}
    for d in (1, 4, 16):
        for j in range(-64, 65):
            mult[j * d] = mult.get(j * d, 0) + 1
    for delta, m in mult.items():
        n = abs(delta)
        ret = 16 if delta > 0 else 0
        nf = np.float32(max(n, 1))
        large = 8 + int(np.float32(np.log(nf / np.float32(8)) / np.float32(math.log(1024 / 8)) * np.float32(8)))
        large = min(large, 15)
        b = ret + (n if n < 8 else large)
        sel[b, delta + 1151] = m
    return sel


def phase_q(P, nc, cfg, x_ext, wq_d, wk_d, wv_d, gmix, gqk_d, kvalid_d, ident_d, bd_d, KT, QT, VE):
    import contextlib
    NE = cfg["NE"]
    own_tiles = cfg["own_tiles"]
    with contextlib.ExitStack() as es:
        sb = lambda n, shp, dt=F32: es.enter_context(nc.sbuf_tensor(n, shp, dt))
        xt = XT(P, nc, es, "q")
        xt.setup(ident_d)
        Wq, Wk, Wv = sb("Wq", [128, DK, DA], BF16), sb("Wk", [128, DK, DA], BF16), sb("Wv", [128, DK, DA], BF16)
        g_sb, gqk, epsT = sb("gq_sb", [128, DK]), sb("gqk_sb", [128, 2]), sb("epsTq", [128, 1])
        ones, BD = sb("ones_q", [128, 128], BF16), sb("BDq", [128, 128], BF16)
        e2 = [sb(f"e2{i}", [128, 512]) for i in range(2)]
        e2c = [sb(f"e2c{i}", [128, 4]) for i in range(2)]
        e2bc = [sb(f"e2bc{i}", [128, 4, 128]) for i in range(2)]
        identf = sb("identf", [128, 128])
        ssq, rcol = [sb(f"ssq{i}", [128, 4]) for i in range(2)], [sb(f"rcol{i}", [128, 4]) for i in range(2)]
        junk = sb("junkq", [128, D], BF16)
        sqk = [sb(f"sqk{i}", [128, 512], BF16) for i in range(2)]
        den = [sb(f"den{i}", [128, 512]) for i in range(2)]
        kn = [sb(f"kn{i}", [128, 512], BF16) for i in range(2)]
        Vt = [sb(f"Vt{i}", [128, NH, HD + 1], BF16) for i in range(2)]
        pss = es.enter_context(nc.psum_tensor("pssq", [128, 512], F32))
        pk = [es.enter_context(nc.psum_tensor(f"pk{i}", [128, 512], F32)) for i in range(2)]
        pst = es.enter_context(nc.psum_tensor("pst", [128, 512], F32))
        pv = es.enter_context(nc.psum_tensor("pv", [128, 2, 512], F32))
        P.op("pool", lambda e: e.memset(ones[:], 1.0), [], [ones])
        P.op("pool", lambda e: e.memset(epsT[:], EPS), [], [epsT])
        for i in range(2):
            P.op("pool", lambda e, i=i: e.memset(Vt[i][:], 1.0), [], [Vt[i]])
            P.op("pool", lambda e, i=i: e.memset(ssq[i][:], 0.0), [], [ssq[i]])
        P.dma("sp", g_sb[:], gmix)
        P.dma("sp", gqk[:], gqk_d)
        P.op("dve", lambda e: e.tensor_scalar(out=gqk[:, 0:1], in0=gqk[:, 0:1], scalar1=0.125, scalar2=None, op0=ALU.mult), [gqk], [gqk])
        stage = xt.xin[0]
        P.dma("sp", stage[:, 0, 0:128], bd_d, None, [stage])
        P.op("dve", lambda e: e.tensor_copy(out=BD[:], in_=stage[:, 0, 0:128]), [stage], [BD])
        P.dma("sp", identf[:], ident_d)
        for h in range(NH):
            P.dma("pool", KT[h, 64:65, :], kvalid_d, [], [("KTrow", h)])
            P.dma("pool", QT[h, 64:65, :], cfg["ones_row"], [], [("QTrow", h)])
        for W, wd in ((Wq, wq_d), (Wk, wk_d), (Wv, wv_d)):
            for j in range(DK):
                P.dma("sp", stage[:, 0, 0:DA], wd[j * 128:(j + 1) * 128, :], None, [stage])
                P.op("dve", lambda e, j=j, W=W: e.tensor_scalar(out=W[:, j, :], in0=stage[:, 0, 0:DA], scalar1=g_sb[:, j:j + 1],
                                                               scalar2=None, op0=ALU.mult), [stage, g_sb], [W])
        for it in range(NE // 512):
            t0 = it * 512
            xin, xbf, xT = xt.load(x_ext[t0:t0 + 512, :])
            b = it % 2
            for s in range(4):
                P.op("act", lambda e, s=s: e.activation(out=junk[:], in_=xin[:, s, :], func=AF.Square, accum_out=ssq[b][:, s:s + 1]), [xin], [junk, ssq[b]])
            P.op("act", lambda e: e.activation(out=rcol[b][:], in_=ssq[b][:], func=AF.Sqrt, bias=epsT[:], scale=1.0 / D), [ssq[b], epsT], [rcol[b]])
            P.op("dve", lambda e: e.reciprocal(out=rcol[b][:], in_=rcol[b][:]), [rcol[b]], [rcol[b]])
            P.op("dve", lambda e: e.tensor_scalar(out=e2c[b][:], in0=ssq[b][:], scalar1=64.0 * EPS / D, scalar2=64.0 * EPS * EPS, op0=ALU.mult, op1=ALU.add),
                 [ssq[b]], [e2c[b]])
            P.op("dve", lambda e: e.tensor_copy(out=e2bc[b][:], in_=bc_last(e2c[b][:, 0:4], 128)), [e2c[b]], [e2bc[b]])
            for s4 in range(4):
                P.op("pe", lambda e, s4=s4: e.matmul(pss[:, s4 * 128:(s4 + 1) * 128], lhsT=e2bc[b][:, s4, :], rhs=identf[:], start=True, stop=True),
                     [e2bc[b], identf], [pss])
            P.op("act", lambda e: e.copy(out=e2[b][:], in_=pss[:]), [pss], [e2[b]])
            P.op("pool", lambda e: e.memset(ssq[b][:], 0.0), [], [ssq[b]])
            jobs = [(Wk, 1, KT, t0)]
            if it in own_tiles:
                jobs.append((Wq, 0, QT, own_tiles[it] * 512))
            n = 0
            for (W, gi, OUT, o0) in jobs:
                for m in range(DA // 128):
                    p = pk[n % 2]
                    i2 = n % 2
                    n += 1
                    for j in range(DK):
                        P.op("pe", lambda e, j=j, m=m, p=p, W=W: e.matmul(p[:], lhsT=W[:, j, m * 128:(m + 1) * 128], rhs=xT[:, j, :],
                                                                          start=(j == 0), stop=(j == DK - 1)), [W, (xT.name, j // 2)], [p])
                    P.op("act", lambda e, p=p, i2=i2: e.activation(out=sqk[i2][:], in_=p[:], func=AF.Square), [p], [sqk[i2]])
                    P.op("pe", lambda e, i2=i2: e.matmul(pst[:], lhsT=BD[:], rhs=sqk[i2][:], start=True, stop=True), [BD, sqk[i2]], [pst])
                    P.op("dve", lambda e, i2=i2: e.tensor_tensor(out=den[i2][:], in0=pst[:], in1=e2[b][:], op=ALU.add), [pst, e2[b]], [den[i2]])
                    P.op("act", lambda e, i2=i2: e.activation(out=den[i2][:], in_=den[i2][:], func=AF.Sqrt, scale=1.0 / 64), [den[i2]], [den[i2]])
                    P.op("dve", lambda e, i2=i2: e.reciprocal(out=den[i2][:], in_=den[i2][:]), [den[i2]], [den[i2]])
                    P.op("dve", lambda e, i2=i2, p=p, gi=gi: e.scalar_tensor_tensor(out=kn[i2][:], in0=p[:], scalar=gqk[:, gi:gi + 1], in1=den[i2][:],
                                                                                    op0=ALU.mult, op1=ALU.mult), [p, gqk, den[i2]], [kn[i2]])
                    for hh in range(2):
                        h = 2 * m + hh
                        P.dma("pool", OUT[h, 0:64, o0:o0 + 512], kn[i2][hh * 64:(hh + 1) * 64, :], [kn[i2]], [(OUT.tensor.name, h)])
            for s in range(4):
                for (c0, cn, bk) in ((0, 512, 0), (512, 256, 1)):
                    for j in range(DK):
                        P.op("pe", lambda e, j=j, s=s, c0=c0, cn=cn, bk=bk: e.matmul(pv[:, bk, 0:cn], lhsT=xT[:, j, s * 128:(s + 1) * 128], rhs=Wv[:, j, c0:c0 + cn],
                                                                                    start=(j == 0), stop=(j == DK - 1)), [Wv, (xT.name, j // 2)], [pv])
                vt = Vt[s % 2]
                P.op("act", lambda e, s=s, vt=vt: e.activation(out=vt[:, 0:8, 0:HD], in_=pv[:, 0, :].rearrange("p (h c) -> p h c", c=HD), func=AF.Identity,
                                                               scale=rcol[b][:, s:s + 1]), [pv, rcol[b]], [vt])
                P.op("act", lambda e, s=s, vt=vt: e.activation(out=vt[:, 8:12, 0:HD], in_=pv[:, 1, 0:256].rearrange("p (h c) -> p h c", c=HD), func=AF.Identity,
                                                               scale=rcol[b][:, s:s + 1]), [pv, rcol[b]], [vt])
                P.dma("pool", VE[t0 + s * 128:t0 + (s + 1) * 128, :], vt[:].rearrange("p h c -> p (h c)"), [vt], [VE])
        P.barrier()


def phase_attn(P, nc, cfg, relb_d, sel_d, jmat_d, KT, QT, VE, EV, EBD, YA):
    import contextlib
    with contextlib.ExitStack() as es:
        sb = lambda n, shp, dt=F32: es.enter_context(nc.sbuf_tensor(n, shp, dt))
        relb, expb, sel = sb("relb_sb", [32, NH]), sb("expb", [32, NH]), sb("sel_sb", [32, 2432])
        ev = sb("ev", [NH, 2432], BF16)
        J, stage = sb("Jm", [128, 128], BF16), sb("stg_a", [128, 128])
        Gall = [sb(f"Gall{i}", [128, 2304], BF16) for i in range(2)]
        EBt = [sb(f"EBt{i}", [128, KB * 128], BF16) for i in range(2)]
        psA = es.enter_context(nc.psum_tensor("psA", [128, 3, 512], F32))
        psB = es.enter_context(nc.psum_tensor("psB", [128, 2, 512], F32))
        po = [es.enter_context(nc.psum_tensor(f"po{i}", [128, 512], F32)) for i in range(2)]
        P.dma("sp", relb[:], relb_d)
        P.dma("sp", sel[:], sel_d)
        P.dma("sp", stage[:], jmat_d)
        P.op("dve", lambda e: e.tensor_copy(out=J[:], in_=stage[:]), [stage], [J])
        P.op("act", lambda e: e.activation(out=expb[:], in_=relb[:], func=AF.Exp), [relb], [expb])
        for c in range(5):
            w = 512 if c < 4 else 2432 - 2048
            pp = psA[0:NH, 0, 0:w]
            P.op("pe", lambda e, c=c, w=w, pp=pp: e.matmul(pp, lhsT=expb[:], rhs=sel[:, c * 512:c * 512 + w], start=True, stop=True), [expb, sel], [psA])
            P.op("dve", lambda e, c=c, w=w, pp=pp: e.tensor_copy(out=ev[:, c * 512:c * 512 + w], in_=pp), [psA], [ev])
        P.dma("sp", EV, ev[:], [ev], [EV])
        for h in range(NH):
            G = Gall[h % 2]
            E = EBt[h % 2]
            src = bass.AP(tensor=EV.tensor, offset=EV[h, 0:1].offset, ap=[[1, 128], [1, 2304]])
            P.dma("sp", G[:], src, [EV], [G])
            for i in range(KB):
                tgt = psA if i < 12 else psB
                ii = i if i < 12 else i - 12
                P.op("pe", lambda e, i=i, tgt=tgt, ii=ii, G=G: e.matmul(tgt[:, ii // 4, (ii % 4) * 128:(ii % 4 + 1) * 128], lhsT=G[:, i * 128:(i + 1) * 128], rhs=J[:],
                                                                        start=True, stop=True), [G, J], [tgt])
            P.op("dve", lambda e, E=E: e.tensor_copy(out=E[:, 0:1536], in_=psA[:].rearrange("p a b -> p (a b)")), [psA], [E])
            P.op("dve", lambda e, E=E: e.tensor_copy(out=E[:, 1536:KB * 128], in_=psB[:].rearrange("p a b -> p (a b)")[:, 0:KB * 128 - 1536]), [psB], [E])
            P.dma("sp", EBD[h], E[:], [E], [(EBD.tensor.name, h)])
        Lx = max(p[1] for p in cfg["pieces"])
        Lo = max(p[3] for p in cfg["pieces"])
        KTh = [sb(f"KTh{i}", [HD + 1, Lx], BF16) for i in range(2)]
        Vh = [sb(f"Vh{i}", [128, Lx // 128, HD + 1], BF16) for i in range(2)]
        QTh = [sb(f"QTh{i}", [HD + 1, Lo], BF16) for i in range(2)]
        EBh = [sb(f"EBh{i}", [128, KB * 128], BF16) for i in range(2)]
        Et = [sb(f"Et{i}", [128, KB * 128], BF16) for i in range(2)]
        Pt = [sb(f"Pt{i}", [128, KB * 128], BF16) for i in range(2)]
        yat = [sb(f"yat{i}", [128, Lo // 128, HD]) for i in range(2)]
        rs = sb("rs_a", [128, 2])
        n = 0
        nq = 0
        for (ext0, Lext, own0, Lown) in cfg["pieces"]:
            for h in range(NH):
                i = n % 2
                n += 1
                kt, vh, qt_, eb, ya = KTh[i], Vh[i], QTh[i], EBh[i], yat[i]
                P.dma("sp", kt[:, 0:Lext], KT[h, :, ext0:ext0 + Lext], [(KT.tensor.name, h), ("KTrow", h)], [kt])
                P.dma("sp", vh[:, 0:Lext // 128, :], VE[ext0:ext0 + Lext, h * (HD + 1):(h + 1) * (HD + 1)].rearrange("(t p) c -> p t c", p=128), [VE], [vh])
                P.dma("sp", qt_[:, 0:Lown], QT[h, :, own0:own0 + Lown], [(QT.tensor.name, h), ("QTrow", h)], [qt_])
                P.dma("sp", eb[:], EBD[h], [(EBD.tensor.name, h)], [eb])
                for q in range(Lown // 128):
                    j = nq % 2
                    nq += 1
                    et, pt, pq = Et[j], Pt[j], po[j]
                    qs = qt_[:, q * 128:(q + 1) * 128]
                    for i2 in range(KB):
                        if i2 < 9:
                            dst = psA[:, i2 // 4, (i2 % 4) * 128:(i2 % 4 + 1) * 128]
                            key = psA
                        else:
                            dst = psB[:, (i2 - 9) // 4, ((i2 - 9) % 4) * 128:((i2 - 9) % 4 + 1) * 128]
                            key = psB
                        P.op("pe", lambda e, dst=dst, i2=i2, q=q, qs=qs: e.matmul(dst, lhsT=kt[:, (q + i2) * 128:(q + i2 + 1) * 128], rhs=qs, start=True, stop=True),
                             [kt, qt_], [key])
                    for (ps_, c0, w, o0) in ((psA, 0, 512, 0), (psA, 1, 512, 512), (psA, 2, 128, 1024), (psB, 0, 512, 1152), (psB, 1, 512, 1664)):
                        P.op("act", lambda e, ps_=ps_, c0=c0, w=w, o0=o0: e.activation(out=et[:, o0:o0 + w], in_=ps_[:, c0, 0:w], func=AF.Exp), [ps_], [(et.name, o0 >= 1152)])
                    P.op("dve", lambda e: e.tensor_tensor(out=pt[:, 0:1152], in0=et[:, 0:1152], in1=eb[:, 0:1152], op=ALU.mult), [(et.name, False), eb], [(pt.name, False)])
                    P.op("dve", lambda e: e.tensor_tensor(out=pt[:, 1152:], in0=et[:, 1152:], in1=eb[:, 1152:], op=ALU.mult), [(et.name, True), eb], [(pt.name, True)])
                    for i2 in range(KB):
                        P.op("pe", lambda e, i2=i2, q=q: e.matmul(pq[:, 0:HD + 1], lhsT=pt[:, i2 * 128:(i2 + 1) * 128], rhs=vh[:, q + i2, :], start=(i2 == 0), stop=(i2 == KB - 1)),
                             [(pt.name, i2 >= 9), vh], [pq])
                    P.op("dve", lambda e, j=j: e.reciprocal(out=rs[:, j:j + 1], in_=pq[:, HD:HD + 1]), [pq], [(rs.name, j)])
                    P.op("dve", lambda e, j=j, q=q: e.tensor_scalar(out=ya[:, q, :], in0=pq[:, 0:HD], scalar1=rs[:, j:j + 1], scalar2=None, op0=ALU.mult),
                         [pq, (rs.name, j)], [ya])
                P.dma("pool", YA[own0:own0 + Lown, h * HD:(h + 1) * HD].rearrange("(t p) c -> p t c", p=128), ya[:, 0:Lown // 128, :], [ya], [YA])
        P.barrier()


NEXP = 32
DE = 1024
BLK = 128
NBLK = 128
NSLOT = NBLK * BLK


def indirect_gather(P, out, in_, idx_ap, reads, writes):
    P.dma("pool", out, in_, reads, writes, fn=lambda e: e.indirect_dma_start(
        out=out, out_offset=None, in_=in_, in_offset=bass.IndirectOffsetOnAxis(ap=idx_ap, axis=0)))


def indirect_scatter(P, out, in_, idx_ap, reads, writes):
    P.dma("pool", out, in_, reads, writes, fn=lambda e: e.indirect_dma_start(
        out=out, out_offset=bass.IndirectOffsetOnAxis(ap=idx_ap, axis=0), in_=in_, in_offset=None))


def phase3(P, nc, cfg, d, YHo, YA, X1, H2B, AAd, GTd):
    import contextlib
    NOWN = cfg["NOWN"]
    NT = NOWN // 128
    CH = 1280 // 128
    CA = DA // 128
    with contextlib.ExitStack() as es:
        sb = lambda n, shp, dt=F32: es.enter_context(nc.sbuf_tensor(n, shp, dt))
        Wo = sb("Wo", [128, DK, D], BF16)
        gh, ga, gfb, Wr, rbb = sb("gh3", [128, CH]), sb("ga3", [128, CA]), sb("gfb", [128, D]), sb("Wr", [128, DK, 36]), sb("rbb", [128, 36])
        identf, identb, ones, epsT = sb("identf3", [128, 128]), sb("identb3", [128, 128], BF16), sb("ones3", [128, 128], BF16), sb("epsT3", [128, 1])
        stage = sb("stage3", [128, D])
        yh = [sb(f"yh{i}", [128, CH, 512], BF16) for i in range(2)]
        sq = sb("sq3", [128, CH, 512], BF16)
        rh = sb("rh3", [128, 512])
        yhn = sb("yhn", [128, CH, 512], BF16)
        yat = [sb(f"yat3{i}", [128, DA]) for i in range(2)]
        yan = sb("yan", [128, DA], BF16)
        yaT = sb("yaT", [128, CA, 512], BF16)
        xo = [sb(f"xo{i}", [128, D]) for i in range(2)]
        x1 = sb("x1t", [128, D])
        h2 = sb("h2t", [128, D])
        h2b = [sb(f"h2b{i}", [128, D], BF16) for i in range(2)]
        h2T = sb("h2T", [128, DK, 128])
        junk = sb("junk3", [128, D], BF16)
        sm = sb("sm3", [128, 16])
        lg = sb("lg3", [128, 36])
        ohg, eg, esel, oh1, e2, oh2 = sb("ohg", [128, 4]), sb("eg", [128, 4]), sb("esel", [128, 8]), sb("oh1", [128, 8]), sb("e2r", [128, 8]), sb("oh2", [128, 8])
        AA = sb("AA3", [128, NT, 64])
        GT = sb("GT3", [128, NT, 2])
        pss = es.enter_context(nc.psum_tensor("pss3", [128, 512], F32))
        ptr = es.enter_context(nc.psum_tensor("ptr3", [128, CA, 128], BF16))
        po = [es.enter_context(nc.psum_tensor(f"po3{i}", [128, 512], F32)) for i in range(4)]
        pt2 = es.enter_context(nc.psum_tensor("pt23", [128, 4, 128], F32))
        pr = es.enter_context(nc.psum_tensor("pr3", [128, 64], F32))
        P.op("pool", lambda e: e.memset(ones[:], 1.0), [], [ones])
        P.op("pool", lambda e: e.memset(epsT[:], EPS), [], [epsT])
        P.op("pool", lambda e: e.memset(sm[:], 0.0), [], [sm])
        P.dma("sp", gh[:], d["gh"])
        P.dma("sp", ga[:], d["ga"])
        P.dma("sp", gfb[:], bcast_rows(d["gf"], 128))
        P.dma("sp", rbb[:], bcast_rows(d["rb"], 128))
        P.dma("sp", Wr[:], d["wr"])
        P.dma("sp", identf[:], d["ident"])
        P.op("dve", lambda e: e.tensor_copy(out=identb[:], in_=identf[:]), [identf], [identb])
        for j in range(DK):
            P.dma("sp", stage[:], d["w_out"][j * 128:(j + 1) * 128, :], None, [stage])
            P.op("dve" if j % 2 else "act", (lambda e, j=j: e.tensor_copy(out=Wo[:, j, :], in_=stage[:])) if j % 2 else (lambda e, j=j: e.copy(out=Wo[:, j, :], in_=stage[:])),
                 [stage], [Wo])
        for st in range(NOWN // 512):
            t0 = st * 512
            y = yh[st % 2]
            P.dma("sp", y[:], YHo[:, t0:t0 + 512].rearrange("(k p) t -> p k t", p=128), [YHo], [y])
            P.op("pool", lambda e, y=y: e.tensor_tensor(out=sq[:], in0=y[:], in1=y[:], op=ALU.mult), [y], [sq])
            for k in range(CH):
                P.op("pe", lambda e, k=k: e.matmul(pss[:], lhsT=ones[:], rhs=sq[:, k, :], start=(k == 0), stop=(k == CH - 1)), [ones, sq], [pss])
            P.op("act", lambda e: e.activation(out=rh[:], in_=pss[:], func=AF.Sqrt, bias=epsT[:], scale=1.0 / 1280), [pss, epsT], [rh])
            P.op("dve", lambda e: e.reciprocal(out=rh[:], in_=rh[:]), [rh], [rh])
            for k in range(CH):
                P.op("dve", lambda e, k=k, y=y: e.scalar_tensor_tensor(out=yhn[:, k, :], in0=y[:, k, :], scalar=gh[:, k:k + 1], in1=rh[:], op0=ALU.mult, op1=ALU.mult),
                     [y, gh, rh], [(yhn.name, k)])
            for s in range(4):
                tt = st * 4 + s
                r0 = t0 + s * 128
                ya, xin, hb = yat[tt % 2], xo[tt % 2], h2b[tt % 2]
                P.dma("sp", ya[:], YA[r0:r0 + 128, :], [YA], [ya])
                P.dma("sp", xin[:], d["x_own_fn"](r0), None, [xin])
                P.op("act", lambda e, ya=ya: e.activation(out=junk[:, 0:DA], in_=ya[:], func=AF.Square, accum_out=sm[:, 0:1]), [ya], [junk, (sm.name, 0)])
                P.op("act", lambda e: e.activation(out=sm[:, 1:2], in_=sm[:, 0:1], func=AF.Sqrt, bias=epsT[:], scale=1.0 / DA), [(sm.name, 0), epsT], [(sm.name, 1)])
                P.op("dve", lambda e: e.reciprocal(out=sm[:, 1:2], in_=sm[:, 1:2]), [(sm.name, 1)], [(sm.name, 1)])
                P.op("pool", lambda e: e.memset(sm[:, 0:1], 0.0), [], [(sm.name, 0)])
                P.op("act", lambda e, ya=ya: e.activation(out=yan[:], in_=ya[:], func=AF.Identity, scale=sm[:, 1:2]), [ya, (sm.name, 1)], [yan])
                for k in range(CA):
                    P.op("pe", lambda e, k=k: e.transpose(out=ptr[:, k, :], in_=yan[:, k * 128:(k + 1) * 128], identity=identb[:]), [yan, identb], [ptr])
                for k in range(CA):
                    P.op("dve", lambda e, k=k, s=s: e.tensor_scalar(out=yaT[:, k, s * 128:(s + 1) * 128], in0=ptr[:, k, :], scalar1=ga[:, k:k + 1], scalar2=None, op0=ALU.mult),
                         [ptr, ga], [(yaT.name, s)])
                for n in range(4):
                    for k in range(DK):
                        if k < CH:
                            lhs, key = yhn[:, k, s * 128:(s + 1) * 128], (yhn.name, k)
                        else:
                            lhs, key = yaT[:, k - CH, s * 128:(s + 1) * 128], (yaT.name, s)
                        P.op("pe", lambda e, n=n, k=k, lhs=lhs: e.matmul(po[n][:], lhsT=lhs, rhs=Wo[:, k, n * 512:(n + 1) * 512], start=(k == 0), stop=(k == DK - 1)),
                             [key, Wo], [po[n]])
                    P.op("dve", lambda e, n=n, xin=xin: e.tensor_tensor(out=x1[:, n * 512:(n + 1) * 512], in0=po[n][:], in1=xin[:, n * 512:(n + 1) * 512], op=ALU.add),
                         [po[n], xin], [x1])
                P.dma("pool", X1[r0:r0 + 128, :], x1[:], [x1], [X1])
                P.op("act", lambda e: e.activation(out=junk[:], in_=x1[:], func=AF.Square, accum_out=sm[:, 2:3]), [x1], [junk, (sm.name, 2)])
                P.op("act", lambda e: e.activation(out=sm[:, 3:4], in_=sm[:, 2:3], func=AF.Sqrt, bias=epsT[:], scale=1.0 / D), [(sm.name, 2), epsT], [(sm.name, 3)])
                P.op("dve", lambda e: e.reciprocal(out=sm[:, 3:4], in_=sm[:, 3:4]), [(sm.name, 3)], [(sm.name, 3)])
                P.op("pool", lambda e: e.memset(sm[:, 2:3], 0.0), [], [(sm.name, 2)])
                P.op("dve", lambda e: e.scalar_tensor_tensor(out=h2[:], in0=x1[:], scalar=sm[:, 3:4], in1=gfb[:], op0=ALU.mult, op1=ALU.mult), [x1, (sm.name, 3), gfb], [h2])
                P.op("act", lambda e, hb=hb: e.copy(out=hb[:], in_=h2[:]), [h2], [hb])
                P.dma("pool", H2B[r0:r0 + 128, :], hb[:], [hb], [H2B])
                for jj in range(4):
                    for j2 in range(4):
                        j = jj * 4 + j2
                        P.op("pe", lambda e, j=j, j2=j2: e.transpose(out=pt2[:, j2, :], in_=h2[:, j * 128:(j + 1) * 128], identity=identf[:]), [h2, identf], [pt2])
                    P.op("act" if jj % 2 else "dve", (lambda e, jj=jj: e.copy(out=h2T[:, jj * 4:(jj + 1) * 4, :], in_=pt2[:])) if jj % 2 else
                         (lambda e, jj=jj: e.tensor_copy(out=h2T[:, jj * 4:(jj + 1) * 4, :], in_=pt2[:])), [pt2], [h2T])
                for j in range(DK):
                    P.op("pe", lambda e, j=j: e.matmul(pr[:, 0:36], lhsT=h2T[:, j, :], rhs=Wr[:, j, :], start=(j == 0), stop=(j == DK - 1)), [h2T, Wr], [pr])
                P.op("dve", lambda e: e.tensor_tensor(out=lg[:], in0=pr[:, 0:36], in1=rbb[:], op=ALU.add), [pr, rbb], [lg])
                R = lambda c: (sm.name, c)
                lgg = lg[:, 0:4]
                lge = lg[:, 4:36].rearrange("p (g j) -> p g j", g=4)
                P.op("dve", lambda e: e.tensor_reduce(out=sm[:, 4:5], in_=lgg, axis=AX.X, op=ALU.max), [lg], [R(4)])
                P.op("dve", lambda e: e.tensor_scalar(out=ohg[:], in0=lgg, scalar1=sm[:, 4:5], scalar2=None, op0=ALU.is_equal), [lg, R(4)], [ohg])
                P.op("dve", lambda e: e.tensor_scalar(out=sm[:, 5:6], in0=sm[:, 4:5], scalar1=-1.0, scalar2=None, op0=ALU.mult), [R(4)], [R(5)])
                P.op("act", lambda e: e.activation(out=eg[:], in_=lgg, func=AF.Exp, bias=sm[:, 5:6], scale=1.0, accum_out=sm[:, 6:7]), [lg, R(5)], [eg, R(6)])
                P.op("dve", lambda e: e.reciprocal(out=sm[:, 7:8], in_=sm[:, 6:7]), [R(6)], [R(7)])
                P.op("pool", lambda e: e.memset(sm[:, 6:7], 0.0), [], [R(6)])
                P.op("dve", lambda e: e.tensor_scalar(out=esel[:], in0=lge[:, 0, :], scalar1=ohg[:, 0:1], scalar2=None, op0=ALU.mult), [lg, ohg], [esel])
                for g in range(1, 4):
                    P.op("dve", lambda e, g=g: e.scalar_tensor_tensor(out=esel[:], in0=lge[:, g, :], scalar=ohg[:, g:g + 1], in1=esel[:], op0=ALU.mult, op1=ALU.add),
                         [lg, ohg, esel], [esel])
                P.op("dve", lambda e: e.tensor_reduce(out=sm[:, 8:9], in_=esel[:], axis=AX.X, op=ALU.max), [esel], [R(8)])
                P.op("dve", lambda e: e.tensor_scalar(out=oh1[:], in0=esel[:], scalar1=sm[:, 8:9], scalar2=None, op0=ALU.is_equal), [esel, R(8)], [oh1])
                P.op("dve", lambda e: e.scalar_tensor_tensor(out=e2[:], in0=oh1[:], scalar=-1.0e9, in1=esel[:], op0=ALU.mult, op1=ALU.add), [oh1, esel], [e2])
                P.op("dve", lambda e: e.tensor_reduce(out=sm[:, 9:10], in_=e2[:], axis=AX.X, op=ALU.max), [e2], [R(9)])
                P.op("dve", lambda e: e.tensor_scalar(out=oh2[:], in0=e2[:], scalar1=sm[:, 9:10], scalar2=None, op0=ALU.is_equal), [e2, R(9)], [oh2])
                P.op("dve", lambda e: e.tensor_tensor(out=sm[:, 10:11], in0=sm[:, 9:10], in1=sm[:, 8:9], op=ALU.subtract), [R(9), R(8)], [R(10)])
                P.op("act", lambda e: e.activation(out=sm[:, 11:12], in_=sm[:, 10:11], func=AF.Exp), [R(10)], [R(11)])
                P.op("dve", lambda e: e.tensor_scalar(out=sm[:, 12:13], in0=sm[:, 11:12], scalar1=1.0, scalar2=None, op0=ALU.add), [R(11)], [R(12)])
                P.op("dve", lambda e: e.reciprocal(out=sm[:, 12:13], in_=sm[:, 12:13]), [R(12)], [R(12)])
                P.op("dve", lambda e, tt=tt: e.tensor_tensor(out=GT[:, tt, 0:1], in0=sm[:, 12:13], in1=sm[:, 7:8], op=ALU.mult), [R(12), R(7)], [GT])
                P.op("dve", lambda e, tt=tt: e.tensor_tensor(out=GT[:, tt, 1:2], in0=GT[:, tt, 0:1], in1=sm[:, 11:12], op=ALU.mult), [GT, R(11)], [GT])
                P.op("dve", lambda e, tt=tt: e.tensor_tensor(out=AA[:, tt, 0:32].rearrange("p (g j) -> p g j", g=4), in0=bc_last(ohg[:], 8), in1=bc_mid(oh1[:], 4), op=ALU.mult),
                     [ohg, oh1], [AA])
                P.op("dve", lambda e, tt=tt: e.tensor_tensor(out=AA[:, tt, 32:64].rearrange("p (g j) -> p g j", g=4), in0=bc_last(ohg[:], 8), in1=bc_mid(oh2[:], 4), op=ALU.mult),
                     [ohg, oh2], [AA])
        P.dma("sp", AAd, AA[:], [AA], [AAd])
        P.dma("sp", GTd, GT[:], [GT], [GTd])
        P.barrier()


def phase_moe(P, nc, cfg, d, X1, H2B, AAd, GTd, XB, YB, y_out):
    import contextlib
    NOWN = cfg["NOWN"]
    NT = NOWN // 128
    NBLK = cfg.get("NBLK", 128)
    with contextlib.ExitStack() as es:
        sb = lambda n, shp, dt=F32: es.enter_context(nc.sbuf_tensor(n, shp, dt))
        es0 = es.enter_context(contextlib.ExitStack())
        sb0 = lambda n, shp, dt=F32: es0.enter_context(nc.sbuf_tensor(n, shp, dt))
        GT = sb("GTm", [128, NT, 2])
        identb = sb("identbm", [128, 128], BF16)
        dst = sb("dsti", [128, NT, 2], I32)
        idxg, idxd = sb("idxg", [128, NBLK, DK], I32), sb("idxd", [128, NBLK, DK // 2], I32)
        AA = sb0("AAm", [128, NT, 64])
        identf, onesf, utri = sb0("identfm", [128, 128]), sb0("onesfm", [128, 128]), sb0("utrim", [128, 128])
        ltm, bpos, offg = sb0("ltm_sb", [128, 32, 32]), sb0("bposm", [128, 1]), sb0("offgm", [128, DK])
        cnt, padi, pad, pend, pstart, base = sb0("cntm", [128, 32]), sb0("padi", [128, 32], I32), sb0("padm", [128, 32]), sb0("pendm", [128, 32]), sb0("pstm", [128, 32]), sb0("basem", [128, 32])
        tmp3 = sb0("tmp3m", [128, 32, 32])
        off, prod = sb0("offm", [128, 32]), sb0("prodm", [128, 32])
        dstf = sb0("dstf", [128, NT, 2])
        cmp_, bef, be2 = sb0("cmpm", [128, 32]), sb0("befm", [128, 1]), sb0("be2m", [128, 128])
        bebc = sb0("bebc", [128, 128])
        idxf = sb0("idxfm", [128, NBLK, DK])
        pm = es.enter_context(nc.psum_tensor("pmm", [128, 128], F32))
        P.dma("sp", AA[:], AAd)
        P.dma("sp", GT[:], GTd)
        P.dma("sp", identf[:], d["ident"])
        P.dma("sp", utri[:], d["utri"])
        P.dma("sp", ltm[:].rearrange("p a b -> p (a b)"), bcast_rows(d["ltm"], 128))
        P.dma("sp", bpos[:], d["bpos"])
        P.dma("sp", offg[:], d["offg"])
        P.op("dve", lambda e: e.tensor_copy(out=identb[:], in_=identf[:]), [identf], [identb])
        P.op("pool", lambda e: e.memset(onesf[:], 1.0), [], [onesf])
        P.op("pool", lambda e: e.memset(base[:], 0.0), [], [base])
        for i in range(NT):
            P.op("pe", lambda e, i=i: e.matmul(pm[:, 0:64], lhsT=onesf[:], rhs=AA[:, i, :], start=(i == 0), stop=(i == NT - 1)), [onesf, AA], [pm])
        P.op("dve", lambda e: e.tensor_copy(out=cnt[:], in_=pm[:, 0:32]), [pm], [cnt])
        P.op("dve", lambda e: e.tensor_tensor(out=cnt[:], in0=cnt[:], in1=pm[:, 32:64], op=ALU.add), [cnt, pm], [cnt])
        P.op("dve", lambda e: e.tensor_scalar(out=padi[:], in0=cnt[:], scalar1=float(BLK - 1), scalar2=None, op0=ALU.add), [cnt], [padi])
        P.op("dve", lambda e: e.tensor_scalar(out=padi[:], in0=padi[:], scalar1=7, scalar2=7, op0=ALU.arith_shift_right, op1=ALU.logical_shift_left), [padi], [padi])
        P.op("dve", lambda e: e.tensor_copy(out=pad[:], in_=padi[:]), [padi], [pad])
        P.op("dve", lambda e: e.tensor_tensor(out=tmp3[:], in0=bc_mid(pad[:], 32), in1=ltm[:], op=ALU.mult), [pad, ltm], [tmp3])
        P.op("dve", lambda e: e.reduce_sum(out=pend[:], in_=tmp3[:], axis=AX.X), [tmp3], [pend])
        P.op("dve", lambda e: e.tensor_tensor(out=pstart[:], in0=pend[:], in1=pad[:], op=ALU.subtract), [pend, pad], [pstart])
        for i in range(NT):
            P.op("pe", lambda e, i=i: e.matmul(pm[:, 0:64], lhsT=utri[:], rhs=AA[:, i, :], start=True, stop=True), [utri, AA], [pm])
            P.op("pe", lambda e, i=i: e.matmul(pm[:, 64:128], lhsT=onesf[:], rhs=AA[:, i, :], start=True, stop=True), [onesf, AA], [pm])
            P.op("dve", lambda e: e.tensor_tensor(out=off[:], in0=pstart[:], in1=base[:], op=ALU.add), [pstart, base], [off])
            P.op("dve", lambda e: e.tensor_tensor(out=prod[:], in0=off[:], in1=pm[:, 0:32], op=ALU.add), [off, pm], [prod])
            P.op("dve", lambda e, i=i: e.tensor_tensor(out=prod[:], in0=prod[:], in1=AA[:, i, 0:32], op=ALU.mult), [prod, AA], [prod])
            P.op("dve", lambda e, i=i: e.reduce_sum(out=dstf[:, i, 0:1], in_=prod[:], axis=AX.X), [prod], [dstf])
            P.op("dve", lambda e: e.tensor_tensor(out=off[:], in0=off[:], in1=pm[:, 64:96], op=ALU.add), [off, pm], [off])
            P.op("dve", lambda e: e.tensor_tensor(out=prod[:], in0=off[:], in1=pm[:, 32:64], op=ALU.add), [off, pm], [prod])
            P.op("dve", lambda e, i=i: e.tensor_tensor(out=prod[:], in0=prod[:], in1=AA[:, i, 32:64], op=ALU.mult), [prod, AA], [prod])
            P.op("dve", lambda e, i=i: e.reduce_sum(out=dstf[:, i, 1:2], in_=prod[:], axis=AX.X), [prod], [dstf])
            P.op("dve", lambda e: e.tensor_tensor(out=base[:], in0=base[:], in1=pm[:, 64:96], op=ALU.add), [base, pm], [base])
            P.op("dve", lambda e: e.tensor_tensor(out=base[:], in0=base[:], in1=pm[:, 96:128], op=ALU.add), [base, pm], [base])
        P.op("dve", lambda e: e.tensor_copy(out=dst[:], in_=dstf[:]), [dstf], [dst])
        P.op("dve", lambda e: e.tensor_scalar(out=cmp_[:], in0=pend[:], scalar1=bpos[:, 0:1], scalar2=None, op0=ALU.is_le), [pend, bpos], [cmp_])
        P.op("dve", lambda e: e.reduce_sum(out=bef[:], in_=cmp_[:], axis=AX.X), [cmp_], [bef])
        P.op("dve", lambda e: e.tensor_scalar(out=bef[:], in0=bef[:], scalar1=float(NEXP - 1), scalar2=None, op0=ALU.min), [bef], [bef])
        P.op("dve", lambda e: e.tensor_copy(out=bebc[:], in_=bc_col(bef[:, 0:1], 128)), [bef], [bebc])
        P.op("pe", lambda e: e.matmul(pm[:], lhsT=bebc[:], rhs=identf[:], start=True, stop=True), [bebc, identf], [pm])
        P.op("dve", lambda e: e.tensor_copy(out=be2[:], in_=pm[:]), [pm], [be2])
        for (scale, tgt, nj) in ((float(D), idxg, DK), (float(DE), idxd, DK // 2)):
            P.op("dve", lambda e, scale=scale, nj=nj: e.scalar_tensor_tensor(out=idxf[:, :, 0:nj], in0=bc_last(be2[:, 0:NBLK], nj), scalar=scale, in1=bc_mid(offg[:, 0:nj], NBLK),
                                                                              op0=ALU.mult, op1=ALU.add), [be2, offg], [idxf])
            P.op("dve", lambda e, tgt=tgt, nj=nj: e.tensor_copy(out=tgt[:], in_=idxf[:, :, 0:nj]), [idxf], [tgt])
        es1 = es.enter_context(contextlib.ExitStack())
        hb = [es1.enter_context(nc.sbuf_tensor(f"hbm{i}", [128, D], BF16)) for i in range(2)]
        for i in range(NT):
            h = hb[i % 2]
            P.dma("sp", h[:], H2B[i * 128:(i + 1) * 128, :], [H2B], [h])
            for k in range(2):
                indirect_scatter(P, XB, h[:], dst[:, i, k:k + 1], [h, dst], [XB])
        P.barrier()
        es1.close()
        es0.close()
        es2 = es.enter_context(contextlib.ExitStack())
        sb2 = lambda n, shp, dt=F32: es2.enter_context(nc.sbuf_tensor(n, shp, dt))
        stg = [sb2(f"wst{i}", [128, 4, DE]) for i in range(2)]
        Wg, Wu, Wd = sb2("Wgb", [128, DK, DE], BF16), sb2("Wub", [128, DK, DE], BF16), sb2("Wdb", [128, DK // 2, D], BF16)
        xb = [sb2(f"xbm{i}", [128, D], BF16) for i in range(2)]
        xbT = [sb2(f"xbT{i}", [128, DK, 128], BF16) for i in range(2)]
        sg = [sb2(f"sgm{i}", [128, 128]) for i in range(2)]
        aT = sb2("aTm", [128, DK // 2, 128], BF16)
        yb = [sb2(f"ybm{i}", [128, D]) for i in range(2)]
        ptx = es2.enter_context(nc.psum_tensor("ptxm", [128, 8, 128], BF16))
        pg = [es2.enter_context(nc.psum_tensor(f"pgm{i}", [128, 128], F32)) for i in range(2)]
        pu = [es2.enter_context(nc.psum_tensor(f"pum{i}", [128, 128], F32)) for i in range(2)]
        pd = [es2.enter_context(nc.psum_tensor(f"pdm{i}", [128, 512], F32)) for i in range(2)]
        ns = 0
        ce = 0
        cast_engs = ("act", "dve", "pool")
        for b in range(NBLK):
            x, xT, y = xb[b % 2], xbT[b % 2], yb[b % 2]
            P.dma("sp", x[:], XB[b * BLK:(b + 1) * BLK, :], [XB], [x])
            for hh in range(2):
                for j2 in range(8):
                    j = hh * 8 + j2
                    P.op("pe", lambda e, j=j, j2=j2: e.transpose(out=ptx[:, j2, :], in_=x[:, j * 128:(j + 1) * 128], identity=identb[:]), [x, identb], [ptx])
                P.op("dve", lambda e, hh=hh: e.tensor_copy(out=xT[:, hh * 8:(hh + 1) * 8, :], in_=ptx[:]), [ptx], [xT])
            for (Wsrc, Wdst, idx, nchunk, width) in ((d["w_gate"], Wg, idxg, DK, DE), (d["w_up"], Wu, idxg, DK, DE), (d["w_down"], Wd, idxd, DK // 2, D)):
                per = 4 * DE // width
                for c0 in range(0, nchunk, per):
                    st = stg[ns % 2]
                    ns += 1
                    stv = st[:].rearrange("p a b -> p (a b)").rearrange("p (a b) -> p a b", b=width)
                    for c in range(per):
                        indirect_gather(P, stv[:, c, :], Wsrc, idx[:, b, c0 + c:c0 + c + 1], [idx], [st])
                    eng = cast_engs[ce % 3]
                    ce += 1
                    dstv = Wdst[:, c0:c0 + per, :]
                    if eng == "act":
                        P.op("act", lambda e, dstv=dstv, stv=stv: e.copy(out=dstv, in_=stv), [st], [Wdst])
                    else:
                        P.op(eng, lambda e, dstv=dstv, stv=stv: e.tensor_copy(out=dstv, in_=stv), [st], [Wdst])
            for fc in range(DE // 128):
                pgg, puu, sgg = pg[fc % 2], pu[fc % 2], sg[fc % 2]
                for j in range(DK):
                    P.op("pe", lambda e, j=j, fc=fc, pgg=pgg: e.matmul(pgg[:], lhsT=Wg[:, j, fc * 128:(fc + 1) * 128], rhs=xT[:, j, :], start=(j == 0), stop=(j == DK - 1)),
                         [Wg, xT], [pgg])
                for j in range(DK):
                    P.op("pe", lambda e, j=j, fc=fc, puu=puu: e.matmul(puu[:], lhsT=Wu[:, j, fc * 128:(fc + 1) * 128], rhs=xT[:, j, :], start=(j == 0), stop=(j == DK - 1)),
                         [Wu, xT], [puu])
                P.op("act", lambda e, pgg=pgg, sgg=sgg: e.activation(out=sgg[:], in_=pgg[:], func=AF.Silu), [pgg], [sgg])
                P.op("dve", lambda e, fc=fc, puu=puu, sgg=sgg: e.tensor_tensor(out=aT[:, fc, :], in0=puu[:], in1=sgg[:], op=ALU.mult), [puu, sgg], [aT])
            for n in range(4):
                pdd = pd[n % 2]
                for fc in range(DE // 128):
                    P.op("pe", lambda e, n=n, fc=fc, pdd=pdd: e.matmul(pdd[:], lhsT=aT[:, fc, :], rhs=Wd[:, fc, n * 512:(n + 1) * 512], start=(fc == 0), stop=(fc == DE // 128 - 1)),
                         [aT, Wd], [pdd])
                if n % 2 == 0:
                    P.op("act", lambda e, n=n, pdd=pdd: e.copy(out=y[:, n * 512:(n + 1) * 512], in_=pdd[:]), [pdd], [y])
                else:
                    P.op("dve", lambda e, n=n, pdd=pdd: e.tensor_copy(out=y[:, n * 512:(n + 1) * 512], in_=pdd[:]), [pdd], [y])
            P.dma("sp", YB[b * BLK:(b + 1) * BLK, :], y[:], [y], [YB])
        P.barrier()
        es2.close()
        y1 = [sb(f"y1m{i}", [128, D]) for i in range(2)]
        y2 = [sb(f"y2m{i}", [128, D]) for i in range(2)]
        xr = [sb(f"xrm{i}", [128, D]) for i in range(2)]
        for i in range(NT):
            a, bb, x = y1[i % 2], y2[i % 2], xr[i % 2]
            P.dma("sp", x[:], X1[i * 128:(i + 1) * 128, :], [X1], [x])
            indirect_gather(P, a[:], YB, dst[:, i, 0:1], [YB, dst], [a])
            indirect_gather(P, bb[:], YB, dst[:, i, 1:2], [YB, dst], [bb])
            P.op("dve", lambda e, i=i, a=a, x=x: e.scalar_tensor_tensor(out=x[:], in0=a[:], scalar=GT[:, i, 0:1], in1=x[:], op0=ALU.mult, op1=ALU.add), [a, GT, x], [x])
            P.op("dve", lambda e, i=i, bb=bb, x=x: e.scalar_tensor_tensor(out=x[:], in0=bb[:], scalar=GT[:, i, 1:2], in1=x[:], op0=ALU.mult, op1=ALU.add), [bb, GT, x], [x])
            P.dma("sp", y_out[i * 128:(i + 1) * 128, :], x[:], [x], [y_out])
        P.barrier()


LP, LS_ = 8192, 16384
CHN_ALL = 1280
NOWN_ = 6144
NE_ = 10240
MIN_DECAY_ = math.log(1e-2) / 1.5
MAX_DECAY_ = math.log(1e-2) / 0.3


def _circle_tables(L):
    pos = np.arange(L, dtype=np.float64)
    t = np.linspace(0.0, 1.0, L)
    bands = np.linspace(1e-4, 15, 16)
    ang = (2 * np.pi / L) * pos[:, None] * bands[None, :]
    feats = np.concatenate([t[:, None], np.cos(ang), -np.sin(ang)], -1)
    idx2 = np.concatenate([[0], np.arange(L - 1, 0, -1)])
    f2 = np.concatenate([feats, feats[idx2]], 0)
    t2 = np.concatenate([t, t[idx2]])
    return np.ascontiguousarray(f2.T, dtype=np.float32), np.ascontiguousarray(t2[None], dtype=np.float32)


def build_program():
    nc = bass.Bass("TRN2", target_bir_lowering=False)
    di = lambda n, shp, dt=F32: nc.dram_tensor(n, list(shp), dt, kind="ExternalInput").ap()
    dn = lambda n, shp, dt=F32: nc.dram_tensor(n, list(shp), dt, kind="Internal").ap()
    T = LP + LS_
    a = {}
    a["x_seq"] = di("x_seq", [T, D])
    a["x_ext"] = di("x_ext", [NE_, D])
    a["kvalid"] = di("kvalid", [1, NE_], BF16)
    a["ones_row"] = di("ones_row", [1, NOWN_], BF16)
    a["w_h"] = di("w_h", [D, 2 * CHN_ALL])
    a["w_g2"] = di("w_g2", [D, CHN_ALL])
    a["wq"], a["wk"], a["wv"] = di("wq", [D, DA]), di("wk", [D, DA]), di("wv", [D, DA])
    a["gmix"] = di("gmix", [128, DK])
    a["scw_h"] = di("scw_h", [128, 20, 4])
    a["scw_g2"] = di("scw_g2", [128, 10, 4])
    a["ident"] = di("ident", [128, 128])
    a["gqk"] = di("gqk", [128, 2])
    a["bd"] = di("bd", [128, 128])
    a["relb"] = di("relb", [32, NH])
    a["sel"] = di("sel", [32, 2432])
    a["jm"] = di("jm", [128, 128])
    fdc = dict(w1=di("f_w1", [33, 64]), w2=di("f_w2", [64, 64]), w3=di("f_w3", [64, 64]), fvec=di("f_vec", [64, 4]),
               woutd=di("f_woutd", [64, 2, 2 * CHN_ALL]), ndelta=di("f_ndelta", [128, 20]))
    a["skip"] = di("skip", [1, 2 * CHN_ALL])
    fl = {}
    for L, tg in ((LP, "p"), (LS_, "s")):
        N1 = 2 * L // 128
        Hown = 32 if L == LP else 16
        cc = {k: di(f"c{tg}_{k}", v.shape) for k, v in fft_consts(L).items()}
        cc["F4own"] = di(f"c{tg}_F4own", [128, N1 // 128, 2, Hown])
        cc["SEL"] = di(f"c{tg}_SEL", [N1 // 2, Hown])
        fd = dict(fdc)
        fd["featsT2"] = di(f"f{tg}_feats", [33, 2 * L])
        fd["trow2"] = di(f"f{tg}_trow", [1, 2 * L])
        fl[L] = (cc, fd, Hown, N1)
    d3 = dict(w_out=di("w_out", [D, D]), gh=di("gh", [128, 10]), ga=di("ga", [128, 6]), gf=di("gf", [1, D]), wr=di("wr", [128, DK, 36]),
              rb=di("rb", [1, 36]), ident=a["ident"], w_gate=di("w_gate", [NEXP * D, DE]), w_up=di("w_up", [NEXP * D, DE]),
              w_down=di("w_down", [NEXP * DE, D]), utri=di("utri", [128, 128]), ltm=di("ltm", [1, 1024]), bpos=di("bpos", [128, 1]),
              offg=di("offg", [128, DK]))
    x_ext = a["x_ext"]
    d3["x_own_fn"] = lambda r0: x_ext[(1024 + r0 if r0 < 4096 else r0 + 3072):(1024 + r0 if r0 < 4096 else r0 + 3072) + 128, :]
    y_out = nc.dram_tensor("y_own", [NOWN_, D], F32, kind="ExternalOutput").ap()
    UH = dn("UH", [2 * CHN_ALL, 1 + T], BF16)
    G2 = dn("G2", [CHN_ALL, NOWN_], BF16)
    YHo = dn("YHo", [CHN_ALL, NOWN_], BF16)
    KT, QT, VE = dn("KT", [NH, HD + 1, NE_], BF16), dn("QT", [NH, HD + 1, NOWN_], BF16), dn("VE", [NE_, NH * (HD + 1)], BF16)
    EV, EBD, YA = dn("EV", [NH, 2432], BF16), dn("EBD", [NH, 128, KB * 128], BF16), dn("YA", [NOWN_, DA])
    X1, H2B = dn("X1", [NOWN_, D]), dn("H2B", [NOWN_, D], BF16)
    AAd, GTd = dn("AAd", [128, NOWN_ // 128, 64]), dn("GTd", [128, NOWN_ // 128, 2])
    XB, YB = dn("XB", [NSLOT, D], BF16), dn("YB", [NSLOT, D])
    P = Prog(nc)
    cfg = dict(NE=NE_, NOWN=NOWN_, NBLK=NBLK, ones_row=a["ones_row"],
               own_tiles={**{2 + i: i for i in range(8)}, **{14 + i: 8 + i for i in range(4)}},
               pieces=[(0, 6144, 0, 4096), (6144, 4096, 4096, 2048)])
    phase_h(P, nc, [(0, LP, 0, LP, 1), (LP, LS_, LP, T, 1 + LP)], 2 * CHN_ALL, a["x_seq"], a["w_h"], a["gmix"], a["scw_h"], a["ident"], UH, tag="h")
    phase_h(P, nc, [(512, 5120, 1024, 5120, 0), (6656, 3072, 7168, 9216, 4096)], CHN_ALL, a["x_ext"], a["w_g2"], a["gmix"], a["scw_g2"], a["ident"], G2, tag="g")
    for L, tg, s0, own_off in ((LP, "p", 0, 0), (LS_, "s", LP, 4096)):
        cc, fd, Hown, N1 = fl[L]
        FILT = dn(f"FILT{tg}", [2 * CHN_ALL, 2 * L], BF16)
        GS = [dn(f"GS{tg}{o}", [128, CHN_ALL, 2 * N1], BF16) for o in range(2)]
        hyena_filters(P, nc, L, fd, FILT, CHN_ALL, tg)
        hyena_conv(P, nc, L, s0, cc, FILT, GS, UH, a["skip"], G2, own_off, Hown, YHo, CHN_ALL, tg)
    phase_q(P, nc, cfg, a["x_ext"], a["wq"], a["wk"], a["wv"], a["gmix"], a["gqk"], a["kvalid"], a["ident"], a["bd"], KT, QT, VE)
    phase_attn(P, nc, cfg, a["relb"], a["sel"], a["jm"], KT, QT, VE, EV, EBD, YA)
    phase3(P, nc, cfg, d3, YHo, YA, X1, H2B, AAd, GTd)
    phase_moe(P, nc, cfg, d3, X1, H2B, AAd, GTd, XB, YB, y_out)
    P.finish()
    return nc, P


def host_inputs(inp):
    import ml_dtypes
    bf = ml_dtypes.bfloat16
    f32 = np.float32
    g = lambda k: np.asarray(inp[k])
    xp, xs = g("x_prompt"), g("x_sample")
    w_in = g("w_in")[0]
    scwv = np.concatenate([g("sconv_w")[0], g("sconv_b")[0][None]], 0).T
    arr = lambda v, n: np.ascontiguousarray(v.reshape(n, 128, -1).transpose(1, 0, 2))
    col = lambda v: np.ascontiguousarray(v.reshape(-1, 128).T)
    shared = {}
    shared["w_h"] = np.ascontiguousarray(w_in[:, 0:2560])
    shared["w_g2"] = np.ascontiguousarray(w_in[:, 2560:3840])
    shared["wq"] = np.ascontiguousarray(w_in[:, 3840:4608])
    shared["wk"] = np.ascontiguousarray(w_in[:, 4608:5376])
    shared["wv"] = np.ascontiguousarray(w_in[:, 5376:6144])
    shared["gmix"] = col(g("mix_norm_g")[0])
    shared["scw_h"] = arr(scwv[0:2560], 20)
    shared["scw_g2"] = arr(scwv[2560:3840], 10)
    shared["ident"] = np.eye(128, dtype=f32)
    shared["gqk"] = np.ascontiguousarray(np.stack([np.tile(g("q_norm_g")[0], 2), np.tile(g("k_norm_g")[0], 2)], 1))
    shared["bd"] = np.kron(np.eye(2), np.ones((64, 64))).astype(f32)
    shared["relb"] = g("rel_bias")
    shared["sel"] = attn_sel_table()
    shared["jm"] = np.ascontiguousarray(np.eye(128, dtype=f32)[::-1])
    shared["ones_row"] = np.ones((1, NOWN_), f32).astype(bf)
    shared["f_w1"], shared["f_w2"], shared["f_w3"] = g("filt_w1")[0], g("filt_w2")[0], g("filt_w3")[0]
    shared["f_vec"] = np.ascontiguousarray(np.stack([g("filt_freq")[0], g("filt_b1")[0], g("filt_b2")[0], g("filt_b3")[0]], 1))
    shared["f_woutd"] = np.ascontiguousarray(g("filt_w_out")[0].reshape(64, 2, 2, CHN_ALL).transpose(0, 2, 1, 3).reshape(64, 2, 2 * CHN_ALL))
    delta = np.abs(np.linspace(MIN_DECAY_, MAX_DECAY_, CHN_ALL)).astype(f32)
    shared["f_ndelta"] = col(-np.tile(delta, 2))
    shared["skip"] = np.ascontiguousarray(g("hyena_skip")[0].reshape(1, -1))
    fcon = {}
    for L, tg in ((LP, "p"), (LS_, "s")):
        fc = fft_consts(L)
        fcon[L] = fc
        for k, v in fc.items():
            shared[f"c{tg}_{k}"] = v
        shared[f"f{tg}_feats"], shared[f"f{tg}_trow"] = _circle_tables(L)
    shared["w_out"] = g("w_out")[0]
    shared["gh"] = col(g("out_norm_h")[0])
    shared["ga"] = col(g("out_norm_a")[0])
    shared["gf"] = np.ascontiguousarray(g("ffn_norm_g")[0][None])
    wcat = np.concatenate([g("group_router_w")[0], g("expert_router_w")[0].transpose(1, 0, 2).reshape(D, 32)], 1)
    shared["wr"] = arr(wcat, DK)
    shared["rb"] = np.ascontiguousarray(np.concatenate([g("group_router_b")[0], g("expert_router_b")[0].reshape(-1)])[None])
    shared["w_gate"] = g("w_gate")[0].reshape(NEXP * D, DE)
    shared["w_up"] = g("w_up")[0].reshape(NEXP * D, DE)
    shared["w_down"] = g("w_down")[0].reshape(NEXP * DE, D)
    shared["utri"] = np.triu(np.ones((128, 128), f32), 1)
    shared["ltm"] = np.tril(np.ones((32, 32), f32)).reshape(1, -1)
    shared["bpos"] = (np.arange(128) * float(BLK)).astype(f32)[:, None]
    shared["offg"] = (np.arange(DK)[None, :] * 128 + np.arange(128)[:, None]).astype(f32)
    maps = []
    for c in range(8):
        b, hf = c // 2, c % 2
        m = dict(shared)
        m["x_seq"] = np.concatenate([xp[b], xs[0]], 0)
        xe = np.zeros((NE_, D), f32)
        kv = np.full((1, NE_), NEGV, f32)
        for (src, Lq, lo, n, e0) in ((xp[b], LP, 4096 * hf - 1024, 6144, 0), (xs[0], LS_, 2048 * c - 1024, 4096, 6144)):
            a0, a1 = max(lo, 0), min(lo + n, Lq)
            xe[e0 + a0 - lo:e0 + a1 - lo] = src[a0:a1]
            kv[0, e0 + a0 - lo:e0 + a1 - lo] = 0.0
        m["x_ext"] = xe
        m["kvalid"] = kv.astype(bf)
        for L, tg, Hown, r0 in ((LP, "p", 32, 32 * hf), (LS_, "s", 16, 16 * c)):
            H = L // 128
            sel = np.zeros((H, Hown), f32)
            sel[r0 + np.arange(Hown), np.arange(Hown)] = 1.0
            m[f"c{tg}_SEL"] = sel
            m[f"c{tg}_F4own"] = np.ascontiguousarray(fcon[L]["F4"][:, :, :, r0:r0 + Hown])
        maps.append(m)
    return maps


_PROG = None


def kernel(**inputs):
    global _PROG
    if _PROG is None:
        _PROG = build_program()[0]
    maps = host_inputs(inputs)
    res = run_bass_kernel_spmd(_PROG, maps, core_ids=list(range(8)))
    yp = np.zeros((4, LP, D), np.float32)
    ys = np.zeros((1, LS_, D), np.float32)
    for c in range(8):
        yo = np.asarray(res.results[c]["y_own"])
        yp[c // 2, 4096 * (c % 2):4096 * (c % 2) + 4096] = yo[0:4096]
        ys[0, 2048 * c:2048 * c + 2048] = yo[4096:6144]
    return yp, ys
```

```python
import numpy as np
import concourse.bass as bass
import concourse.mybir as mybir
from concourse.bass_utils import run_bass_kernel_spmd

F32 = mybir.dt.float32
BF16 = mybir.dt.bfloat16
I32 = mybir.dt.int32
U32 = mybir.dt.uint32
ALU = mybir.AluOpType
AF = mybir.ActivationFunctionType
AX = mybir.AxisListType


def _key(item):
    if isinstance(item, (tuple, str)):
        return item
    t = getattr(item, "tensor", None)
    if t is not None:
        return t.name
    return item.name


class Prog:
    NDMA = 6

    def __init__(self, nc):
        self.nc = nc
        self.E = {"pe": nc.tensor, "dve": nc.vector, "act": nc.scalar, "pool": nc.gpsimd, "sp": nc.sync}
        self.sem = {e: nc.alloc_semaphore(name=f"c_{e}") for e in ("pe", "dve", "act", "pool")}
        self.cnt = {e: 0 for e in self.sem}
        self.known = {e: {} for e in self.E}
        self.res = {}
        self.dsem = {q: [nc.alloc_semaphore(name=f"d_{q}{i}") for i in range(self.NDMA)] for q in ("sp", "act", "pool")}
        self.dcnt = {q: [0] * self.NDMA for q in self.dsem}
        self.drr = {q: 0 for q in self.dsem}
        self.nops = 0

    def _wait(self, eng, sem, val):
        k = self.known[eng]
        name = sem.name if hasattr(sem, "name") else id(sem)
        if k.get(name, 0) >= val:
            return
        self.E[eng].wait_ge(sem, val)
        k[name] = val

    def _deps(self, eng, reads, writes):
        for r in reads:
            st = self.res.get(_key(r))
            if st is None:
                continue
            w = st["w"]
            if w is not None and not (w[2] == "pe" and eng == "pe"):
                self._wait(eng, w[0], w[1])
        for wr in writes:
            st = self.res.get(_key(wr))
            if st is None:
                continue
            w = st["w"]
            if w is not None and not (w[2] == "pe" and eng == "pe"):
                self._wait(eng, w[0], w[1])
            for (s, v, e2) in st["r"].values():
                if e2 == "pe" and eng == "pe":
                    continue
                self._wait(eng, s, v)

    def _commit(self, tok, reads, writes):
        for r in reads:
            st = self.res.setdefault(_key(r), {"w": None, "r": {}})
            nm = tok[0].name
            st["r"][nm] = tok
        for wr in writes:
            self.res[_key(wr)] = {"w": tok, "r": {}}

    def op(self, eng, fn, reads=(), writes=()):
        self._deps(eng, reads, writes)
        inst = fn(self.E[eng])
        self.cnt[eng] += 1
        inst.then_inc(self.sem[eng], 1)
        tok = (self.sem[eng], self.cnt[eng], eng)
        self._commit(tok, reads, writes)
        self.nops += 1
        return inst

    def dma(self, q, out, in_, reads=None, writes=None, fn=None, **kw):
        if reads is None:
            reads = [in_]
        if writes is None:
            writes = [out]
        i = self.drr[q]
        self.drr[q] = (i + 1) % self.NDMA
        s = self.dsem[q][i]
        if self.dcnt[q][i] > 0:
            self._wait(q, s, 16 * self.dcnt[q][i])
        self._deps(q, reads, writes)
        if fn is None:
            inst = self.E[q].dma_start(out=out, in_=in_, **kw)
        else:
            inst = fn(self.E[q])
        inst.then_inc(s, 16)
        self.dcnt[q][i] += 1
        tok = (s, 16 * self.dcnt[q][i], "dma")
        self._commit(tok, reads, writes)
        self.nops += 1
        return inst

    def mark(self, name):
        if not hasattr(self, 'marks'):
            self.marks = []
        self.marks.append((name, dict(self.cnt)))

    def barrier(self):
        for eng in self.E:
            for q in self.dsem:
                for i, sm in enumerate(self.dsem[q]):
                    if self.dcnt[q][i]:
                        self._wait(eng, sm, 16 * self.dcnt[q][i])
            for e, sm in self.sem.items():
                if self.cnt[e] and e != eng:
                    self._wait(eng, sm, self.cnt[e])
        self.res = {}

    def finish(self):
        for q in self.dsem:
            for i, s in enumerate(self.dsem[q]):
                if self.dcnt[q][i]:
                    self._wait("sp", s, 16 * self.dcnt[q][i])
        for e, s in self.sem.items():
            if self.cnt[e]:
                self._wait("sp", s, self.cnt[e])


D = 2048
DK = D // 128
EPS = 1e-6


def bcast_rows(ap_row, nparts):
    t = ap_row.tensor
    n = ap_row.shape[-1]
    return bass.AP(tensor=t, offset=ap_row.offset, ap=[[0, nparts], [1, n]])


class XT:
    def __init__(self, P, nc, es, tag):
        self.P, self.nc = P, nc
        self.xin = [es.enter_context(nc.sbuf_tensor(f"xin{tag}0", [128, 4, D], F32))] * 2
        self.xbf = [es.enter_context(nc.sbuf_tensor(f"xbf{tag}{i}", [128, 4, D], BF16)) for i in range(2)]
        self.xT = [es.enter_context(nc.sbuf_tensor(f"xT{tag}{i}", [128, DK, 512], BF16)) for i in range(2)]
        self.pt = [es.enter_context(nc.psum_tensor(f"ptr{tag}{i}", [128, 2, 512], BF16)) for i in range(2)]
        self.ident = es.enter_context(nc.sbuf_tensor(f"ident{tag}", [128, 128], BF16))
        self.n = 0

    def setup(self, ident_dram):
        P = self.P
        tmp = self.xin[0]
        P.dma("sp", tmp[:, 0, 0:128], ident_dram)
        P.op("dve", lambda e: e.tensor_copy(out=self.ident[:], in_=tmp[:, 0, 0:128]), [tmp], [self.ident])
        self.identf = None

    def load(self, x_rows_ap):
        P = self.P
        i = self.n % 2
        self.n += 1
        xin, xbf, xT = self.xin[i], self.xbf[i], self.xT[i]
        src = x_rows_ap.rearrange("(s p) d -> p s d", p=128)
        P.dma("sp", xin[:], src, [x_rows_ap], [xin])
        for s in range(4):
            eng = "act" if s % 2 == 0 else "pool"
            if eng == "act":
                P.op("act", lambda e, s=s: e.copy(out=xbf[:, s, :], in_=xin[:, s, :]), [xin], [(xbf.name, s)])
            else:
                P.op("pool", lambda e, s=s: e.tensor_copy(out=xbf[:, s, :], in_=xin[:, s, :]), [xin], [(xbf.name, s)])
        for jj in range(DK // 2):
            pt = self.pt[jj % 2]
            for j2 in range(2):
                j = jj * 2 + j2
                for s in range(4):
                    P.op("pe", lambda e, s=s, j=j, j2=j2, pt=pt: e.transpose(
                        out=pt[:, j2, s * 128:(s + 1) * 128], in_=xbf[:, s, j * 128:(j + 1) * 128], identity=self.ident[:]),
                        [(xbf.name, s), self.ident], [pt])
            eng = "dve" if jj % 2 == 0 else "act"
            if eng == "dve":
                P.op("dve", lambda e, jj=jj, pt=pt: e.tensor_copy(out=xT[:, 2 * jj:2 * jj + 2, :], in_=pt[:, :, :]), [pt], [(xT.name, jj)])
            else:
                P.op("act", lambda e, jj=jj, pt=pt: e.copy(out=xT[:, 2 * jj:2 * jj + 2, :], in_=pt[:, :, :]), [pt], [(xT.name, jj)])
        return xin, xbf, xT


import math
PI = math.pi


class ShortConv:
    def __init__(self, P, nc, es, nchunks, RC, tag):
        self.P, self.RC, self.nchunks = P, RC, nchunks
        sb = lambda n, shp, dt=F32: es.enter_context(nc.sbuf_tensor(f"{n}{tag}", shp, dt))
        self.carry = sb("carry", [RC, nchunks, 2])
        self.U = [sb(f"U{i}", [RC, 516]) for i in range(2)]
        self.ta = [sb(f"ta{i}", [RC, 512]) for i in range(2)]
        self.tb = [sb(f"tb{i}", [RC, 512]) for i in range(2)]
        self.ob = [sb(f"ob{i}", [RC, 512], BF16) for i in range(2)]
        self.n = 0

    def reset(self):
        self.P.op("pool", lambda e: e.memset(self.carry[:], 0.0), [], [(self.carry.name, m) for m in range(self.nchunks)])

    def step(self, m, w, fill, emit):
        P, RC = self.P, self.RC
        k = self.n % 2
        self.n += 1
        U, ta, tb, ob = self.U[k], self.ta[k], self.tb[k], self.ob[k]
        P.op("pool", lambda e: e.tensor_copy(out=U[:, 0:2], in_=self.carry[:, m, :]), [(self.carry.name, m)], [U])
        fill(U)
        P.op("pool", lambda e: e.tensor_copy(out=self.carry[:, m, :], in_=U[:, 512:514]), [U], [(self.carry.name, m)])
        if emit is None:
            return
        lo, hi = emit[1], emit[2]
        n = hi - lo
        P.op("act", lambda e: e.activation(out=ta[:, 0:n], in_=U[:, 1 + lo:1 + hi], func=AF.Identity, bias=w[:, m, 3:4], scale=w[:, m, 1:2]), [U, w], [ta])
        P.op("dve", lambda e: e.scalar_tensor_tensor(out=tb[:, 0:n], in0=U[:, lo:hi], scalar=w[:, m, 0:1], in1=ta[:, 0:n], op0=ALU.mult, op1=ALU.add), [U, w, ta], [tb])
        P.op("dve", lambda e: e.scalar_tensor_tensor(out=ob[:, lo:hi], in0=U[:, 2 + lo:2 + hi], scalar=w[:, m, 2:3], in1=tb[:, 0:n], op0=ALU.mult, op1=ALU.add), [U, w, tb], [ob])
        emit[0](ob)

    def flush(self, m, w, emit):
        P = self.P
        k = self.n % 2
        self.n += 1
        ta, ob = self.ta[k], self.ob[k]
        c = self.carry
        P.op("dve", lambda e: e.tensor_scalar(out=ta[:, 0:1], in0=c[:, m, 1:2], scalar1=w[:, m, 1:2], scalar2=w[:, m, 3:4], op0=ALU.mult, op1=ALU.add),
             [(c.name, m), w], [ta])
        P.op("dve", lambda e: e.scalar_tensor_tensor(out=ob[:, 0:1], in0=c[:, m, 0:1], scalar=w[:, m, 0:1], in1=ta[:, 0:1], op0=ALU.mult, op1=ALU.add),
             [(c.name, m), w, ta], [ob])
        emit(ob)


def phase_h(P, nc, seqs, NCOL, x_seq, w_h, gmix, scw, ident_d, UH, tag="h"):
    import contextlib
    RC = min(128, NCOL)
    NCH = NCOL // RC
    with contextlib.ExitStack() as es:
        sb = lambda n, shp, dt=F32: es.enter_context(nc.sbuf_tensor(n, shp, dt))
        xt = XT(P, nc, es, tag)
        xt.setup(ident_d)
        Wh = sb(f"Wh{tag}", [128, DK, NCOL], BF16)
        g_sb, scw_sb, epsT = sb(f"g_sb{tag}", [128, DK]), sb(f"scw_sb{tag}", [RC, NCH, 4]), sb(f"epsT{tag}", [128, 1])
        identf = sb(f"identf_h{tag}", [128, 128])
        ssq, e2c = [sb(f"ssq{tag}{i}", [128, 4]) for i in range(2)], [sb(f"rc{tag}{i}", [128, 4]) for i in range(2)]
        rbc = [sb(f"rbc{tag}{i}", [128, 4, 128]) for i in range(2)]
        rstd = [sb(f"rstd{tag}{i}", [128, 512]) for i in range(2)]
        junk = sb(f"junkh{tag}", [128, D], BF16)
        sc = ShortConv(P, nc, es, NCH, RC, tag)
        pss = es.enter_context(nc.psum_tensor(f"pss{tag}", [128, 512], F32))
        pu = [es.enter_context(nc.psum_tensor(f"pu{tag}{i}", [128, 512], F32)) for i in range(3)]
        P.op("pool", lambda e: e.memset(epsT[:], EPS), [], [epsT])
        for i in range(2):
            P.op("pool", lambda e, i=i: e.memset(ssq[i][:], 0.0), [], [ssq[i]])
        P.dma("sp", g_sb[:], gmix)
        P.dma("sp", scw_sb[:], scw)
        P.dma("sp", identf[:], ident_d)
        stage = xt.xin[0]
        for j in range(DK):
            for c0 in range(0, NCOL, 2048):
                cn = min(2048, NCOL - c0)
                P.dma("sp", stage[:, 0, 0:cn], w_h[j * 128:(j + 1) * 128, c0:c0 + cn], None, [stage])
                P.op("dve", lambda e, j=j, c0=c0, cn=cn: e.tensor_scalar(out=Wh[:, j, c0:c0 + cn], in0=stage[:, 0, 0:cn], scalar1=g_sb[:, j:j + 1],
                                                                       scalar2=None, op0=ALU.mult), [stage, g_sb], [Wh])
        ntile = 0
        for (s0, L, elo, ehi, oc0) in seqs:
            sc.reset()
            nt = L // 512
            for it in range(nt):
                t0 = s0 + it * 512
                lo_i = max(0, elo - (t0 - 1))
                hi_i = min(512, ehi - (t0 - 1))
                xin, xbf, xT = xt.load(x_seq[t0:t0 + 512, :])
                b = ntile % 2
                ntile += 1
                for s in range(4):
                    P.op("act", lambda e, s=s: e.activation(out=junk[:], in_=xin[:, s, :], func=AF.Square, accum_out=ssq[b][:, s:s + 1]), [xin], [junk, ssq[b]])
                P.op("act", lambda e: e.activation(out=e2c[b][:], in_=ssq[b][:], func=AF.Sqrt, bias=epsT[:], scale=1.0 / D), [ssq[b], epsT], [e2c[b]])
                P.op("dve", lambda e: e.reciprocal(out=e2c[b][:], in_=e2c[b][:]), [e2c[b]], [e2c[b]])
                P.op("pool", lambda e: e.memset(ssq[b][:], 0.0), [], [ssq[b]])
                P.op("dve", lambda e: e.tensor_copy(out=rbc[b][:], in_=bc_last(e2c[b][:, 0:4], 128)), [e2c[b]], [rbc[b]])
                for s4 in range(4):
                    P.op("pe", lambda e, s4=s4: e.matmul(pss[:, s4 * 128:(s4 + 1) * 128], lhsT=rbc[b][:, s4, :], rhs=identf[:], start=True, stop=True),
                         [rbc[b], identf], [pss])
                P.op("act", lambda e: e.copy(out=rstd[b][:], in_=pss[:]), [pss], [rstd[b]])
                for m in range(NCH):
                    pum = pu[m % 3]
                    for j in range(DK):
                        P.op("pe", lambda e, j=j, m=m, pum=pum: e.matmul(pum[0:RC, :], lhsT=Wh[:, j, m * RC:(m + 1) * RC], rhs=xT[:, j, :],
                                                                         start=(j == 0), stop=(j == DK - 1)), [Wh, (xT.name, j // 2)], [pum])

                    def fill(U, pum=pum):
                        P.op("dve", lambda e: e.tensor_tensor(out=U[:, 2:514], in0=pum[0:RC, :], in1=rstd[b][0:RC, :], op=ALU.mult), [pum, rstd[b]], [U])

                    def emit(ob, m=m, t0=t0, lo_i=lo_i, hi_i=hi_i):
                        c0 = oc0 + (t0 - 1 + lo_i) - elo
                        P.dma("act", UH[m * RC:(m + 1) * RC, c0:c0 + hi_i - lo_i], ob[:, lo_i:hi_i], [ob], [(UH.tensor.name, m)],
                              allow_slow_non_contiguous=(hi_i - lo_i == 1))
                    sc.step(m, scw_sb, fill, (emit, lo_i, hi_i) if hi_i > lo_i else None)
                    if it == nt - 1 and ehi == s0 + L:
                        def emit2(ob, m=m):
                            c0 = oc0 + (ehi - 1) - elo
                            P.dma("act", UH[m * RC:(m + 1) * RC, c0:c0 + 1], ob[:, 0:1], [ob], [(UH.tensor.name, m)], allow_slow_non_contiguous=True)
                        sc.flush(m, scw_sb, emit2)
        P.barrier()


def flat2(ap):
    nd = len(ap.shape)
    if nd == 2:
        return ap
    names = "abcdef"[:nd - 1]
    return ap.rearrange("p " + " ".join(names) + " -> p (" + " ".join(names) + ")")


def bc_col(ap, n):
    a = [list(x) for x in ap.ap]
    return bass.AP(tensor=ap.tensor, offset=ap.offset, ap=[a[0], [0, n]])


def bc_mid(ap, n):
    a = [list(x) for x in ap.ap]
    return bass.AP(tensor=ap.tensor, offset=ap.offset, ap=[a[0], [0, n]] + a[1:])


def bc_last(ap, n):
    a = [list(x) for x in ap.ap]
    return bass.AP(tensor=ap.tensor, offset=ap.offset, ap=a + [[0, n]])


def fft_consts(L):
    N = 2 * L
    N1 = N // 128
    H = N1 // 2
    c = {}
    n1 = np.arange(N1)[:, None]
    k1 = np.arange(N1)[None, :]
    th = 2 * np.pi * n1 * k1 / N1
    f1 = np.concatenate([np.cos(th), -np.sin(th)], 1)
    c["F1"] = f1.reshape(N1 // 128, 128, 2 * N1).transpose(1, 0, 2)
    n2 = np.arange(128)[:, None]
    th = 2 * np.pi * n2 * k1 / N
    c["TW1"] = np.stack([np.cos(th), -np.sin(th)], 1)
    k2 = np.arange(128)[None, :]
    th = 2 * np.pi * n2 * k2 / 128
    c["F2"] = np.stack([np.cos(th), -np.sin(th), np.sin(th)], 1)
    th = 2 * np.pi * np.arange(128)[:, None] * np.arange(128)[None, :] / 128
    c["F3"] = np.stack([np.concatenate([np.cos(th), np.sin(th)], 1),
                        np.concatenate([-np.sin(th), np.cos(th)], 1)], 1)
    k1c = np.arange(N1)[:, None]
    n1p = np.arange(128)[None, :]
    th = 2 * np.pi * k1c * n1p / N
    tw2 = np.stack([np.cos(th), np.sin(th)], 1)
    c["TW2"] = tw2.reshape(N1 // 128, 128, 2, 128).transpose(1, 0, 2, 3)
    n2p = np.arange(H)[None, :]
    th = 2 * np.pi * k1c * n2p / N1
    f4 = np.stack([np.cos(th) / N, -np.sin(th) / N], 1)
    c["F4"] = f4.reshape(N1 // 128, 128, 2, H).transpose(1, 0, 2, 3)
    return {k: np.ascontiguousarray(v, dtype=np.float32) for k, v in c.items()}


class FFT:
    def __init__(self, P, nc, es, L, cd, tag, Hown=None):
        self.P, self.nc, self.L = P, nc, L
        N1 = self.N1 = 2 * L // 128
        H = self.H = N1 // 2
        self.nch = N1 // 128
        self.Cb = 512 // N1
        Cb = self.Cb
        sb = lambda n, shp, dt: es.enter_context(nc.sbuf_tensor(f"{n}{tag}", shp, dt))
        self.F1 = sb("F1", [128, self.nch, 2 * N1], BF16)
        self.TW1 = sb("TW1", [128, 2, N1], F32)
        self.F2 = sb("F2", [128, 3, 128], BF16)
        self.F3 = sb("F3", [128, 2, 256], BF16)
        self.TW2 = sb("TW2", [128, self.nch, 2, 128], F32)
        self.F4 = sb("F4", [128, self.nch, 2, H], BF16)
        lst = [("F1", self.F1), ("F2", self.F2), ("F3", self.F3), ("F4", self.F4)]
        if Hown is not None:
            self.Hown = Hown
            self.F4o = sb("F4o", [128, self.nch, 2, Hown], BF16)
            self.SEL = sb("SELo", [H, Hown], BF16)
            lst += [("F4own", self.F4o), ("SEL", self.SEL)]
        stg = sb("fstage", [128, 1024], F32)
        for nm, dst in lst:
            n = int(np.prod(dst.shape[1:]))
            p = dst.shape[0]
            dflat = flat2(dst[:])
            sflat = flat2(cd[nm])
            for c0 in range(0, n, 1024):
                cn = min(1024, n - c0)
                P.dma("sp", stg[0:p, 0:cn], sflat[:, c0:c0 + cn], None, [stg])
                P.op("dve", lambda e, dflat=dflat, p=p, c0=c0, cn=cn: e.tensor_copy(out=dflat[:, c0:c0 + cn], in_=stg[0:p, 0:cn]), [stg], [dst])
        P.dma("sp", self.TW1[:], cd["TW1"])
        P.dma("sp", self.TW2[:], cd["TW2"])
        self.t1 = [sb(f"ft1_{i}", [128, 1024], F32) for i in range(2)]
        self.t2 = [sb(f"ft2_{i}", [128, 1024], F32) for i in range(2)]
        self.Ar = [sb(f"Ar{i}", [128, Cb, N1], BF16) for i in range(2)]
        self.Ai = [sb(f"Ai{i}", [128, Cb, N1], BF16) for i in range(2)]
        self.Yr = [sb(f"Yr{i}", [128, Cb, N1], BF16) for i in range(2)]
        self.Yi = [sb(f"Yi{i}", [128, Cb, N1], BF16) for i in range(2)]
        self.Cr = [sb(f"Cr{i}", [128, Cb, self.nch, 128], BF16) for i in range(2)]
        self.Ci = [sb(f"Ci{i}", [128, Cb, self.nch, 128], BF16) for i in range(2)]
        self.g = 0

    def fwd(self, zl, zkey, pa, pb):
        P, N1, Cb = self.P, self.N1, self.Cb
        i = self.g % 2
        t1, t2, Ar, Ai = self.t1[i], self.t2[i], self.Ar[i], self.Ai[i]
        for c in range(Cb):
            for n, (kc, z) in enumerate(zl):
                K = z.shape[0]
                P.op("pe", lambda e, c=c, n=n, kc=kc, z=z, K=K: e.matmul(pa[:, c * 2 * N1:(c + 1) * 2 * N1], lhsT=z[:, c, :], rhs=self.F1[0:K, kc, :],
                                                                        start=(n == 0), stop=(n == len(zl) - 1)), [zkey, self.F1], [pa])
        pav = pa[:].rearrange("p (c r k) -> p c r k", c=Cb, r=2)
        t1v = t1[:].rearrange("p (c r k) -> p c r k", c=Cb, r=2)
        t2v = t2[:].rearrange("p (c r k) -> p c r k", c=Cb, r=2)
        twr = bc_mid(bc_mid(self.TW1[:, 0, :], 2), Cb)
        twi = bc_mid(bc_mid(self.TW1[:, 1, :], 2), Cb)
        P.op("dve", lambda e: e.tensor_tensor(out=t1v, in0=pav, in1=twr, op=ALU.mult), [pa, self.TW1], [t1])
        P.op("dve", lambda e: e.tensor_tensor(out=t2v, in0=pav, in1=twi, op=ALU.mult), [pa, self.TW1], [t2])
        P.op("dve", lambda e: e.tensor_tensor(out=Ar[:], in0=t1v[:, :, 0, :], in1=t2v[:, :, 1, :], op=ALU.subtract), [t1, t2], [Ar])
        P.op("dve", lambda e: e.tensor_tensor(out=Ai[:], in0=t2v[:, :, 0, :], in1=t1v[:, :, 1, :], op=ALU.add), [t1, t2], [Ai])
        Arf = Ar[:].rearrange("p c k -> p (c k)")
        Aif = Ai[:].rearrange("p c k -> p (c k)")
        F2 = self.F2
        P.op("pe", lambda e: e.matmul(pb[:, 0:512], lhsT=F2[:, 0, :], rhs=Arf, start=True, stop=False), [F2, Ar], [pb])
        P.op("pe", lambda e: e.matmul(pb[:, 0:512], lhsT=F2[:, 2, :], rhs=Aif, start=False, stop=True), [F2, Ai], [pb])
        P.op("pe", lambda e: e.matmul(pb[:, 512:1024], lhsT=F2[:, 0, :], rhs=Aif, start=True, stop=False), [F2, Ai], [pb])
        P.op("pe", lambda e: e.matmul(pb[:, 512:1024], lhsT=F2[:, 1, :], rhs=Arf, start=False, stop=True), [F2, Ar], [pb])

    def inv(self, G, pa, pb, own=False, extra=None):
        P, N1, Cb, nch, H = self.P, self.N1, self.Cb, self.nch, self.H
        i = self.g % 2
        t1, t2, Yr, Yi, Cr, Ci = self.t1[i], self.t2[i], self.Yr[i], self.Yi[i], self.Cr[i], self.Ci[i]
        Xr = pb[:, 0:512].rearrange("p (c k) -> p c k", c=Cb)
        Xi = pb[:, 512:1024].rearrange("p (c k) -> p c k", c=Cb)
        Gr, Gi = G[:, :, 0:N1], G[:, :, N1:2 * N1]
        q = lambda t, j: t[:, j * 512:(j + 1) * 512].rearrange("p (c k) -> p c k", c=Cb)
        P.op("dve", lambda e: e.tensor_tensor(out=q(t1, 0), in0=Xr, in1=Gr, op=ALU.mult), [pb, G], [t1])
        P.op("dve", lambda e: e.tensor_tensor(out=q(t1, 1), in0=Xi, in1=Gi, op=ALU.mult), [pb, G], [t1])
        P.op("dve", lambda e: e.tensor_tensor(out=q(t2, 0), in0=Xr, in1=Gi, op=ALU.mult), [pb, G], [t2])
        P.op("dve", lambda e: e.tensor_tensor(out=q(t2, 1), in0=Xi, in1=Gr, op=ALU.mult), [pb, G], [t2])
        P.op("dve", lambda e: e.tensor_tensor(out=Yr[:], in0=q(t1, 0), in1=q(t1, 1), op=ALU.subtract), [t1], [Yr])
        P.op("dve", lambda e: e.tensor_tensor(out=Yi[:], in0=q(t2, 0), in1=q(t2, 1), op=ALU.add), [t2], [Yi])
        F3 = self.F3
        for c in range(Cb):
            for ch in range(nch):
                o = (c * nch + ch) * 256
                P.op("pe", lambda e, c=c, ch=ch, o=o: e.matmul(pa[:, o:o + 256], lhsT=Yr[:, c, ch * 128:(ch + 1) * 128], rhs=F3[:, 0, :], start=True, stop=False),
                     [Yr, F3], [pa])
                P.op("pe", lambda e, c=c, ch=ch, o=o: e.matmul(pa[:, o:o + 256], lhsT=Yi[:, c, ch * 128:(ch + 1) * 128], rhs=F3[:, 1, :], start=False, stop=True),
                     [Yi, F3], [pa])
        pav = pa[:].rearrange("p (c h r k) -> p c h r k", c=Cb, h=nch, r=2)
        t1v = t1[:].rearrange("p (c h r k) -> p c h r k", c=Cb, h=nch, r=2)
        t2v = t2[:].rearrange("p (c h r k) -> p c h r k", c=Cb, h=nch, r=2)
        for ch in range(nch):
            twr = bc_mid(bc_mid(self.TW2[:, ch, 0, :], 2), Cb)
            twi = bc_mid(bc_mid(self.TW2[:, ch, 1, :], 2), Cb)
            P.op("dve", lambda e, ch=ch, twr=twr: e.tensor_tensor(out=t1v[:, :, ch], in0=pav[:, :, ch], in1=twr, op=ALU.mult), [pa, self.TW2], [t1])
            P.op("dve", lambda e, ch=ch, twi=twi: e.tensor_tensor(out=t2v[:, :, ch], in0=pav[:, :, ch], in1=twi, op=ALU.mult), [pa, self.TW2], [t2])
        P.op("dve", lambda e: e.tensor_tensor(out=Cr[:], in0=t1v[:, :, :, 0, :], in1=t2v[:, :, :, 1, :], op=ALU.subtract), [t1, t2], [Cr])
        P.op("dve", lambda e: e.tensor_tensor(out=Ci[:], in0=t2v[:, :, :, 0, :], in1=t1v[:, :, :, 1, :], op=ALU.add), [t1, t2], [Ci])
        F4 = self.F4o if own else self.F4
        Hout = self.Hown if own else H
        k = 0
        tot = 2 * nch + (1 if extra is not None else 0)
        outv = pb[0:Hout, 0:Cb * 128].rearrange("p (c k) -> p c k", c=Cb)
        for ch in range(nch):
            for r, Cx in ((0, Cr), (1, Ci)):
                P.op("pe", lambda e, ch=ch, r=r, Cx=Cx, k=k: e.matmul(outv, lhsT=F4[:, ch, r, :], rhs=Cx[:, :, ch, :],
                                                                      start=(k == 0), stop=(k == tot - 1)), [F4, Cx], [pb])
                k += 1
        if extra is not None:
            P.op("pe", lambda e: e.matmul(outv, lhsT=extra[0], rhs=extra[1], start=False, stop=True), list(extra[2]), [pb])
        self.g += 1


def hyena_filters(P, nc, L, fd, FILT, CHN, tag):
    import contextlib
    NR = 2 * CHN
    RC = min(128, NR)
    NCH = NR // RC
    nt = 2 * L // 512
    nth = nt // 2
    with contextlib.ExitStack() as es:
        sb = lambda n, shp, dt=F32: es.enter_context(nc.sbuf_tensor(f"{n}{tag}", shp, dt))
        w1s, w2s, w3s = sb("w1s", [33, 64]), sb("w2s", [64, 64]), sb("w3s", [64, 64])
        fv, fb, wouts, nd = sb("fv", [64, 4]), sb("fb", [64, 3]), sb("wouts", [64, 2, NR]), sb("nd", [RC, NCH])
        ft = [sb(f"ft{i}", [33, 512]) for i in range(2)]
        tbc = [sb(f"tbc{i}", [RC, 512]) for i in range(2)]
        arg, arg2 = sb("arg", [64, 512]), sb("arg2", [64, 512])
        aa = [sb(f"aa{i}", [64, 512], F32 if i < 2 else BF16) for i in range(3)]
        woutb = sb("woutb", [64, 2, NR], BF16)
        dec = [sb(f"dec{i}", [RC, 512]) for i in range(4)]
        gt = [sb(f"gt{i}", [RC, 512]) for i in range(4)]
        junk = sb("junk", [RC, 512])
        gob = [sb(f"gob{i}", [RC, 512], BF16) for i in range(4)]
        acc = sb("acc", [RC, NCH, nt])
        accs, inv = sb("accs", [RC, NCH]), sb("inv", [RC, NCH])
        pz = [es.enter_context(nc.psum_tensor(f"pz{tag}{i}", [64, 512], F32)) for i in range(2)]
        phr = [es.enter_context(nc.psum_tensor(f"phr{tag}{i}", [128, 512], F32)) for i in range(4)]
        for dst, src in ((w1s, fd["w1"]), (w2s, fd["w2"]), (w3s, fd["w3"]), (fv, fd["fvec"]), (wouts, fd["woutd"]), (nd, fd["ndelta"])):
            P.dma("sp", dst[:], src)
        P.op("pool", lambda e: e.memset(acc[:], 0.0), [], [acc])
        P.op("dve", lambda e: e.tensor_copy(out=woutb[:], in_=wouts[:]), [wouts], [woutb])
        P.op("dve", lambda e: e.tensor_scalar(out=fb[:], in0=fv[:, 1:4], scalar1=fv[:, 0:1], scalar2=None, op0=ALU.mult), [fv], [fb])
        ws = [w1s, w2s, w3s]
        cnt = [0]
        MAGIC = 12582912.0

        def mlp(it):
            k = cnt[0] % 2
            cnt[0] += 1
            P.dma("sp", ft[k][:], fd["featsT2"][:, it * 512:(it + 1) * 512])
            P.dma("sp", tbc[k][:], bcast_rows(fd["trow2"][:, it * 512:(it + 1) * 512], RC))
            src = ft[k]
            for l in range(3):
                p = pz[l % 2]
                P.op("pe", lambda e, l=l, p=p, src=src: e.matmul(p[:], lhsT=ws[l][:], rhs=src[:], start=True, stop=True), [ws[l], src], [p])
                P.op("dve", lambda e, l=l, p=p: e.tensor_scalar(out=arg[:], in0=p[:], scalar1=fv[:, 0:1], scalar2=fb[:, l:l + 1],
                                                                op0=ALU.mult, op1=ALU.add), [p, fv, fb], [arg])
                P.op("dve", lambda e: e.tensor_scalar(out=arg2[:], in0=arg[:], scalar1=1.0 / (2 * PI), scalar2=MAGIC, op0=ALU.mult, op1=ALU.add), [arg], [arg2])
                P.op("dve", lambda e: e.tensor_scalar(out=arg2[:], in0=arg2[:], scalar1=-MAGIC, scalar2=None, op0=ALU.add), [arg2], [arg2])
                P.op("dve", lambda e: e.scalar_tensor_tensor(out=arg[:], in0=arg2[:], scalar=-2 * PI, in1=arg[:], op0=ALU.mult, op1=ALU.add), [arg2, arg], [arg])
                P.op("dve", lambda e: e.tensor_scalar(out=arg[:], in0=arg[:], scalar1=-PI, scalar2=PI, op0=ALU.max, op1=ALU.min), [arg], [arg])
                P.op("act", lambda e, l=l: e.activation(out=aa[l][:], in_=arg[:], func=AF.Sin), [arg], [aa[l]])
                src = aa[l]
            return aa[2], tbc[k]

        def hr_chunk(a3, tb, r, it):
            j = r % 4
            p = phr[j]
            d = it // nth
            P.op("pe", lambda e: e.matmul(p[0:RC, :], lhsT=woutb[:, d, r * RC:(r + 1) * RC], rhs=a3[:], start=True, stop=True), [woutb, a3], [p])
            P.op("act", lambda e: e.activation(out=dec[j][:], in_=tb[:], func=AF.Exp, scale=nd[:, r:r + 1]), [tb, nd], [dec[j]])
            P.op("dve", lambda e: e.tensor_tensor(out=gt[j][:], in0=p[0:RC, :], in1=dec[j][:], op=ALU.mult), [p, dec[j]], [gt[j]])
            if it == nth:
                P.op("pool", lambda e: e.memset(gt[j][:, 0:1], 0.0), [], [gt[j]])
            return gt[j]

        for it in range(nt):
            a3, tb = mlp(it)
            for r in range(NCH):
                g = hr_chunk(a3, tb, r, it)
                P.op("act", lambda e, g=g, r=r, it=it: e.activation(out=junk[:], in_=g[:], func=AF.Abs, accum_out=acc[:, r, it:it + 1]), [g], [junk, acc])
        P.op("dve", lambda e: e.reduce_sum(out=accs[:], in_=acc[:], axis=AX.X), [acc], [accs])
        P.op("dve", lambda e: e.reciprocal(out=inv[:], in_=accs[:]), [accs], [inv])
        for it in range(nt):
            a3, tb = mlp(it)
            for r in range(NCH):
                g = hr_chunk(a3, tb, r, it)
                ob = gob[r % 4]
                P.op("act", lambda e, g=g, ob=ob, r=r: e.activation(out=ob[:], in_=g[:], func=AF.Identity, scale=inv[:, r:r + 1]), [g, inv], [ob])
                P.dma("act", FILT[r * RC:(r + 1) * RC, it * 512:(it + 1) * 512], ob[:], [ob], [FILT])
        P.barrier()


def hyena_conv(P, nc, L, s0, cd, FILT, GS, UH, skip_d, G2, own_off, Hown, YHo, CHN, tag):
    import contextlib
    with contextlib.ExitStack() as es:
        fft = FFT(P, nc, es, L, cd, tag, Hown=Hown)
        N1, H, Cb, nch = fft.N1, fft.H, fft.Cb, fft.nch
        ncg = CHN // Cb
        sb = lambda n, shp, dt=F32: es.enter_context(nc.sbuf_tensor(f"{n}{tag}", shp, dt))
        pa = [es.enter_context(nc.psum_tensor(f"pa{tag}{i}", [128, 1024], F32)) for i in range(2)]
        pb = [es.enter_context(nc.psum_tensor(f"pb{tag}{i}", [128, 1024], F32)) for i in range(2)]
        zf = [sb(f"zf{i}", [128, nch, Cb, 128], BF16) for i in range(2)]
        Gt = [sb(f"Gt{i}", [128, Cb, 2 * N1], BF16) for i in range(2)]
        skipbc = sb("skipbc", [128, 2, CHN])
        P.dma("sp", skipbc[:].rearrange("p o c -> p (o c)"), bcast_rows(skip_d, 128))
        n = 0
        for o in range(2):
            for cg in range(ncg):
                z = zf[n % 2]
                G = Gt[n % 2]
                row0 = o * CHN + cg * Cb
                for kc in range(nch):
                    P.dma("sp", z[:, kc], FILT[row0:row0 + Cb, kc * 16384:(kc + 1) * 16384].rearrange("c (a b) -> a c b", b=128), [FILT], [z])
                k = fft.g % 2
                fft.fwd([(kc, z[:, kc]) for kc in range(nch)], z, pa[k], pb[k])
                P.op("act", lambda e, k=k, G=G: e.copy(out=G[:, :, 0:N1], in_=pb[k][:, 0:512].rearrange("p (c k) -> p c k", c=Cb)), [pb[k]], [G])
                P.op("act", lambda e, k=k, G=G: e.copy(out=G[:, :, N1:2 * N1], in_=pb[k][:, 512:1024].rearrange("p (c k) -> p c k", c=Cb)), [pb[k]], [G])
                P.dma("act", GS[o][:, cg * Cb:(cg + 1) * Cb, :], G[:], [G], [GS[o]])
                fft.g += 1
                n += 1
        vt = [sb(f"vt{i}", [H, Cb, 128], BF16) for i in range(2)]
        g1t = [sb(f"g1t{i}", [H, Cb, 128], BF16) for i in range(2)]
        g2t = [sb(f"g2t{i}", [Hown, Cb, 128], BF16) for i in range(2)]
        zt = [sb(f"zt{i}", [H, Cb, 128], BF16) for i in range(2)]
        zs = [sb(f"zs{i}", [H, Cb, 128], BF16) for i in range(2)]
        yt = [sb(f"yt{i}", [Hown, Cb, 128], BF16) for i in range(2)]
        tm = [sb(f"tm{i}", [H, Cb, 128]) for i in range(2)]
        G0 = [sb(f"G0{i}", [128, Cb, 2 * N1], BF16) for i in range(2)]
        G1 = [sb(f"G1{i}", [128, Cb, 2 * N1], BF16) for i in range(2)]
        for cg in range(ncg):
            c0 = cg * Cb
            i = cg % 2
            g0, g1, v, ga, gb, z, zz, y, t = G0[i], G1[i], vt[i], g1t[i], g2t[i], zt[i], zs[i], yt[i], tm[i]
            P.dma("sp", g0[:], GS[0][:, c0:c0 + Cb, :], [GS[0]], [g0])
            P.dma("sp", g1[:], GS[1][:, c0:c0 + Cb, :], [GS[1]], [g1])
            P.dma("sp", v[:], UH[c0:c0 + Cb, 1 + s0:1 + s0 + L].rearrange("c (a b) -> a c b", b=128), [UH], [v])
            P.dma("sp", ga[:], UH[CHN + c0:CHN + c0 + Cb, 1 + s0:1 + s0 + L].rearrange("c (a b) -> a c b", b=128), [UH], [ga])
            P.dma("sp", gb[:], G2[c0:c0 + Cb, own_off:own_off + Hown * 128].rearrange("c (a b) -> a c b", b=128), [G2], [gb])
            k = fft.g % 2
            fft.fwd([(0, v[:])], v, pa[k], pb[k])
            fft.inv(g0, pa[k], pb[k])
            sk = bc_last(skipbc[0:H, 0, c0:c0 + Cb], 128)
            P.op("pool", lambda e, v=v, sk=sk, t=t: e.tensor_tensor(out=t[:], in0=v[:], in1=sk, op=ALU.mult), [v, skipbc], [t])
            P.op("dve", lambda e, t=t, k=k: e.tensor_tensor(out=t[:], in0=t[:], in1=pb[k][0:H, 0:Cb * 128].rearrange("p (c k) -> p c k", c=Cb), op=ALU.add),
                 [t, pb[k]], [t])
            P.op("dve", lambda e, t=t, ga=ga, z=z: e.tensor_tensor(out=z[:], in0=t[:], in1=ga[:], op=ALU.mult), [t, ga], [z])
            sk1 = bc_last(skipbc[0:H, 1, c0:c0 + Cb], 128)
            P.op("pool", lambda e, z=z, sk1=sk1, zz=zz: e.tensor_tensor(out=zz[:], in0=z[:], in1=sk1, op=ALU.mult), [z, skipbc], [zz])
            k = fft.g % 2
            fft.fwd([(0, z[:])], z, pa[k], pb[k])
            fft.inv(g1, pa[k], pb[k], own=True, extra=(fft.SEL[:], zz[:], (fft.SEL, zz)))
            P.op("dve", lambda e, k=k, gb=gb, y=y: e.tensor_tensor(out=y[:], in0=pb[k][0:Hown, 0:Cb * 128].rearrange("p (c k) -> p c k", c=Cb), in1=gb[:], op=ALU.mult),
                 [pb[k], gb], [y])
            P.dma("act", YHo[c0:c0 + Cb, own_off:own_off + Hown * 128].rearrange("c (a b) -> a c b", b=128), y[:], [y], [YHo])
        P.barrier()


NH = 12
HD = 64
DA = NH * HD
KB = 17
NEGV = -30000.0


def attn_sel_table():
    sel = np.zeros((32, 2432), np.float32)
    mult = {}
    for d in (1, 4, 16):
        for j in range(-64, 65):
            mult[j * d] = mult.get(j * d, 0) + 1
    for delta, m in mult.items():
        n = abs(delta)
        ret = 16 if delta > 0 else 0
        nf = np.float32(max(n, 1))
        large = 8 + int(np.float32(np.log(nf / np.float32(8)) / np.float32(math.log(1024 / 8)) * np.float32(8)))
        large = min(large, 15)
        b = ret + (n if n < 8 else large)
        sel[b, delta + 1151] = m
    return sel


def phase_q(P, nc, cfg, x_ext, wq_d, wk_d, wv_d, gmix, gqk_d, kvalid_d, ident_d, bd_d, KT, QT, VE):
    import contextlib
    NE = cfg["NE"]
    own_tiles = cfg["own_tiles"]
    with contextlib.ExitStack() as es:
        sb = lambda n, shp, dt=F32: es.enter_context(nc.sbuf_tensor(n, shp, dt))
        xt = XT(P, nc, es, "q")
        xt.setup(ident_d)
        Wq, Wk, Wv = sb("Wq", [128, DK, DA], BF16), sb("Wk", [128, DK, DA], BF16), sb("Wv", [128, DK, DA], BF16)
        g_sb, gqk, epsT = sb("gq_sb", [128, DK]), sb("gqk_sb", [128, 2]), sb("epsTq", [128, 1])
        ones, BD = sb("ones_q", [128, 128], BF16), sb("BDq", [128, 128], BF16)
        e2 = [sb(f"e2{i}", [128, 512]) for i in range(2)]
        e2c = [sb(f"e2c{i}", [128, 4]) for i in range(2)]
        e2bc = [sb(f"e2bc{i}", [128, 4, 128]) for i in range(2)]
        identf = sb("identf", [128, 128])
        ssq, rcol = [sb(f"ssq{i}", [128, 4]) for i in range(2)], [sb(f"rcol{i}", [128, 4]) for i in range(2)]
        junk = sb("junkq", [128, D], BF16)
        sqk = [sb(f"sqk{i}", [128, 512], BF16) for i in range(2)]
        den = [sb(f"den{i}", [128, 512]) for i in range(2)]
        kn = [sb(f"kn{i}", [128, 512], BF16) for i in range(2)]
        Vt = [sb(f"Vt{i}", [128, NH, HD + 1], BF16) for i in range(2)]
        pss = es.enter_context(nc.psum_tensor("pssq", [128, 512], F32))
        pk = [es.enter_context(nc.psum_tensor(f"pk{i}", [128, 512], F32)) for i in range(2)]
        pst = es.enter_context(nc.psum_tensor("pst", [128, 512], F32))
        pv = es.enter_context(nc.psum_tensor("pv", [128, 2, 512], F32))
        P.op("pool", lambda e: e.memset(ones[:], 1.0), [], [ones])
        P.op("pool", lambda e: e.memset(epsT[:], EPS), [], [epsT])
        for i in range(2):
            P.op("pool", lambda e, i=i: e.memset(Vt[i][:], 1.0), [], [Vt[i]])
            P.op("pool", lambda e, i=i: e.memset(ssq[i][:], 0.0), [], [ssq[i]])
        P.dma("sp", g_sb[:], gmix)
        P.dma("sp", gqk[:], gqk_d)
        P.op("dve", lambda e: e.tensor_scalar(out=gqk[:, 0:1], in0=gqk[:, 0:1], scalar1=0.125, scalar2=None, op0=ALU.mult), [gqk], [gqk])
        stage = xt.xin[0]
        P.dma("sp", stage[:, 0, 0:128], bd_d, None, [stage])
        P.op("dve", lambda e: e.tensor_copy(out=BD[:], in_=stage[:, 0, 0:128]), [stage], [BD])
        P.dma("sp", identf[:], ident_d)
        for h in range(NH):
            P.dma("act", KT[h, 64:65, :], kvalid_d, [], [("KTrow", h)])
            P.dma("act", QT[h, 64:65, :], cfg["ones_row"], [], [("QTrow", h)])
        for W, wd in ((Wq, wq_d), (Wk, wk_d), (Wv, wv_d)):
            for j in range(DK):
                P.dma("sp", stage[:, 0, 0:DA], wd[j * 128:(j + 1) * 128, :], None, [stage])
                P.op("dve", lambda e, j=j, W=W: e.tensor_scalar(out=W[:, j, :], in0=stage[:, 0, 0:DA], scalar1=g_sb[:, j:j + 1],
                                                               scalar2=None, op0=ALU.mult), [stage, g_sb], [W])
        for it in range(NE // 512):
            t0 = it * 512
            xin, xbf, xT = xt.load(x_ext[t0:t0 + 512, :])
            b = it % 2
            for s in range(4):
                P.op("act", lambda e, s=s: e.activation(out=junk[:], in_=xin[:, s, :], func=AF.Square, accum_out=ssq[b][:, s:s + 1]), [xin], [junk, ssq[b]])
            P.op("act", lambda e: e.activation(out=rcol[b][:], in_=ssq[b][:], func=AF.Sqrt, bias=epsT[:], scale=1.0 / D), [ssq[b], epsT], [rcol[b]])
            P.op("dve", lambda e: e.reciprocal(out=rcol[b][:], in_=rcol[b][:]), [rcol[b]], [rcol[b]])
            P.op("dve", lambda e: e.tensor_scalar(out=e2c[b][:], in0=ssq[b][:], scalar1=64.0 * EPS / D, scalar2=64.0 * EPS * EPS, op0=ALU.mult, op1=ALU.add),
                 [ssq[b]], [e2c[b]])
            P.op("dve", lambda e: e.tensor_copy(out=e2bc[b][:], in_=bc_last(e2c[b][:, 0:4], 128)), [e2c[b]], [e2bc[b]])
            for s4 in range(4):
                P.op("pe", lambda e, s4=s4: e.matmul(pss[:, s4 * 128:(s4 + 1) * 128], lhsT=e2bc[b][:, s4, :], rhs=identf[:], start=True, stop=True),
                     [e2bc[b], identf], [pss])
            P.op("act", lambda e: e.copy(out=e2[b][:], in_=pss[:]), [pss], [e2[b]])
            P.op("pool", lambda e: e.memset(ssq[b][:], 0.0), [], [ssq[b]])
            jobs = [(Wk, 1, KT, t0)]
            if it in own_tiles:
                jobs.append((Wq, 0, QT, own_tiles[it] * 512))
            n = 0
            for (W, gi, OUT, o0) in jobs:
                for m in range(DA // 128):
                    p = pk[n % 2]
                    i2 = n % 2
                    n += 1
                    for j in range(DK):
                        P.op("pe", lambda e, j=j, m=m, p=p, W=W: e.matmul(p[:], lhsT=W[:, j, m * 128:(m + 1) * 128], rhs=xT[:, j, :],
                                                                          start=(j == 0), stop=(j == DK - 1)), [W, (xT.name, j // 2)], [p])
                    P.op("act", lambda e, p=p, i2=i2: e.activation(out=sqk[i2][:], in_=p[:], func=AF.Square), [p], [sqk[i2]])
                    P.op("pe", lambda e, i2=i2: e.matmul(pst[:], lhsT=BD[:], rhs=sqk[i2][:], start=True, stop=True), [BD, sqk[i2]], [pst])
                    P.op("dve", lambda e, i2=i2: e.tensor_tensor(out=den[i2][:], in0=pst[:], in1=e2[b][:], op=ALU.add), [pst, e2[b]], [den[i2]])
                    P.op("act", lambda e, i2=i2: e.activation(out=den[i2][:], in_=den[i2][:], func=AF.Sqrt, scale=1.0 / 64), [den[i2]], [den[i2]])
                    P.op("dve", lambda e, i2=i2: e.reciprocal(out=den[i2][:], in_=den[i2][:]), [den[i2]], [den[i2]])
                    P.op("dve", lambda e, i2=i2, p=p, gi=gi: e.scalar_tensor_tensor(out=kn[i2][:], in0=p[:], scalar=gqk[:, gi:gi + 1], in1=den[i2][:],
                                                                                    op0=ALU.mult, op1=ALU.mult), [p, gqk, den[i2]], [kn[i2]])
                    for hh in range(2):
                        h = 2 * m + hh
                        P.dma("act", OUT[h, 0:64, o0:o0 + 512], kn[i2][hh * 64:(hh + 1) * 64, :], [kn[i2]], [(OUT.tensor.name, h)])
            for s in range(4):
                for (c0, cn, bk) in ((0, 512, 0), (512, 256, 1)):
                    for j in range(DK):
                        P.op("pe", lambda e, j=j, s=s, c0=c0, cn=cn, bk=bk: e.matmul(pv[:, bk, 0:cn], lhsT=xT[:, j, s * 128:(s + 1) * 128], rhs=Wv[:, j, c0:c0 + cn],
                                                                                    start=(j == 0), stop=(j == DK - 1)), [Wv, (xT.name, j // 2)], [pv])
                vt = Vt[s % 2]
                P.op("act", lambda e, s=s, vt=vt: e.activation(out=vt[:, 0:8, 0:HD], in_=pv[:, 0, :].rearrange("p (h c) -> p h c", c=HD), func=AF.Identity,
                                                               scale=rcol[b][:, s:s + 1]), [pv, rcol[b]], [vt])
                P.op("act", lambda e, s=s, vt=vt: e.activation(out=vt[:, 8:12, 0:HD], in_=pv[:, 1, 0:256].rearrange("p (h c) -> p h c", c=HD), func=AF.Identity,
                                                               scale=rcol[b][:, s:s + 1]), [pv, rcol[b]], [vt])
                P.dma("act", VE[t0 + s * 128:t0 + (s + 1) * 128, :], vt[:].rearrange("p h c -> p (h c)"), [vt], [VE])
        P.barrier()


def phase_attn(P, nc, cfg, relb_d, sel_d, jmat_d, KT, QT, VE, EV, EBD, YA):
    import contextlib
    with contextlib.ExitStack() as es:
        sb = lambda n, shp, dt=F32: es.enter_context(nc.sbuf_tensor(n, shp, dt))
        relb, expb, sel = sb("relb_sb", [32, NH]), sb("expb", [32, NH]), sb("sel_sb", [32, 2432])
        ev = sb("ev", [NH, 2432], BF16)
        J, stage = sb("Jm", [128, 128], BF16), sb("stg_a", [128, 128])
        Gall = [sb(f"Gall{i}", [128, 2304], BF16) for i in range(2)]
        EBt = [sb(f"EBt{i}", [128, KB * 128], BF16) for i in range(2)]
        psA = es.enter_context(nc.psum_tensor("psA", [128, 3, 512], F32))
        psB = es.enter_context(nc.psum_tensor("psB", [128, 2, 512], F32))
        po = [es.enter_context(nc.psum_tensor(f"po{i}", [128, 512], F32)) for i in range(2)]
        P.dma("sp", relb[:], relb_d)
        P.dma("sp", sel[:], sel_d)
        P.dma("sp", stage[:], jmat_d)
        P.op("dve", lambda e: e.tensor_copy(out=J[:], in_=stage[:]), [stage], [J])
        P.op("act", lambda e: e.activation(out=expb[:], in_=relb[:], func=AF.Exp), [relb], [expb])
        for c in range(5):
            w = 512 if c < 4 else 2432 - 2048
            pp = psA[0:NH, 0, 0:w]
            P.op("pe", lambda e, c=c, w=w, pp=pp: e.matmul(pp, lhsT=expb[:], rhs=sel[:, c * 512:c * 512 + w], start=True, stop=True), [expb, sel], [psA])
            P.op("dve", lambda e, c=c, w=w, pp=pp: e.tensor_copy(out=ev[:, c * 512:c * 512 + w], in_=pp), [psA], [ev])
        P.dma("sp", EV, ev[:], [ev], [EV])
        for h in range(NH):
            G = Gall[h % 2]
            E = EBt[h % 2]
            src = bass.AP(tensor=EV.tensor, offset=EV[h, 0:1].offset, ap=[[1, 128], [1, 2304]])
            P.dma("sp", G[:], src, [EV], [G])
            for i in range(KB):
                tgt = psA if i < 12 else psB
                ii = i if i < 12 else i - 12
                P.op("pe", lambda e, i=i, tgt=tgt, ii=ii, G=G: e.matmul(tgt[:, ii // 4, (ii % 4) * 128:(ii % 4 + 1) * 128], lhsT=G[:, i * 128:(i + 1) * 128], rhs=J[:],
                                                                        start=True, stop=True), [G, J], [tgt])
            P.op("dve", lambda e, E=E: e.tensor_copy(out=E[:, 0:1536], in_=psA[:].rearrange("p a b -> p (a b)")), [psA], [E])
            P.op("dve", lambda e, E=E: e.tensor_copy(out=E[:, 1536:KB * 128], in_=psB[:].rearrange("p a b -> p (a b)")[:, 0:KB * 128 - 1536]), [psB], [E])
            P.dma("sp", EBD[h], E[:], [E], [(EBD.tensor.name, h)])
        Lx = max(p[1] for p in cfg["pieces"])
        Lo = max(p[3] for p in cfg["pieces"])
        KTh = [sb(f"KTh{i}", [HD + 1, Lx], BF16) for i in range(2)]
        Vh = [sb(f"Vh{i}", [128, Lx // 128, HD + 1], BF16) for i in range(2)]
        QTh = [sb(f"QTh{i}", [HD + 1, Lo], BF16) for i in range(2)]
        EBh = [sb(f"EBh{i}", [128, KB * 128], BF16) for i in range(2)]
        Et = [sb(f"Et{i}", [128, KB * 128], BF16) for i in range(2)]
        Pt = [sb(f"Pt{i}", [128, KB * 128], BF16) for i in range(2)]
        yat = [sb(f"yat{i}", [128, Lo // 128, HD]) for i in range(2)]
        rs = sb("rs_a", [128, 2])
        n = 0
        nq = 0
        for (ext0, Lext, own0, Lown) in cfg["pieces"]:
            for h in range(NH):
                i = n % 2
                n += 1
                kt, vh, qt_, eb, ya = KTh[i], Vh[i], QTh[i], EBh[i], yat[i]
                P.dma("sp", kt[:, 0:Lext], KT[h, :, ext0:ext0 + Lext], [(KT.tensor.name, h), ("KTrow", h)], [kt])
                P.dma("sp", vh[:, 0:Lext // 128, :], VE[ext0:ext0 + Lext, h * (HD + 1):(h + 1) * (HD + 1)].rearrange("(t p) c -> p t c", p=128), [VE], [vh])
                P.dma("sp", qt_[:, 0:Lown], QT[h, :, own0:own0 + Lown], [(QT.tensor.name, h), ("QTrow", h)], [qt_])
                P.dma("sp", eb[:], EBD[h], [(EBD.tensor.name, h)], [eb])
                for q in range(Lown // 128):
                    j = nq % 2
                    nq += 1
                    et, pt, pq = Et[j], Pt[j], po[j]
                    qs = qt_[:, q * 128:(q + 1) * 128]
                    for i2 in range(KB):
                        if i2 < 9:
                            dst = psA[:, i2 // 4, (i2 % 4) * 128:(i2 % 4 + 1) * 128]
                            key = psA
                        else:
                            dst = psB[:, (i2 - 9) // 4, ((i2 - 9) % 4) * 128:((i2 - 9) % 4 + 1) * 128]
                            key = psB
                        P.op("pe", lambda e, dst=dst, i2=i2, q=q, qs=qs: e.matmul(dst, lhsT=kt[:, (q + i2) * 128:(q + i2 + 1) * 128], rhs=qs, start=True, stop=True),
                             [kt, qt_], [key])
                    for (ps_, c0, w, o0) in ((psA, 0, 512, 0), (psA, 1, 512, 512), (psA, 2, 128, 1024), (psB, 0, 512, 1152), (psB, 1, 512, 1664)):
                        P.op("act", lambda e, ps_=ps_, c0=c0, w=w, o0=o0: e.activation(out=et[:, o0:o0 + w], in_=ps_[:, c0, 0:w], func=AF.Exp), [ps_], [(et.name, o0 >= 1152)])
                    P.op("dve", lambda e: e.tensor_tensor(out=pt[:, 0:1152], in0=et[:, 0:1152], in1=eb[:, 0:1152], op=ALU.mult), [(et.name, False), eb], [(pt.name, False)])
                    P.op("dve", lambda e: e.tensor_tensor(out=pt[:, 1152:], in0=et[:, 1152:], in1=eb[:, 1152:], op=ALU.mult), [(et.name, True), eb], [(pt.name, True)])
                    for i2 in range(KB):
                        P.op("pe", lambda e, i2=i2, q=q: e.matmul(pq[:, 0:HD + 1], lhsT=pt[:, i2 * 128:(i2 + 1) * 128], rhs=vh[:, q + i2, :], start=(i2 == 0), stop=(i2 == KB - 1)),
                             [(pt.name, i2 >= 9), vh], [pq])
                    P.op("dve", lambda e, j=j: e.reciprocal(out=rs[:, j:j + 1], in_=pq[:, HD:HD + 1]), [pq], [(rs.name, j)])
                    P.op("dve", lambda e, j=j, q=q: e.tensor_scalar(out=ya[:, q, :], in0=pq[:, 0:HD], scalar1=rs[:, j:j + 1], scalar2=None, op0=ALU.mult),
                         [pq, (rs.name, j)], [ya])
                P.dma("act", YA[own0:own0 + Lown, h * HD:(h + 1) * HD].rearrange("(t p) c -> p t c", p=128), ya[:, 0:Lown // 128, :], [ya], [YA])
        P.barrier()


NEXP = 32
DE = 1024
BLK = 256
NBLK = 80
NSLOT = NBLK * BLK


def indirect_gather(P, out, in_, idx_ap, reads, writes):
    P.dma("pool", out, in_, reads, writes, fn=lambda e: e.indirect_dma_start(
        out=out, out_offset=None, in_=in_, in_offset=bass.IndirectOffsetOnAxis(ap=idx_ap, axis=0)))


def indirect_scatter(P, out, in_, idx_ap, reads, writes):
    P.dma("pool", out, in_, reads, writes, fn=lambda e: e.indirect_dma_start(
        out=out, out_offset=bass.IndirectOffsetOnAxis(ap=idx_ap, axis=0), in_=in_, in_offset=None))


def phase3(P, nc, cfg, d, YHo, YA, X1, H2B, AAd, GTd):
    import contextlib
    NOWN = cfg["NOWN"]
    NT = NOWN // 128
    CH = 1280 // 128
    CA = DA // 128
    with contextlib.ExitStack() as es:
        sb = lambda n, shp, dt=F32: es.enter_context(nc.sbuf_tensor(n, shp, dt))
        Wo = sb("Wo", [128, DK, D], BF16)
        gh, ga, gfb, Wr, rbb = sb("gh3", [128, CH]), sb("ga3", [128, CA]), sb("gfb", [128, D]), sb("Wr", [128, DK, 36]), sb("rbb", [128, 36])
        identf, identb, ones, epsT = sb("identf3", [128, 128]), sb("identb3", [128, 128], BF16), sb("ones3", [128, 128], BF16), sb("epsT3", [128, 1])
        stage = sb("stage3", [128, D])
        yh = [sb(f"yh{i}", [128, CH, 512], BF16) for i in range(2)]
        sq = sb("sq3", [128, CH, 512], BF16)
        rh = sb("rh3", [128, 512])
        yhn = sb("yhn", [128, CH, 512], BF16)
        yat = [sb(f"yat3{i}", [128, DA]) for i in range(2)]
        yan = sb("yan", [128, DA], BF16)
        yaT = sb("yaT", [128, CA, 512], BF16)
        xo = [sb(f"xo{i}", [128, D]) for i in range(2)]
        x1 = sb("x1t", [128, D])
        h2 = sb("h2t", [128, D])
        h2b = [sb(f"h2b{i}", [128, D], BF16) for i in range(2)]
        h2T = sb("h2T", [128, DK, 128])
        junk = sb("junk3", [128, D], BF16)
        sm = sb("sm3", [128, 16])
        lg = sb("lg3", [128, 36])
        ohg, eg, esel, oh1, e2, oh2 = sb("ohg", [128, 4]), sb("eg", [128, 4]), sb("esel", [128, 8]), sb("oh1", [128, 8]), sb("e2r", [128, 8]), sb("oh2", [128, 8])
        AA = sb("AA3", [128, NT, 64])
        GT = sb("GT3", [128, NT, 2])
        pss = es.enter_context(nc.psum_tensor("pss3", [128, 512], F32))
        ptr = es.enter_context(nc.psum_tensor("ptr3", [128, CA, 128], BF16))
        po = [es.enter_context(nc.psum_tensor(f"po3{i}", [128, 512], F32)) for i in range(4)]
        pt2 = es.enter_context(nc.psum_tensor("pt23", [128, 4, 128], F32))
        pr = es.enter_context(nc.psum_tensor("pr3", [128, 64], F32))
        P.op("pool", lambda e: e.memset(ones[:], 1.0), [], [ones])
        P.op("pool", lambda e: e.memset(epsT[:], EPS), [], [epsT])
        P.op("pool", lambda e: e.memset(sm[:], 0.0), [], [(sm.name, c) for c in range(16)])
        P.dma("sp", gh[:], d["gh"])
        P.dma("sp", ga[:], d["ga"])
        P.dma("sp", gfb[:], bcast_rows(d["gf"], 128))
        P.dma("sp", rbb[:], bcast_rows(d["rb"], 128))
        P.dma("sp", Wr[:], d["wr"])
        P.dma("sp", identf[:], d["ident"])
        P.op("dve", lambda e: e.tensor_copy(out=identb[:], in_=identf[:]), [identf], [identb])
        for j in range(DK):
            P.dma("sp", stage[:], d["w_out"][j * 128:(j + 1) * 128, :], None, [stage])
            P.op("dve" if j % 2 else "act", (lambda e, j=j: e.tensor_copy(out=Wo[:, j, :], in_=stage[:])) if j % 2 else (lambda e, j=j: e.copy(out=Wo[:, j, :], in_=stage[:])),
                 [stage], [Wo])
        for st in range(NOWN // 512):
            t0 = st * 512
            y = yh[st % 2]
            P.dma("sp", y[:], YHo[:, t0:t0 + 512].rearrange("(k p) t -> p k t", p=128), [YHo], [y])
            P.op("pool", lambda e, y=y: e.tensor_tensor(out=sq[:], in0=y[:], in1=y[:], op=ALU.mult), [y], [sq])
            for k in range(CH):
                P.op("pe", lambda e, k=k: e.matmul(pss[:], lhsT=ones[:], rhs=sq[:, k, :], start=(k == 0), stop=(k == CH - 1)), [ones, sq], [pss])
            P.op("act", lambda e: e.activation(out=rh[:], in_=pss[:], func=AF.Sqrt, bias=epsT[:], scale=1.0 / 1280), [pss, epsT], [rh])
            P.op("dve", lambda e: e.reciprocal(out=rh[:], in_=rh[:]), [rh], [rh])
            for k in range(CH):
                P.op("dve", lambda e, k=k, y=y: e.scalar_tensor_tensor(out=yhn[:, k, :], in0=y[:, k, :], scalar=gh[:, k:k + 1], in1=rh[:], op0=ALU.mult, op1=ALU.mult),
                     [y, gh, rh], [(yhn.name, k)])
            for s in range(4):
                tt = st * 4 + s
                r0 = t0 + s * 128
                ya, xin, hb = yat[tt % 2], xo[tt % 2], h2b[tt % 2]
                P.dma("sp", ya[:], YA[r0:r0 + 128, :], [YA], [ya])
                P.dma("sp", xin[:], d["x_own_fn"](r0), None, [xin])
                P.op("act", lambda e, ya=ya: e.activation(out=junk[:, 0:DA], in_=ya[:], func=AF.Square, accum_out=sm[:, 0:1]), [ya], [junk, (sm.name, 0)])
                P.op("act", lambda e: e.activation(out=sm[:, 1:2], in_=sm[:, 0:1], func=AF.Sqrt, bias=epsT[:], scale=1.0 / DA), [(sm.name, 0), epsT], [(sm.name, 1)])
                P.op("dve", lambda e: e.reciprocal(out=sm[:, 1:2], in_=sm[:, 1:2]), [(sm.name, 1)], [(sm.name, 1)])
                P.op("pool", lambda e: e.memset(sm[:, 0:1], 0.0), [], [(sm.name, 0)])
                P.op("act", lambda e, ya=ya: e.activation(out=yan[:], in_=ya[:], func=AF.Identity, scale=sm[:, 1:2]), [ya, (sm.name, 1)], [yan])
                for k in range(CA):
                    P.op("pe", lambda e, k=k: e.transpose(out=ptr[:, k, :], in_=yan[:, k * 128:(k + 1) * 128], identity=identb[:]), [yan, identb], [ptr])
                for k in range(CA):
                    P.op("dve", lambda e, k=k, s=s: e.tensor_scalar(out=yaT[:, k, s * 128:(s + 1) * 128], in0=ptr[:, k, :], scalar1=ga[:, k:k + 1], scalar2=None, op0=ALU.mult),
                         [ptr, ga], [(yaT.name, s)])
                for n in range(4):
                    for k in range(DK):
                        if k < CH:
                            lhs, key = yhn[:, k, s * 128:(s + 1) * 128], (yhn.name, k)
                        else:
                            lhs, key = yaT[:, k - CH, s * 128:(s + 1) * 128], (yaT.name, s)
                        P.op("pe", lambda e, n=n, k=k, lhs=lhs: e.matmul(po[n][:], lhsT=lhs, rhs=Wo[:, k, n * 512:(n + 1) * 512], start=(k == 0), stop=(k == DK - 1)),
                             [key, Wo], [po[n]])
                    P.op("dve", lambda e, n=n, xin=xin: e.tensor_tensor(out=x1[:, n * 512:(n + 1) * 512], in0=po[n][:], in1=xin[:, n * 512:(n + 1) * 512], op=ALU.add),
                         [po[n], xin], [x1])
                P.dma("act", X1[r0:r0 + 128, :], x1[:], [x1], [X1])
                P.op("act", lambda e: e.activation(out=junk[:], in_=x1[:], func=AF.Square, accum_out=sm[:, 2:3]), [x1], [junk, (sm.name, 2)])
                P.op("act", lambda e: e.activation(out=sm[:, 3:4], in_=sm[:, 2:3], func=AF.Sqrt, bias=epsT[:], scale=1.0 / D), [(sm.name, 2), epsT], [(sm.name, 3)])
                P.op("dve", lambda e: e.reciprocal(out=sm[:, 3:4], in_=sm[:, 3:4]), [(sm.name, 3)], [(sm.name, 3)])
                P.op("pool", lambda e: e.memset(sm[:, 2:3], 0.0), [], [(sm.name, 2)])
                P.op("dve", lambda e: e.scalar_tensor_tensor(out=h2[:], in0=x1[:], scalar=sm[:, 3:4], in1=gfb[:], op0=ALU.mult, op1=ALU.mult), [x1, (sm.name, 3), gfb], [h2])
                P.op("act", lambda e, hb=hb: e.copy(out=hb[:], in_=h2[:]), [h2], [hb])
                P.dma("act", H2B[r0:r0 + 128, :], hb[:], [hb], [H2B])
                for jj in range(4):
                    for j2 in range(4):
                        j = jj * 4 + j2
                        P.op("pe", lambda e, j=j, j2=j2: e.transpose(out=pt2[:, j2, :], in_=h2[:, j * 128:(j + 1) * 128], identity=identf[:]), [h2, identf], [pt2])
                    P.op("act" if jj % 2 else "dve", (lambda e, jj=jj: e.copy(out=h2T[:, jj * 4:(jj + 1) * 4, :], in_=pt2[:])) if jj % 2 else
                         (lambda e, jj=jj: e.tensor_copy(out=h2T[:, jj * 4:(jj + 1) * 4, :], in_=pt2[:])), [pt2], [h2T])
                for j in range(DK):
                    P.op("pe", lambda e, j=j: e.matmul(pr[:, 0:36], lhsT=h2T[:, j, :], rhs=Wr[:, j, :], start=(j == 0), stop=(j == DK - 1)), [h2T, Wr], [pr])
                P.op("dve", lambda e: e.tensor_tensor(out=lg[:], in0=pr[:, 0:36], in1=rbb[:], op=ALU.add), [pr, rbb], [lg])
                R = lambda c: (sm.name, c)
                lgg = lg[:, 0:4]
                lge = lg[:, 4:36].rearrange("p (g j) -> p g j", g=4)
                P.op("dve", lambda e: e.tensor_reduce(out=sm[:, 4:5], in_=lgg, axis=AX.X, op=ALU.max), [lg], [R(4)])
                P.op("dve", lambda e: e.tensor_scalar(out=ohg[:], in0=lgg, scalar1=sm[:, 4:5], scalar2=None, op0=ALU.is_equal), [lg, R(4)], [ohg])
                P.op("dve", lambda e: e.tensor_scalar(out=sm[:, 5:6], in0=sm[:, 4:5], scalar1=-1.0, scalar2=None, op0=ALU.mult), [R(4)], [R(5)])
                P.op("act", lambda e: e.activation(out=eg[:], in_=lgg, func=AF.Exp, bias=sm[:, 5:6], scale=1.0, accum_out=sm[:, 6:7]), [lg, R(5)], [eg, R(6)])
                P.op("dve", lambda e: e.reciprocal(out=sm[:, 7:8], in_=sm[:, 6:7]), [R(6)], [R(7)])
                P.op("pool", lambda e: e.memset(sm[:, 6:7], 0.0), [], [R(6)])
                P.op("dve", lambda e: e.tensor_scalar(out=esel[:], in0=lge[:, 0, :], scalar1=ohg[:, 0:1], scalar2=None, op0=ALU.mult), [lg, ohg], [esel])
                for g in range(1, 4):
                    P.op("dve", lambda e, g=g: e.scalar_tensor_tensor(out=esel[:], in0=lge[:, g, :], scalar=ohg[:, g:g + 1], in1=esel[:], op0=ALU.mult, op1=ALU.add),
                         [lg, ohg, esel], [esel])
                P.op("dve", lambda e: e.tensor_reduce(out=sm[:, 8:9], in_=esel[:], axis=AX.X, op=ALU.max), [esel], [R(8)])
                P.op("dve", lambda e: e.tensor_scalar(out=oh1[:], in0=esel[:], scalar1=sm[:, 8:9], scalar2=None, op0=ALU.is_equal), [esel, R(8)], [oh1])
                P.op("dve", lambda e: e.scalar_tensor_tensor(out=e2[:], in0=oh1[:], scalar=-1.0e9, in1=esel[:], op0=ALU.mult, op1=ALU.add), [oh1, esel], [e2])
                P.op("dve", lambda e: e.tensor_reduce(out=sm[:, 9:10], in_=e2[:], axis=AX.X, op=ALU.max), [e2], [R(9)])
                P.op("dve", lambda e: e.tensor_scalar(out=oh2[:], in0=e2[:], scalar1=sm[:, 9:10], scalar2=None, op0=ALU.is_equal), [e2, R(9)], [oh2])
                P.op("dve", lambda e: e.tensor_tensor(out=sm[:, 10:11], in0=sm[:, 9:10], in1=sm[:, 8:9], op=ALU.subtract), [R(9), R(8)], [R(10)])
                P.op("act", lambda e: e.activation(out=sm[:, 11:12], in_=sm[:, 10:11], func=AF.Exp), [R(10)], [R(11)])
                P.op("dve", lambda e: e.tensor_scalar(out=sm[:, 12:13], in0=sm[:, 11:12], scalar1=1.0, scalar2=None, op0=ALU.add), [R(11)], [R(12)])
                P.op("dve", lambda e: e.reciprocal(out=sm[:, 12:13], in_=sm[:, 12:13]), [R(12)], [R(12)])
                P.op("dve", lambda e, tt=tt: e.tensor_tensor(out=GT[:, tt, 0:1], in0=sm[:, 12:13], in1=sm[:, 7:8], op=ALU.mult), [R(12), R(7)], [GT])
                P.op("dve", lambda e, tt=tt: e.tensor_tensor(out=GT[:, tt, 1:2], in0=GT[:, tt, 0:1], in1=sm[:, 11:12], op=ALU.mult), [GT, R(11)], [GT])
                P.op("dve", lambda e, tt=tt: e.tensor_tensor(out=AA[:, tt, 0:32].rearrange("p (g j) -> p g j", g=4), in0=bc_last(ohg[:], 8), in1=bc_mid(oh1[:], 4), op=ALU.mult),
                     [ohg, oh1], [AA])
                P.op("dve", lambda e, tt=tt: e.tensor_tensor(out=AA[:, tt, 32:64].rearrange("p (g j) -> p g j", g=4), in0=bc_last(ohg[:], 8), in1=bc_mid(oh2[:], 4), op=ALU.mult),
                     [ohg, oh2], [AA])
        P.dma("sp", AAd, AA[:], [AA], [AAd])
        P.dma("sp", GTd, GT[:], [GT], [GTd])
        P.barrier()


def phase_moe(P, nc, cfg, d, X1, H2B, AAd, GTd, XB, YB, y_out):
    import contextlib
    NOWN = cfg["NOWN"]
    NT = NOWN // 128
    NBLK = cfg.get("NBLK", 128)
    with contextlib.ExitStack() as es:
        sb = lambda n, shp, dt=F32: es.enter_context(nc.sbuf_tensor(n, shp, dt))
        es0 = es.enter_context(contextlib.ExitStack())
        sb0 = lambda n, shp, dt=F32: es0.enter_context(nc.sbuf_tensor(n, shp, dt))
        GT = sb("GTm", [128, NT, 2])
        identb = sb("identbm", [128, 128], BF16)
        dst = sb("dsti", [128, NT, 2], I32)
        idxg, idxd = sb("idxg", [128, NBLK, DK], I32), sb("idxd", [128, NBLK, DK // 2], I32)
        AA = sb0("AAm", [128, NT, 64])
        identf, onesf, utri = sb0("identfm", [128, 128]), sb0("onesfm", [128, 128]), sb0("utrim", [128, 128])
        ltm, bpos, offg = sb0("ltm_sb", [128, 32, 32]), sb0("bposm", [128, 1]), sb0("offgm", [128, DK])
        cnt, padi, pad, pend, pstart, base = sb0("cntm", [128, 32]), sb0("padi", [128, 32], I32), sb0("padm", [128, 32]), sb0("pendm", [128, 32]), sb0("pstm", [128, 32]), sb0("basem", [128, 32])
        tmp3 = sb0("tmp3m", [128, 32, 32])
        off, prod = sb0("offm", [128, 32]), sb0("prodm", [128, 32])
        dstf = sb0("dstf", [128, NT, 2])
        cmp_, bef, be2 = sb0("cmpm", [128, 32]), sb0("befm", [128, 1]), sb0("be2m", [128, 128])
        bebc = sb0("bebc", [128, 128])
        idxf = sb0("idxfm", [128, NBLK, DK])
        pm = es.enter_context(nc.psum_tensor("pmm", [128, 128], F32))
        P.dma("sp", AA[:], AAd)
        P.dma("sp", GT[:], GTd)
        P.dma("sp", identf[:], d["ident"])
        P.dma("sp", utri[:], d["utri"])
        P.dma("sp", ltm[:].rearrange("p a b -> p (a b)"), bcast_rows(d["ltm"], 128))
        P.dma("sp", bpos[:], d["bpos"])
        P.dma("sp", offg[:], d["offg"])
        P.op("dve", lambda e: e.tensor_copy(out=identb[:], in_=identf[:]), [identf], [identb])
        P.op("pool", lambda e: e.memset(onesf[:], 1.0), [], [onesf])
        P.op("pool", lambda e: e.memset(base[:], 0.0), [], [base])
        for i in range(NT):
            P.op("pe", lambda e, i=i: e.matmul(pm[:, 0:64], lhsT=onesf[:], rhs=AA[:, i, :], start=(i == 0), stop=(i == NT - 1)), [onesf, AA], [pm])
        P.op("dve", lambda e: e.tensor_copy(out=cnt[:], in_=pm[:, 0:32]), [pm], [cnt])
        P.op("dve", lambda e: e.tensor_tensor(out=cnt[:], in0=cnt[:], in1=pm[:, 32:64], op=ALU.add), [cnt, pm], [cnt])
        P.op("dve", lambda e: e.tensor_scalar(out=padi[:], in0=cnt[:], scalar1=float(BLK - 1), scalar2=None, op0=ALU.add), [cnt], [padi])
        P.op("dve", lambda e: e.tensor_scalar(out=padi[:], in0=padi[:], scalar1=8, scalar2=8, op0=ALU.arith_shift_right, op1=ALU.logical_shift_left), [padi], [padi])
        P.op("dve", lambda e: e.tensor_copy(out=pad[:], in_=padi[:]), [padi], [pad])
        P.op("dve", lambda e: e.tensor_tensor(out=tmp3[:], in0=bc_mid(pad[:], 32), in1=ltm[:], op=ALU.mult), [pad, ltm], [tmp3])
        P.op("dve", lambda e: e.reduce_sum(out=pend[:], in_=tmp3[:], axis=AX.X), [tmp3], [pend])
        P.op("dve", lambda e: e.tensor_tensor(out=pstart[:], in0=pend[:], in1=pad[:], op=ALU.subtract), [pend, pad], [pstart])
        for i in range(NT):
            P.op("pe", lambda e, i=i: e.matmul(pm[:, 0:64], lhsT=utri[:], rhs=AA[:, i, :], start=True, stop=True), [utri, AA], [pm])
            P.op("pe", lambda e, i=i: e.matmul(pm[:, 64:128], lhsT=onesf[:], rhs=AA[:, i, :], start=True, stop=True), [onesf, AA], [pm])
            P.op("dve", lambda e: e.tensor_tensor(out=off[:], in0=pstart[:], in1=base[:], op=ALU.add), [pstart, base], [off])
            P.op("dve", lambda e: e.tensor_tensor(out=prod[:], in0=off[:], in1=pm[:, 0:32], op=ALU.add), [off, pm], [prod])
            P.op("dve", lambda e, i=i: e.tensor_tensor(out=prod[:], in0=prod[:], in1=AA[:, i, 0:32], op=ALU.mult), [prod, AA], [prod])
            P.op("dve", lambda e, i=i: e.reduce_sum(out=dstf[:, i, 0:1], in_=prod[:], axis=AX.X), [prod], [dstf])
            P.op("dve", lambda e: e.tensor_tensor(out=off[:], in0=off[:], in1=pm[:, 64:96], op=ALU.add), [off, pm], [off])
            P.op("dve", lambda e: e.tensor_tensor(out=prod[:], in0=off[:], in1=pm[:, 32:64], op=ALU.add), [off, pm], [prod])
            P.op("dve", lambda e, i=i: e.tensor_tensor(out=prod[:], in0=prod[:], in1=AA[:, i, 32:64], op=ALU.mult), [prod, AA], [prod])
            P.op("dve", lambda e, i=i: e.reduce_sum(out=dstf[:, i, 1:2], in_=prod[:], axis=AX.X), [prod], [dstf])
            P.op("dve", lambda e: e.tensor_tensor(out=base[:], in0=base[:], in1=pm[:, 64:96], op=ALU.add), [base, pm], [base])
            P.op("dve", lambda e: e.tensor_tensor(out=base[:], in0=base[:], in1=pm[:, 96:128], op=ALU.add), [base, pm], [base])
        P.op("dve", lambda e: e.tensor_copy(out=dst[:], in_=dstf[:]), [dstf], [dst])
        P.op("dve", lambda e: e.tensor_scalar(out=cmp_[:], in0=pend[:], scalar1=bpos[:, 0:1], scalar2=None, op0=ALU.is_le), [pend, bpos], [cmp_])
        P.op("dve", lambda e: e.reduce_sum(out=bef[:], in_=cmp_[:], axis=AX.X), [cmp_], [bef])
        P.op("dve", lambda e: e.tensor_scalar(out=bef[:], in0=bef[:], scalar1=float(NEXP - 1), scalar2=None, op0=ALU.min), [bef], [bef])
        P.op("dve", lambda e: e.tensor_copy(out=bebc[:], in_=bc_col(bef[:, 0:1], 128)), [bef], [bebc])
        P.op("pe", lambda e: e.matmul(pm[:], lhsT=bebc[:], rhs=identf[:], start=True, stop=True), [bebc, identf], [pm])
        P.op("dve", lambda e: e.tensor_copy(out=be2[:], in_=pm[:]), [pm], [be2])
        for (scale, tgt, nj) in ((float(D), idxg, DK), (float(DE), idxd, DK // 2)):
            P.op("dve", lambda e, scale=scale, nj=nj: e.scalar_tensor_tensor(out=idxf[:, :, 0:nj], in0=bc_last(be2[:, 0:NBLK], nj), scalar=scale, in1=bc_mid(offg[:, 0:nj], NBLK),
                                                                              op0=ALU.mult, op1=ALU.add), [be2, offg], [idxf])
            P.op("dve", lambda e, tgt=tgt, nj=nj: e.tensor_copy(out=tgt[:], in_=idxf[:, :, 0:nj]), [idxf], [tgt])
        es1 = es.enter_context(contextlib.ExitStack())
        hb = [es1.enter_context(nc.sbuf_tensor(f"hbm{i}", [128, D], BF16)) for i in range(2)]
        for i in range(NT):
            h = hb[i % 2]
            P.dma("sp", h[:], H2B[i * 128:(i + 1) * 128, :], [H2B], [h])
            for k in range(2):
                indirect_scatter(P, XB, h[:], dst[:, i, k:k + 1], [h, dst], [XB])
        P.barrier()
        es1.close()
        es0.close()
        es2 = es.enter_context(contextlib.ExitStack())
        sb2 = lambda n, shp, dt=F32: es2.enter_context(nc.sbuf_tensor(n, shp, dt))
        stg = [sb2(f"wst{i}", [128, 4, DE]) for i in range(2)]
        Wg, Wu, Wd = sb2("Wgb", [128, DK, DE], BF16), sb2("Wub", [128, DK, DE], BF16), sb2("Wdb", [128, DK // 2, D], BF16)
        xb = [sb2(f"xbm{i}", [128, D], BF16) for i in range(2)]
        xbT = [sb2(f"xbT{i}", [128, DK, 128], BF16) for i in range(2)]
        sg = [sb2(f"sgm{i}", [128, 128]) for i in range(2)]
        aT = sb2("aTm", [128, DK // 2, 128], BF16)
        yb = [sb2(f"ybm{i}", [128, D]) for i in range(2)]
        ptx = es2.enter_context(nc.psum_tensor("ptxm", [128, 8, 128], BF16))
        pg = [es2.enter_context(nc.psum_tensor(f"pgm{i}", [128, 128], F32)) for i in range(2)]
        pu = [es2.enter_context(nc.psum_tensor(f"pum{i}", [128, 128], F32)) for i in range(2)]
        pd = [es2.enter_context(nc.psum_tensor(f"pdm{i}", [128, 512], F32)) for i in range(2)]
        ns = 0
        ce = 0
        cast_engs = ("act", "dve")
        nsub = 0
        for b in range(NBLK):
            for (Wsrc, Wdst, idx, nchunk, width) in ((d["w_gate"], Wg, idxg, DK, DE), (d["w_up"], Wu, idxg, DK, DE), (d["w_down"], Wd, idxd, DK // 2, D)):
                per = 4 * DE // width
                for c0 in range(0, nchunk, per):
                    st = stg[ns % 2]
                    ns += 1
                    stv = st[:].rearrange("p a b -> p (a b)").rearrange("p (a b) -> p a b", b=width)
                    for c in range(per):
                        indirect_gather(P, stv[:, c, :], Wsrc, idx[:, b, c0 + c:c0 + c + 1], [idx], [st])
                    eng = cast_engs[ce % 2]
                    ce += 1
                    dstv = Wdst[:, c0:c0 + per, :]
                    if eng == "act":
                        P.op("act", lambda e, dstv=dstv, stv=stv: e.copy(out=dstv, in_=stv), [st], [Wdst])
                    else:
                        P.op(eng, lambda e, dstv=dstv, stv=stv: e.tensor_copy(out=dstv, in_=stv), [st], [Wdst])
            for sub in range(BLK // 128):
                r0 = b * BLK + sub * 128
                x, xT, y = xb[nsub % 2], xbT[nsub % 2], yb[nsub % 2]
                nsub += 1
                P.dma("sp", x[:], XB[r0:r0 + 128, :], [XB], [x])
                for hh in range(2):
                    for j2 in range(8):
                        j = hh * 8 + j2
                        P.op("pe", lambda e, j=j, j2=j2: e.transpose(out=ptx[:, j2, :], in_=x[:, j * 128:(j + 1) * 128], identity=identb[:]), [x, identb], [ptx])
                    P.op("dve", lambda e, hh=hh: e.tensor_copy(out=xT[:, hh * 8:(hh + 1) * 8, :], in_=ptx[:]), [ptx], [xT])
                for fc in range(DE // 128):
                    pgg, puu, sgg = pg[fc % 2], pu[fc % 2], sg[fc % 2]
                    for j in range(DK):
                        P.op("pe", lambda e, j=j, fc=fc, pgg=pgg: e.matmul(pgg[:], lhsT=Wg[:, j, fc * 128:(fc + 1) * 128], rhs=xT[:, j, :], start=(j == 0), stop=(j == DK - 1)),
                             [Wg, xT], [pgg])
                    for j in range(DK):
                        P.op("pe", lambda e, j=j, fc=fc, puu=puu: e.matmul(puu[:], lhsT=Wu[:, j, fc * 128:(fc + 1) * 128], rhs=xT[:, j, :], start=(j == 0), stop=(j == DK - 1)),
                             [Wu, xT], [puu])
                    P.op("act", lambda e, pgg=pgg, sgg=sgg: e.activation(out=sgg[:], in_=pgg[:], func=AF.Silu), [pgg], [sgg])
                    P.op("dve", lambda e, fc=fc, puu=puu, sgg=sgg: e.tensor_tensor(out=aT[:, fc, :], in0=puu[:], in1=sgg[:], op=ALU.mult), [puu, sgg], [aT])
                for n in range(4):
                    pdd = pd[n % 2]
                    for fc in range(DE // 128):
                        P.op("pe", lambda e, n=n, fc=fc, pdd=pdd: e.matmul(pdd[:], lhsT=aT[:, fc, :], rhs=Wd[:, fc, n * 512:(n + 1) * 512], start=(fc == 0), stop=(fc == DE // 128 - 1)),
                             [aT, Wd], [pdd])
                    if n % 2 == 0:
                        P.op("act", lambda e, n=n, pdd=pdd: e.copy(out=y[:, n * 512:(n + 1) * 512], in_=pdd[:]), [pdd], [y])
                    else:
                        P.op("dve", lambda e, n=n, pdd=pdd: e.tensor_copy(out=y[:, n * 512:(n + 1) * 512], in_=pdd[:]), [pdd], [y])
                P.dma("sp", YB[r0:r0 + 128, :], y[:], [y], [YB])
        P.barrier()
        es2.close()
        y1 = [sb(f"y1m{i}", [128, D]) for i in range(2)]
        y2 = [sb(f"y2m{i}", [128, D]) for i in range(2)]
        xr = [sb(f"xrm{i}", [128, D]) for i in range(2)]
        for i in range(NT):
            a, bb, x = y1[i % 2], y2[i % 2], xr[i % 2]
            P.dma("sp", x[:], X1[i * 128:(i + 1) * 128, :], [X1], [x])
            indirect_gather(P, a[:], YB, dst[:, i, 0:1], [YB, dst], [a])
            indirect_gather(P, bb[:], YB, dst[:, i, 1:2], [YB, dst], [bb])
            P.op("dve", lambda e, i=i, a=a, x=x: e.scalar_tensor_tensor(out=x[:], in0=a[:], scalar=GT[:, i, 0:1], in1=x[:], op0=ALU.mult, op1=ALU.add), [a, GT, x], [x])
            P.op("dve", lambda e, i=i, bb=bb, x=x: e.scalar_tensor_tensor(out=x[:], in0=bb[:], scalar=GT[:, i, 1:2], in1=x[:], op0=ALU.mult, op1=ALU.add), [bb, GT, x], [x])
            P.dma("sp", y_out[i * 128:(i + 1) * 128, :], x[:], [x], [y_out])
        P.barrier()


LP, LS_ = 8192, 16384
CHN_ALL = 1280
NOWN_ = 6144
NE_ = 10240
MIN_DECAY_ = math.log(1e-2) / 1.5
MAX_DECAY_ = math.log(1e-2) / 0.3


def _circle_tables(L):
    pos = np.arange(L, dtype=np.float64)
    t = np.linspace(0.0, 1.0, L)
    bands = np.linspace(1e-4, 15, 16)
    ang = (2 * np.pi / L) * pos[:, None] * bands[None, :]
    feats = np.concatenate([t[:, None], np.cos(ang), -np.sin(ang)], -1)
    idx2 = np.concatenate([[0], np.arange(L - 1, 0, -1)])
    f2 = np.concatenate([feats, feats[idx2]], 0)
    t2 = np.concatenate([t, t[idx2]])
    return np.ascontiguousarray(f2.T, dtype=np.float32), np.ascontiguousarray(t2[None], dtype=np.float32)


def build_program():
    nc = bass.Bass("TRN2", target_bir_lowering=False)
    di = lambda n, shp, dt=F32: nc.dram_tensor(n, list(shp), dt, kind="ExternalInput").ap()
    dn = lambda n, shp, dt=F32: nc.dram_tensor(n, list(shp), dt, kind="Internal").ap()
    T = LP + LS_
    a = {}
    a["x_seq"] = di("x_seq", [T, D])
    a["x_ext"] = di("x_ext", [NE_, D])
    a["kvalid"] = di("kvalid", [1, NE_], BF16)
    a["ones_row"] = di("ones_row", [1, NOWN_], BF16)
    a["w_h"] = di("w_h", [D, 2 * CHN_ALL])
    a["w_g2"] = di("w_g2", [D, CHN_ALL])
    a["wq"], a["wk"], a["wv"] = di("wq", [D, DA]), di("wk", [D, DA]), di("wv", [D, DA])
    a["gmix"] = di("gmix", [128, DK])
    a["scw_h"] = di("scw_h", [128, 20, 4])
    a["scw_g2"] = di("scw_g2", [128, 10, 4])
    a["ident"] = di("ident", [128, 128])
    a["gqk"] = di("gqk", [128, 2])
    a["bd"] = di("bd", [128, 128])
    a["relb"] = di("relb", [32, NH])
    a["sel"] = di("sel", [32, 2432])
    a["jm"] = di("jm", [128, 128])
    fdc = dict(w1=di("f_w1", [33, 64]), w2=di("f_w2", [64, 64]), w3=di("f_w3", [64, 64]), fvec=di("f_vec", [64, 4]),
               woutd=di("f_woutd", [64, 2, 2 * CHN_ALL]), ndelta=di("f_ndelta", [128, 20]))
    a["skip"] = di("skip", [1, 2 * CHN_ALL])
    fl = {}
    for L, tg in ((LP, "p"), (LS_, "s")):
        N1 = 2 * L // 128
        Hown = 32 if L == LP else 16
        cc = {k: di(f"c{tg}_{k}", v.shape) for k, v in fft_consts(L).items()}
        cc["F4own"] = di(f"c{tg}_F4own", [128, N1 // 128, 2, Hown])
        cc["SEL"] = di(f"c{tg}_SEL", [N1 // 2, Hown])
        fd = dict(fdc)
        fd["featsT2"] = di(f"f{tg}_feats", [33, 2 * L])
        fd["trow2"] = di(f"f{tg}_trow", [1, 2 * L])
        fl[L] = (cc, fd, Hown, N1)
    d3 = dict(w_out=di("w_out", [D, D]), gh=di("gh", [128, 10]), ga=di("ga", [128, 6]), gf=di("gf", [1, D]), wr=di("wr", [128, DK, 36]),
              rb=di("rb", [1, 36]), ident=a["ident"], w_gate=di("w_gate", [NEXP * D, DE]), w_up=di("w_up", [NEXP * D, DE]),
              w_down=di("w_down", [NEXP * DE, D]), utri=di("utri", [128, 128]), ltm=di("ltm", [1, 1024]), bpos=di("bpos", [128, 1]),
              offg=di("offg", [128, DK]))
    x_ext = a["x_ext"]
    d3["x_own_fn"] = lambda r0: x_ext[(1024 + r0 if r0 < 4096 else r0 + 3072):(1024 + r0 if r0 < 4096 else r0 + 3072) + 128, :]
    y_out = nc.dram_tensor("y_own", [NOWN_, D], F32, kind="ExternalOutput").ap()
    UH = dn("UH", [2 * CHN_ALL, 1 + T], BF16)
    G2 = dn("G2", [CHN_ALL, NOWN_], BF16)
    YHo = dn("YHo", [CHN_ALL, NOWN_], BF16)
    KT, QT, VE = dn("KT", [NH, HD + 1, NE_], BF16), dn("QT", [NH, HD + 1, NOWN_], BF16), dn("VE", [NE_, NH * (HD + 1)], BF16)
    EV, EBD, YA = dn("EV", [NH, 2432], BF16), dn("EBD", [NH, 128, KB * 128], BF16), dn("YA", [NOWN_, DA])
    X1, H2B = dn("X1", [NOWN_, D]), dn("H2B", [NOWN_, D], BF16)
    AAd, GTd = dn("AAd", [128, NOWN_ // 128, 64]), dn("GTd", [128, NOWN_ // 128, 2])
    XB, YB = dn("XB", [NSLOT, D], BF16), dn("YB", [NSLOT, D])
    P = Prog(nc)
    cfg = dict(NE=NE_, NOWN=NOWN_, NBLK=NBLK, ones_row=a["ones_row"],
               own_tiles={**{2 + i: i for i in range(8)}, **{14 + i: 8 + i for i in range(4)}},
               pieces=[(0, 6144, 0, 4096), (6144, 4096, 4096, 2048)])
    P.mark("start")
    phase_h(P, nc, [(0, LP, 0, LP, 1), (LP, LS_, LP, T, 1 + LP)], 2 * CHN_ALL, a["x_seq"], a["w_h"], a["gmix"], a["scw_h"], a["ident"], UH, tag="h")
    P.mark("phase_h")
    phase_h(P, nc, [(512, 5120, 1024, 5120, 0), (6656, 3072, 7168, 9216, 4096)], CHN_ALL, a["x_ext"], a["w_g2"], a["gmix"], a["scw_g2"], a["ident"], G2, tag="g")
    P.mark("g2")
    for L, tg, s0, own_off in ((LP, "p", 0, 0), (LS_, "s", LP, 4096)):
        cc, fd, Hown, N1 = fl[L]
        FILT = dn(f"FILT{tg}", [2 * CHN_ALL, 2 * L], BF16)
        GS = [dn(f"GS{tg}{o}", [128, CHN_ALL, 2 * N1], BF16) for o in range(2)]
        hyena_filters(P, nc, L, fd, FILT, CHN_ALL, tg)
        P.mark("filt" + tg)
        hyena_conv(P, nc, L, s0, cc, FILT, GS, UH, a["skip"], G2, own_off, Hown, YHo, CHN_ALL, tg)
        P.mark("conv" + tg)
    phase_q(P, nc, cfg, a["x_ext"], a["wq"], a["wk"], a["wv"], a["gmix"], a["gqk"], a["kvalid"], a["ident"], a["bd"], KT, QT, VE)
    P.mark("phase_q")
    phase_attn(P, nc, cfg, a["relb"], a["sel"], a["jm"], KT, QT, VE, EV, EBD, YA)
    P.mark("attn")
    phase3(P, nc, cfg, d3, YHo, YA, X1, H2B, AAd, GTd)
    P.mark("phase3")
    phase_moe(P, nc, cfg, d3, X1, H2B, AAd, GTd, XB, YB, y_out)
    P.mark("moe")
    P.finish()
    return nc, P


def host_inputs(inp):
    import ml_dtypes
    bf = ml_dtypes.bfloat16
    f32 = np.float32
    g = lambda k: np.asarray(inp[k])
    xp, xs = g("x_prompt"), g("x_sample")
    w_in = g("w_in")[0]
    scwv = np.concatenate([g("sconv_w")[0], g("sconv_b")[0][None]], 0).T
    arr = lambda v, n: np.ascontiguousarray(v.reshape(n, 128, -1).transpose(1, 0, 2))
    col = lambda v: np.ascontiguousarray(v.reshape(-1, 128).T)
    shared = {}
    shared["w_h"] = np.ascontiguousarray(w_in[:, 0:2560])
    shared["w_g2"] = np.ascontiguousarray(w_in[:, 2560:3840])
    shared["wq"] = np.ascontiguousarray(w_in[:, 3840:4608])
    shared["wk"] = np.ascontiguousarray(w_in[:, 4608:5376])
    shared["wv"] = np.ascontiguousarray(w_in[:, 5376:6144])
    shared["gmix"] = col(g("mix_norm_g")[0])
    shared["scw_h"] = arr(scwv[0:2560], 20)
    shared["scw_g2"] = arr(scwv[2560:3840], 10)
    shared["ident"] = np.eye(128, dtype=f32)
    shared["gqk"] = np.ascontiguousarray(np.stack([np.tile(g("q_norm_g")[0], 2), np.tile(g("k_norm_g")[0], 2)], 1))
    shared["bd"] = np.kron(np.eye(2), np.ones((64, 64))).astype(f32)
    shared["relb"] = g("rel_bias")
    shared["sel"] = attn_sel_table()
    shared["jm"] = np.ascontiguousarray(np.eye(128, dtype=f32)[::-1])
    shared["ones_row"] = np.ones((1, NOWN_), f32).astype(bf)
    shared["f_w1"], shared["f_w2"], shared["f_w3"] = g("filt_w1")[0], g("filt_w2")[0], g("filt_w3")[0]
    shared["f_vec"] = np.ascontiguousarray(np.stack([g("filt_freq")[0], g("filt_b1")[0], g("filt_b2")[0], g("filt_b3")[0]], 1))
    shared["f_woutd"] = np.ascontiguousarray(g("filt_w_out")[0].reshape(64, 2, 2, CHN_ALL).transpose(0, 2, 1, 3).reshape(64, 2, 2 * CHN_ALL))
    delta = np.abs(np.linspace(MIN_DECAY_, MAX_DECAY_, CHN_ALL)).astype(f32)
    shared["f_ndelta"] = col(-np.tile(delta, 2))
    shared["skip"] = np.ascontiguousarray(g("hyena_skip")[0].reshape(1, -1))
    fcon = {}
    for L, tg in ((LP, "p"), (LS_, "s")):
        fc = fft_consts(L)
        fcon[L] = fc
        for k, v in fc.items():
            shared[f"c{tg}_{k}"] = v
        shared[f"f{tg}_feats"], shared[f"f{tg}_trow"] = _circle_tables(L)
    shared["w_out"] = g("w_out")[0]
    shared["gh"] = col(g("out_norm_h")[0])
    shared["ga"] = col(g("out_norm_a")[0])
    shared["gf"] = np.ascontiguousarray(g("ffn_norm_g")[0][None])
    wcat = np.concatenate([g("group_router_w")[0], g("expert_router_w")[0].transpose(1, 0, 2).reshape(D, 32)], 1)
    shared["wr"] = arr(wcat, DK)
    shared["rb"] = np.ascontiguousarray(np.concatenate([g("group_router_b")[0], g("expert_router_b")[0].reshape(-1)])[None])
    shared["w_gate"] = g("w_gate")[0].reshape(NEXP * D, DE)
    shared["w_up"] = g("w_up")[0].reshape(NEXP * D, DE)
    shared["w_down"] = g("w_down")[0].reshape(NEXP * DE, D)
    shared["utri"] = np.triu(np.ones((128, 128), f32), 1)
    shared["ltm"] = np.tril(np.ones((32, 32), f32)).reshape(1, -1)
    shared["bpos"] = (np.arange(128) * float(BLK)).astype(f32)[:, None]
    shared["offg"] = (np.arange(DK)[None, :] * 128 + np.arange(128)[:, None]).astype(f32)
    maps = []
    for c in range(8):
        b, hf = c // 2, c % 2
        m = dict(shared)
        m["x_seq"] = np.concatenate([xp[b], xs[0]], 0)
        xe = np.zeros((NE_, D), f32)
        kv = np.full((1, NE_), NEGV, f32)
        for (src, Lq, lo, n, e0) in ((xp[b], LP, 4096 * hf - 1024, 6144, 0), (xs[0], LS_, 2048 * c - 1024, 4096, 6144)):
            a0, a1 = max(lo, 0), min(lo + n, Lq)
            xe[e0 + a0 - lo:e0 + a1 - lo] = src[a0:a1]
            kv[0, e0 + a0 - lo:e0 + a1 - lo] = 0.0
        m["x_ext"] = xe
        m["kvalid"] = kv.astype(bf)
        for L, tg, Hown, r0 in ((LP, "p", 32, 32 * hf), (LS_, "s", 16, 16 * c)):
            H = L // 128
            sel = np.zeros((H, Hown), f32)
            sel[r0 + np.arange(Hown), np.arange(Hown)] = 1.0
            m[f"c{tg}_SEL"] = sel
            m[f"c{tg}_F4own"] = np.ascontiguousarray(fcon[L]["F4"][:, :, :, r0:r0 + Hown])
        maps.append(m)
    return maps


_PROG = None


def kernel(**inputs):
    global _PROG
    if _PROG is None:
        _PROG = build_program()[0]
    maps = host_inputs(inputs)
    res = run_bass_kernel_spmd(_PROG, maps, core_ids=list(range(8)))
    yp = np.zeros((4, LP, D), np.float32)
    ys = np.zeros((1, LS_, D), np.float32)
    for c in range(8):
        yo = np.asarray(res.results[c]["y_own"])
        yp[c // 2, 4096 * (c % 2):4096 * (c % 2) + 4096] = yo[0:4096]
        ys[0, 2048 * c:2048 * c + 2048] = yo[4096:6144]
    return yp, ys
```

```python
import numpy as np
import concourse.bass as bass
import concourse.mybir as mybir
from concourse.bass_utils import run_bass_kernel_spmd

F32 = mybir.dt.float32
BF16 = mybir.dt.bfloat16
I32 = mybir.dt.int32
U32 = mybir.dt.uint32
ALU = mybir.AluOpType
AF = mybir.ActivationFunctionType
AX = mybir.AxisListType


def _key(item):
    if isinstance(item, (tuple, str)):
        return item
    t = getattr(item, "tensor", None)
    if t is not None:
        return t.name
    return item.name


class Prog:
    NDMA = 6

    def __init__(self, nc):
        self.nc = nc
        self.E = {"pe": nc.tensor, "dve": nc.vector, "act": nc.scalar, "pool": nc.gpsimd, "sp": nc.sync}
        self.sem = {e: nc.alloc_semaphore(name=f"c_{e}") for e in ("pe", "dve", "act", "pool")}
        self.cnt = {e: 0 for e in self.sem}
        self.known = {e: {} for e in self.E}
        self.res = {}
        self.dsem = {q: [nc.alloc_semaphore(name=f"d_{q}{i}") for i in range(self.NDMA)] for q in ("sp", "act", "pool")}
        self.dcnt = {q: [0] * self.NDMA for q in self.dsem}
        self.drr = {q: 0 for q in self.dsem}
        self.nops = 0

    def _wait(self, eng, sem, val):
        k = self.known[eng]
        name = sem.name if hasattr(sem, "name") else id(sem)
        if k.get(name, 0) >= val:
            return
        self.E[eng].wait_ge(sem, val)
        k[name] = val

    def _deps(self, eng, reads, writes):
        for r in reads:
            st = self.res.get(_key(r))
            if st is None:
                continue
            w = st["w"]
            if w is not None and not (w[2] == "pe" and eng == "pe"):
                self._wait(eng, w[0], w[1])
        for wr in writes:
            st = self.res.get(_key(wr))
            if st is None:
                continue
            w = st["w"]
            if w is not None and not (w[2] == "pe" and eng == "pe"):
                self._wait(eng, w[0], w[1])
            for (s, v, e2) in st["r"].values():
                if e2 == "pe" and eng == "pe":
                    continue
                self._wait(eng, s, v)

    def _commit(self, tok, reads, writes):
        for r in reads:
            st = self.res.setdefault(_key(r), {"w": None, "r": {}})
            nm = tok[0].name
            st["r"][nm] = tok
        for wr in writes:
            self.res[_key(wr)] = {"w": tok, "r": {}}

    def op(self, eng, fn, reads=(), writes=()):
        self._deps(eng, reads, writes)
        inst = fn(self.E[eng])
        self.cnt[eng] += 1
        inst.then_inc(self.sem[eng], 1)
        tok = (self.sem[eng], self.cnt[eng], eng)
        self._commit(tok, reads, writes)
        self.nops += 1
        return inst

    def dma(self, q, out, in_, reads=None, writes=None, fn=None, **kw):
        if reads is None:
            reads = [in_]
        if writes is None:
            writes = [out]
        i = self.drr[q]
        self.drr[q] = (i + 1) % self.NDMA
        s = self.dsem[q][i]
        if self.dcnt[q][i] > 0:
            self._wait(q, s, 16 * self.dcnt[q][i])
        self._deps(q, reads, writes)
        if fn is None:
            inst = self.E[q].dma_start(out=out, in_=in_, **kw)
        else:
            inst = fn(self.E[q])
        inst.then_inc(s, 16)
        self.dcnt[q][i] += 1
        tok = (s, 16 * self.dcnt[q][i], "dma")
        self._commit(tok, reads, writes)
        self.nops += 1
        return inst

    def mark(self, name):
        if not hasattr(self, 'marks'):
            self.marks = []
        self.marks.append((name, dict(self.cnt)))

    def barrier(self):
        for eng in self.E:
            for q in self.dsem:
                for i, sm in enumerate(self.dsem[q]):
                    if self.dcnt[q][i]:
                        self._wait(eng, sm, 16 * self.dcnt[q][i])
            for e, sm in self.sem.items():
                if self.cnt[e] and e != eng:
                    self._wait(eng, sm, self.cnt[e])
        self.res = {}

    def finish(self):
        for q in self.dsem:
            for i, s in enumerate(self.dsem[q]):
                if self.dcnt[q][i]:
                    self._wait("sp", s, 16 * self.dcnt[q][i])
        for e, s in self.sem.items():
            if self.cnt[e]:
                self._wait("sp", s, self.cnt[e])


D = 2048
DK = D // 128
EPS = 1e-6


def bcast_rows(ap_row, nparts):
    t = ap_row.tensor
    n = ap_row.shape[-1]
    return bass.AP(tensor=t, offset=ap_row.offset, ap=[[0, nparts], [1, n]])


class XT:
    def __init__(self, P, nc, es, tag):
        self.P, self.nc = P, nc
        self.xin = [es.enter_context(nc.sbuf_tensor(f"xin{tag}0", [128, 4, D], F32))] * 2
        self.xbf = [es.enter_context(nc.sbuf_tensor(f"xbf{tag}{i}", [128, 4, D], BF16)) for i in range(2)]
        self.xT = [es.enter_context(nc.sbuf_tensor(f"xT{tag}{i}", [128, DK, 512], BF16)) for i in range(2)]
        self.pt = [es.enter_context(nc.psum_tensor(f"ptr{tag}{i}", [128, 2, 512], BF16)) for i in range(2)]
        self.ident = es.enter_context(nc.sbuf_tensor(f"ident{tag}", [128, 128], BF16))
        self.n = 0

    def setup(self, ident_dram):
        P = self.P
        tmp = self.xin[0]
        P.dma("sp", tmp[:, 0, 0:128], ident_dram)
        P.op("dve", lambda e: e.tensor_copy(out=self.ident[:], in_=tmp[:, 0, 0:128]), [tmp], [self.ident])
        self.identf = None

    def load(self, x_rows_ap):
        P = self.P
        i = self.n % 2
        self.n += 1
        xin, xbf, xT = self.xin[i], self.xbf[i], self.xT[i]
        src = x_rows_ap.rearrange("(s p) d -> p s d", p=128)
        P.dma("sp", xin[:], src, [x_rows_ap], [xin])
        for s in range(4):
            eng = "act" if s % 2 == 0 else "pool"
            if eng == "act":
                P.op("act", lambda e, s=s: e.copy(out=xbf[:, s, :], in_=xin[:, s, :]), [xin], [(xbf.name, s)])
            else:
                P.op("pool", lambda e, s=s: e.tensor_copy(out=xbf[:, s, :], in_=xin[:, s, :]), [xin], [(xbf.name, s)])
        for jj in range(DK // 2):
            pt = self.pt[jj % 2]
            for j2 in range(2):
                j = jj * 2 + j2
                for s in range(4):
                    P.op("pe", lambda e, s=s, j=j, j2=j2, pt=pt: e.transpose(
                        out=pt[:, j2, s * 128:(s + 1) * 128], in_=xbf[:, s, j * 128:(j + 1) * 128], identity=self.ident[:]),
                        [(xbf.name, s), self.ident], [pt])
            eng = "dve" if jj % 2 == 0 else "act"
            if eng == "dve":
                P.op("dve", lambda e, jj=jj, pt=pt: e.tensor_copy(out=xT[:, 2 * jj:2 * jj + 2, :], in_=pt[:, :, :]), [pt], [(xT.name, jj)])
            else:
                P.op("act", lambda e, jj=jj, pt=pt: e.copy(out=xT[:, 2 * jj:2 * jj + 2, :], in_=pt[:, :, :]), [pt], [(xT.name, jj)])
        return xin, xbf, xT


import math
PI = math.pi


class ShortConv:
    def __init__(self, P, nc, es, nchunks, RC, tag):
        self.P, self.RC, self.nchunks = P, RC, nchunks
        sb = lambda n, shp, dt=F32: es.enter_context(nc.sbuf_tensor(f"{n}{tag}", shp, dt))
        self.carry = sb("carry", [RC, nchunks, 2])
        self.U = [sb(f"U{i}", [RC, 516]) for i in range(2)]
        self.ta = [sb(f"ta{i}", [RC, 512]) for i in range(2)]
        self.tb = [sb(f"tb{i}", [RC, 512]) for i in range(2)]
        self.ob = [sb(f"ob{i}", [RC, 512], BF16) for i in range(2)]
        self.n = 0

    def reset(self):
        self.P.op("pool", lambda e: e.memset(self.carry[:], 0.0), [], [(self.carry.name, m) for m in range(self.nchunks)])

    def step(self, m, w, fill, emit):
        P, RC = self.P, self.RC
        k = self.n % 2
        self.n += 1
        U, ta, tb, ob = self.U[k], self.ta[k], self.tb[k], self.ob[k]
        P.op("pool", lambda e: e.tensor_copy(out=U[:, 0:2], in_=self.carry[:, m, :]), [(self.carry.name, m)], [U])
        fill(U)
        P.op("pool", lambda e: e.tensor_copy(out=self.carry[:, m, :], in_=U[:, 512:514]), [U], [(self.carry.name, m)])
        if emit is None:
            return
        lo, hi = emit[1], emit[2]
        n = hi - lo
        P.op("act", lambda e: e.activation(out=ta[:, 0:n], in_=U[:, 1 + lo:1 + hi], func=AF.Identity, bias=w[:, m, 3:4], scale=w[:, m, 1:2]), [U, w], [ta])
        P.op("dve", lambda e: e.scalar_tensor_tensor(out=tb[:, 0:n], in0=U[:, lo:hi], scalar=w[:, m, 0:1], in1=ta[:, 0:n], op0=ALU.mult, op1=ALU.add), [U, w, ta], [tb])
        P.op("dve", lambda e: e.scalar_tensor_tensor(out=ob[:, lo:hi], in0=U[:, 2 + lo:2 + hi], scalar=w[:, m, 2:3], in1=tb[:, 0:n], op0=ALU.mult, op1=ALU.add), [U, w, tb], [ob])
        emit[0](ob)

    def flush(self, m, w, emit):
        P = self.P
        k = self.n % 2
        self.n += 1
        ta, ob = self.ta[k], self.ob[k]
        c = self.carry
        P.op("dve", lambda e: e.tensor_scalar(out=ta[:, 0:1], in0=c[:, m, 1:2], scalar1=w[:, m, 1:2], scalar2=w[:, m, 3:4], op0=ALU.mult, op1=ALU.add),
             [(c.name, m), w], [ta])
        P.op("dve", lambda e: e.scalar_tensor_tensor(out=ob[:, 0:1], in0=c[:, m, 0:1], scalar=w[:, m, 0:1], in1=ta[:, 0:1], op0=ALU.mult, op1=ALU.add),
             [(c.name, m), w, ta], [ob])
        emit(ob)


def phase_h(P, nc, seqs, NCOL, x_seq, w_h, gmix, scw, ident_d, UH, tag="h"):
    import contextlib
    RC = min(128, NCOL)
    NCH = NCOL // RC
    with contextlib.ExitStack() as es:
        sb = lambda n, shp, dt=F32: es.enter_context(nc.sbuf_tensor(n, shp, dt))
        xt = XT(P, nc, es, tag)
        xt.setup(ident_d)
        Wh = sb(f"Wh{tag}", [128, DK, NCOL], BF16)
        g_sb, scw_sb, epsT = sb(f"g_sb{tag}", [128, DK]), sb(f"scw_sb{tag}", [RC, NCH, 4]), sb(f"epsT{tag}", [128, 1])
        identf = sb(f"identf_h{tag}", [128, 128])
        ssq, e2c = [sb(f"ssq{tag}{i}", [128, 4]) for i in range(2)], [sb(f"rc{tag}{i}", [128, 4]) for i in range(2)]
        rbc = [sb(f"rbc{tag}{i}", [128, 4, 128]) for i in range(2)]
        rstd = [sb(f"rstd{tag}{i}", [128, 512]) for i in range(2)]
        junk = sb(f"junkh{tag}", [128, D], BF16)
        sc = ShortConv(P, nc, es, NCH, RC, tag)
        pss = es.enter_context(nc.psum_tensor(f"pss{tag}", [128, 512], F32))
        pu = [es.enter_context(nc.psum_tensor(f"pu{tag}{i}", [128, 512], F32)) for i in range(3)]
        P.op("pool", lambda e: e.memset(epsT[:], EPS), [], [epsT])
        for i in range(2):
            P.op("pool", lambda e, i=i: e.memset(ssq[i][:], 0.0), [], [ssq[i]])
        P.dma("sp", g_sb[:], gmix)
        P.dma("sp", scw_sb[:], scw)
        P.dma("sp", identf[:], ident_d)
        stage = xt.xin[0]
        for j in range(DK):
            for c0 in range(0, NCOL, 2048):
                cn = min(2048, NCOL - c0)
                P.dma("sp", stage[:, 0, 0:cn], w_h[j * 128:(j + 1) * 128, c0:c0 + cn], None, [stage])
                P.op("dve", lambda e, j=j, c0=c0, cn=cn: e.tensor_scalar(out=Wh[:, j, c0:c0 + cn], in0=stage[:, 0, 0:cn], scalar1=g_sb[:, j:j + 1],
                                                                       scalar2=None, op0=ALU.mult), [stage, g_sb], [Wh])
        ntile = 0
        for (s0, L, elo, ehi, oc0) in seqs:
            sc.reset()
            nt = L // 512
            for it in range(nt):
                t0 = s0 + it * 512
                lo_i = max(0, elo - (t0 - 1))
                hi_i = min(512, ehi - (t0 - 1))
                xin, xbf, xT = xt.load(x_seq[t0:t0 + 512, :])
                b = ntile % 2
                ntile += 1
                for s in range(4):
                    P.op("act", lambda e, s=s: e.activation(out=junk[:], in_=xin[:, s, :], func=AF.Square, accum_out=ssq[b][:, s:s + 1]), [xin], [junk, ssq[b]])
                P.op("act", lambda e: e.activation(out=e2c[b][:], in_=ssq[b][:], func=AF.Sqrt, bias=epsT[:], scale=1.0 / D), [ssq[b], epsT], [e2c[b]])
                P.op("dve", lambda e: e.reciprocal(out=e2c[b][:], in_=e2c[b][:]), [e2c[b]], [e2c[b]])
                P.op("pool", lambda e: e.memset(ssq[b][:], 0.0), [], [ssq[b]])
                P.op("dve", lambda e: e.tensor_copy(out=rbc[b][:], in_=bc_last(e2c[b][:, 0:4], 128)), [e2c[b]], [rbc[b]])
                for s4 in range(4):
                    P.op("pe", lambda e, s4=s4: e.matmul(pss[:, s4 * 128:(s4 + 1) * 128], lhsT=rbc[b][:, s4, :], rhs=identf[:], start=True, stop=True),
                         [rbc[b], identf], [pss])
                P.op("act", lambda e: e.copy(out=rstd[b][:], in_=pss[:]), [pss], [rstd[b]])
                for m in range(NCH):
                    pum = pu[m % 3]
                    for j in range(DK):
                        P.op("pe", lambda e, j=j, m=m, pum=pum: e.matmul(pum[0:RC, :], lhsT=Wh[:, j, m * RC:(m + 1) * RC], rhs=xT[:, j, :],
                                                                         start=(j == 0), stop=(j == DK - 1)), [Wh, (xT.name, j // 2)], [pum])

                    def fill(U, pum=pum):
                        P.op("dve", lambda e: e.tensor_tensor(out=U[:, 2:514], in0=pum[0:RC, :], in1=rstd[b][0:RC, :], op=ALU.mult), [pum, rstd[b]], [U])

                    def emit(ob, m=m, t0=t0, lo_i=lo_i, hi_i=hi_i):
                        c0 = oc0 + (t0 - 1 + lo_i) - elo
                        P.dma("act", UH[m * RC:(m + 1) * RC, c0:c0 + hi_i - lo_i], ob[:, lo_i:hi_i], [ob], [(UH.tensor.name, m)],
                              allow_slow_non_contiguous=(hi_i - lo_i == 1))
                    sc.step(m, scw_sb, fill, (emit, lo_i, hi_i) if hi_i > lo_i else None)
                    if it == nt - 1 and ehi == s0 + L:
                        def emit2(ob, m=m):
                            c0 = oc0 + (ehi - 1) - elo
                            P.dma("act", UH[m * RC:(m + 1) * RC, c0:c0 + 1], ob[:, 0:1], [ob], [(UH.tensor.name, m)], allow_slow_non_contiguous=True)
                        sc.flush(m, scw_sb, emit2)
        P.barrier()


def flat2(ap):
    nd = len(ap.shape)
    if nd == 2:
        return ap
    names = "abcdef"[:nd - 1]
    return ap.rearrange("p " + " ".join(names) + " -> p (" + " ".join(names) + ")")


def bc_col(ap, n):
    a = [list(x) for x in ap.ap]
    return bass.AP(tensor=ap.tensor, offset=ap.offset, ap=[a[0], [0, n]])


def bc_mid(ap, n):
    a = [list(x) for x in ap.ap]
    return bass.AP(tensor=ap.tensor, offset=ap.offset, ap=[a[0], [0, n]] + a[1:])


def bc_last(ap, n):
    a = [list(x) for x in ap.ap]
    return bass.AP(tensor=ap.tensor, offset=ap.offset, ap=a + [[0, n]])


def fft_consts(L):
    N = 2 * L
    N1 = N // 128
    H = N1 // 2
    c = {}
    n1 = np.arange(N1)[:, None]
    k1 = np.arange(N1)[None, :]
    th = 2 * np.pi * n1 * k1 / N1
    f1 = np.concatenate([np.cos(th), -np.sin(th)], 1)
    c["F1"] = f1.reshape(N1 // 128, 128, 2 * N1).transpose(1, 0, 2)
    n2 = np.arange(128)[:, None]
    th = 2 * np.pi * n2 * k1 / N
    c["TW1"] = np.stack([np.cos(th), -np.sin(th)], 1)
    k2 = np.arange(128)[None, :]
    th = 2 * np.pi * n2 * k2 / 128
    c["F2"] = np.stack([np.cos(th), -np.sin(th), np.sin(th)], 1)
    th = 2 * np.pi * np.arange(128)[:, None] * np.arange(128)[None, :] / 128
    c["F3"] = np.stack([np.concatenate([np.cos(th), np.sin(th)], 1),
                        np.concatenate([-np.sin(th), np.cos(th)], 1)], 1)
    k1c = np.arange(N1)[:, None]
    n1p = np.arange(128)[None, :]
    th = 2 * np.pi * k1c * n1p / N
    tw2 = np.stack([np.cos(th), np.sin(th)], 1)
    c["TW2"] = tw2.reshape(N1 // 128, 128, 2, 128).transpose(1, 0, 2, 3)
    n2p = np.arange(H)[None, :]
    th = 2 * np.pi * k1c * n2p / N1
    f4 = np.stack([np.cos(th) / N, -np.sin(th) / N], 1)
    c["F4"] = f4.reshape(N1 // 128, 128, 2, H).transpose(1, 0, 2, 3)
    return {k: np.ascontiguousarray(v, dtype=np.float32) for k, v in c.items()}


class FFT:
    def __init__(self, P, nc, es, L, cd, tag, Hown=None):
        self.P, self.nc, self.L = P, nc, L
        N1 = self.N1 = 2 * L // 128
        H = self.H = N1 // 2
        self.nch = N1 // 128
        self.Cb = 512 // N1
        Cb = self.Cb
        sb = lambda n, shp, dt: es.enter_context(nc.sbuf_tensor(f"{n}{tag}", shp, dt))
        self.F1 = sb("F1", [128, self.nch, 2 * N1], BF16)
        self.TW1 = sb("TW1", [128, 2, N1], F32)
        self.F2 = sb("F2", [128, 3, 128], BF16)
        self.F3 = sb("F3", [128, 2, 256], BF16)
        self.TW2 = sb("TW2", [128, self.nch, 2, 128], F32)
        self.F4 = sb("F4", [128, self.nch, 2, H], BF16)
        lst = [("F1", self.F1), ("F2", self.F2), ("F3", self.F3), ("F4", self.F4)]
        if Hown is not None:
            self.Hown = Hown
            self.F4o = sb("F4o", [128, self.nch, 2, Hown], BF16)
            self.SEL = sb("SELo", [H, Hown], BF16)
            lst += [("F4own", self.F4o), ("SEL", self.SEL)]
        stg = sb("fstage", [128, 1024], F32)
        for nm, dst in lst:
            n = int(np.prod(dst.shape[1:]))
            p = dst.shape[0]
            dflat = flat2(dst[:])
            sflat = flat2(cd[nm])
            for c0 in range(0, n, 1024):
                cn = min(1024, n - c0)
                P.dma("sp", stg[0:p, 0:cn], sflat[:, c0:c0 + cn], None, [stg])
                P.op("dve", lambda e, dflat=dflat, p=p, c0=c0, cn=cn: e.tensor_copy(out=dflat[:, c0:c0 + cn], in_=stg[0:p, 0:cn]), [stg], [dst])
        P.dma("sp", self.TW1[:], cd["TW1"])
        P.dma("sp", self.TW2[:], cd["TW2"])
        self.t1 = [sb(f"ft1_{i}", [128, 1024], F32) for i in range(2)]
        self.t2 = [sb(f"ft2_{i}", [128, 1024], F32) for i in range(2)]
        self.Ar = [sb(f"Ar{i}", [128, Cb, N1], BF16) for i in range(2)]
        self.Ai = [sb(f"Ai{i}", [128, Cb, N1], BF16) for i in range(2)]
        self.Yr = [sb(f"Yr{i}", [128, Cb, N1], BF16) for i in range(2)]
        self.Yi = [sb(f"Yi{i}", [128, Cb, N1], BF16) for i in range(2)]
        self.Cr = [sb(f"Cr{i}", [128, Cb, self.nch, 128], BF16) for i in range(2)]
        self.Ci = [sb(f"Ci{i}", [128, Cb, self.nch, 128], BF16) for i in range(2)]
        self.g = 0

    def fwd(self, zl, zkey, pa, pb, par):
        P, N1, Cb = self.P, self.N1, self.Cb
        i = par
        t1, t2, Ar, Ai = self.t1[i], self.t2[i], self.Ar[i], self.Ai[i]
        for c in range(Cb):
            for n, (kc, z) in enumerate(zl):
                K = z.shape[0]
                P.op("pe", lambda e, c=c, n=n, kc=kc, z=z, K=K: e.matmul(pa[:, c * 2 * N1:(c + 1) * 2 * N1], lhsT=z[:, c, :], rhs=self.F1[0:K, kc, :],
                                                                        start=(n == 0), stop=(n == len(zl) - 1)), [zkey, self.F1], [pa])
        yield
        pav = pa[:].rearrange("p (c r k) -> p c r k", c=Cb, r=2)
        t1v = t1[:].rearrange("p (c r k) -> p c r k", c=Cb, r=2)
        t2v = t2[:].rearrange("p (c r k) -> p c r k", c=Cb, r=2)
        twr = bc_mid(bc_mid(self.TW1[:, 0, :], 2), Cb)
        twi = bc_mid(bc_mid(self.TW1[:, 1, :], 2), Cb)
        P.op("dve", lambda e: e.tensor_tensor(out=t1v, in0=pav, in1=twr, op=ALU.mult), [pa, self.TW1], [t1])
        P.op("dve", lambda e: e.tensor_tensor(out=t2v, in0=pav, in1=twi, op=ALU.mult), [pa, self.TW1], [t2])
        P.op("dve", lambda e: e.tensor_tensor(out=Ar[:], in0=t1v[:, :, 0, :], in1=t2v[:, :, 1, :], op=ALU.subtract), [t1, t2], [Ar])
        P.op("dve", lambda e: e.tensor_tensor(out=Ai[:], in0=t2v[:, :, 0, :], in1=t1v[:, :, 1, :], op=ALU.add), [t1, t2], [Ai])
        yield
        Arf = Ar[:].rearrange("p c k -> p (c k)")
        Aif = Ai[:].rearrange("p c k -> p (c k)")
        F2 = self.F2
        P.op("pe", lambda e: e.matmul(pb[:, 0:512], lhsT=F2[:, 0, :], rhs=Arf, start=True, stop=False), [F2, Ar], [pb])
        P.op("pe", lambda e: e.matmul(pb[:, 0:512], lhsT=F2[:, 2, :], rhs=Aif, start=False, stop=True), [F2, Ai], [pb])
        P.op("pe", lambda e: e.matmul(pb[:, 512:1024], lhsT=F2[:, 0, :], rhs=Aif, start=True, stop=False), [F2, Ai], [pb])
        P.op("pe", lambda e: e.matmul(pb[:, 512:1024], lhsT=F2[:, 1, :], rhs=Arf, start=False, stop=True), [F2, Ar], [pb])
        yield

    def inv(self, G, pa, pb, par, own=False, extra=None):
        P, N1, Cb, nch, H = self.P, self.N1, self.Cb, self.nch, self.H
        i = par
        t1, t2, Yr, Yi, Cr, Ci = self.t1[i], self.t2[i], self.Yr[i], self.Yi[i], self.Cr[i], self.Ci[i]
        Xr = pb[:, 0:512].rearrange("p (c k) -> p c k", c=Cb)
        Xi = pb[:, 512:1024].rearrange("p (c k) -> p c k", c=Cb)
        Gr, Gi = G[:, :, 0:N1], G[:, :, N1:2 * N1]
        q = lambda t, j: t[:, j * 512:(j + 1) * 512].rearrange("p (c k) -> p c k", c=Cb)
        P.op("dve", lambda e: e.tensor_tensor(out=q(t1, 0), in0=Xr, in1=Gr, op=ALU.mult), [pb, G], [t1])
        P.op("dve", lambda e: e.tensor_tensor(out=q(t1, 1), in0=Xi, in1=Gi, op=ALU.mult), [pb, G], [t1])
        P.op("dve", lambda e: e.tensor_tensor(out=q(t2, 0), in0=Xr, in1=Gi, op=ALU.mult), [pb, G], [t2])
        P.op("dve", lambda e: e.tensor_tensor(out=q(t2, 1), in0=Xi, in1=Gr, op=ALU.mult), [pb, G], [t2])
        P.op("dve", lambda e: e.tensor_tensor(out=Yr[:], in0=q(t1, 0), in1=q(t1, 1), op=ALU.subtract), [t1], [Yr])
        P.op("dve", lambda e: e.tensor_tensor(out=Yi[:], in0=q(t2, 0), in1=q(t2, 1), op=ALU.add), [t2], [Yi])
        yield
        F3 = self.F3
        for c in range(Cb):
            for ch in range(nch):
                o = (c * nch + ch) * 256
                P.op("pe", lambda e, c=c, ch=ch, o=o: e.matmul(pa[:, o:o + 256], lhsT=Yr[:, c, ch * 128:(ch + 1) * 128], rhs=F3[:, 0, :], start=True, stop=False),
                     [Yr, F3], [pa])
                P.op("pe", lambda e, c=c, ch=ch, o=o: e.matmul(pa[:, o:o + 256], lhsT=Yi[:, c, ch * 128:(ch + 1) * 128], rhs=F3[:, 1, :], start=False, stop=True),
                     [Yi, F3], [pa])
        yield
        pav = pa[:].rearrange("p (c h r k) -> p c h r k", c=Cb, h=nch, r=2)
        t1v = t1[:].rearrange("p (c h r k) -> p c h r k", c=Cb, h=nch, r=2)
        t2v = t2[:].rearrange("p (c h r k) -> p c h r k", c=Cb, h=nch, r=2)
        for ch in range(nch):
            twr = bc_mid(bc_mid(self.TW2[:, ch, 0, :], 2), Cb)
            twi = bc_mid(bc_mid(self.TW2[:, ch, 1, :], 2), Cb)
            P.op("dve", lambda e, ch=ch, twr=twr: e.tensor_tensor(out=t1v[:, :, ch], in0=pav[:, :, ch], in1=twr, op=ALU.mult), [pa, self.TW2], [t1])
            P.op("dve", lambda e, ch=ch, twi=twi: e.tensor_tensor(out=t2v[:, :, ch], in0=pav[:, :, ch], in1=twi, op=ALU.mult), [pa, self.TW2], [t2])
        P.op("dve", lambda e: e.tensor_tensor(out=Cr[:], in0=t1v[:, :, :, 0, :], in1=t2v[:, :, :, 1, :], op=ALU.subtract), [t1, t2], [Cr])
        P.op("dve", lambda e: e.tensor_tensor(out=Ci[:], in0=t2v[:, :, :, 0, :], in1=t1v[:, :, :, 1, :], op=ALU.add), [t1, t2], [Ci])
        yield
        F4 = self.F4o if own else self.F4
        Hout = self.Hown if own else H
        k = 0
        tot = 2 * nch + (1 if extra is not None else 0)
        outv = pb[0:Hout, 0:Cb * 128].rearrange("p (c k) -> p c k", c=Cb)
        for ch in range(nch):
            for r, Cx in ((0, Cr), (1, Ci)):
                P.op("pe", lambda e, ch=ch, r=r, Cx=Cx, k=k: e.matmul(outv, lhsT=F4[:, ch, r, :], rhs=Cx[:, :, ch, :],
                                                                      start=(k == 0), stop=(k == tot - 1)), [F4, Cx], [pb])
                k += 1
        if extra is not None:
            P.op("pe", lambda e: e.matmul(outv, lhsT=extra[0], rhs=extra[1], start=False, stop=True), list(extra[2]), [pb])
        yield


def hyena_filters(P, nc, L, fd, FILT, CHN, tag):
    import contextlib
    NR = 2 * CHN
    RC = min(128, NR)
    NCH = NR // RC
    nt = 2 * L // 512
    nth = nt // 2
    with contextlib.ExitStack() as es:
        sb = lambda n, shp, dt=F32: es.enter_context(nc.sbuf_tensor(f"{n}{tag}", shp, dt))
        w1s, w2s, w3s = sb("w1s", [33, 64]), sb("w2s", [64, 64]), sb("w3s", [64, 64])
        fv, fb, wouts, nd = sb("fv", [64, 4]), sb("fb", [64, 3]), sb("wouts", [64, 2, NR]), sb("nd", [RC, NCH])
        ft = [sb(f"ft{i}", [33, 512]) for i in range(2)]
        tbc = [sb(f"tbc{i}", [RC, 512]) for i in range(2)]
        arg, arg2 = sb("arg", [64, 512]), sb("arg2", [64, 512])
        aa = [sb(f"aa{i}", [64, 512], F32 if i < 2 else BF16) for i in range(3)]
        woutb = sb("woutb", [64, 2, NR], BF16)
        dec = [sb(f"dec{i}", [RC, 512]) for i in range(4)]
        gt = [sb(f"gt{i}", [RC, 512]) for i in range(4)]
        junk = sb("junk", [RC, 512])
        gob = [sb(f"gob{i}", [RC, 512], BF16) for i in range(4)]
        acc = sb("acc", [RC, NCH, nt])
        accs, inv = sb("accs", [RC, NCH]), sb("inv", [RC, NCH])
        pz = [es.enter_context(nc.psum_tensor(f"pz{tag}{i}", [64, 512], F32)) for i in range(2)]
        phr = [es.enter_context(nc.psum_tensor(f"phr{tag}{i}", [128, 512], F32)) for i in range(4)]
        for dst, src in ((w1s, fd["w1"]), (w2s, fd["w2"]), (w3s, fd["w3"]), (fv, fd["fvec"]), (wouts, fd["woutd"]), (nd, fd["ndelta"])):
            P.dma("sp", dst[:], src)
        P.op("pool", lambda e: e.memset(acc[:], 0.0), [], [acc])
        P.op("dve", lambda e: e.tensor_copy(out=woutb[:], in_=wouts[:]), [wouts], [woutb])
        P.op("dve", lambda e: e.tensor_scalar(out=fb[:], in0=fv[:, 1:4], scalar1=fv[:, 0:1], scalar2=None, op0=ALU.mult), [fv], [fb])
        ws = [w1s, w2s, w3s]
        cnt = [0]
        MAGIC = 12582912.0

        def mlp(it):
            k = cnt[0] % 2
            cnt[0] += 1
            P.dma("sp", ft[k][:], fd["featsT2"][:, it * 512:(it + 1) * 512])
            P.dma("sp", tbc[k][:], bcast_rows(fd["trow2"][:, it * 512:(it + 1) * 512], RC))
            src = ft[k]
            for l in range(3):
                p = pz[l % 2]
                P.op("pe", lambda e, l=l, p=p, src=src: e.matmul(p[:], lhsT=ws[l][:], rhs=src[:], start=True, stop=True), [ws[l], src], [p])
                P.op("dve", lambda e, l=l, p=p: e.tensor_scalar(out=arg[:], in0=p[:], scalar1=fv[:, 0:1], scalar2=fb[:, l:l + 1],
                                                                op0=ALU.mult, op1=ALU.add), [p, fv, fb], [arg])
                P.op("dve", lambda e: e.tensor_scalar(out=arg2[:], in0=arg[:], scalar1=1.0 / (2 * PI), scalar2=MAGIC, op0=ALU.mult, op1=ALU.add), [arg], [arg2])
                P.op("dve", lambda e: e.tensor_scalar(out=arg2[:], in0=arg2[:], scalar1=-MAGIC, scalar2=None, op0=ALU.add), [arg2], [arg2])
                P.op("dve", lambda e: e.scalar_tensor_tensor(out=arg[:], in0=arg2[:], scalar=-2 * PI, in1=arg[:], op0=ALU.mult, op1=ALU.add), [arg2, arg], [arg])
                P.op("dve", lambda e: e.tensor_scalar(out=arg[:], in0=arg[:], scalar1=-PI, scalar2=PI, op0=ALU.max, op1=ALU.min), [arg], [arg])
                P.op("act", lambda e, l=l: e.activation(out=aa[l][:], in_=arg[:], func=AF.Sin), [arg], [aa[l]])
                src = aa[l]
            return aa[2], tbc[k]

        def hr_chunk(a3, tb, r, it):
            j = r % 4
            p = phr[j]
            d = it // nth
            P.op("pe", lambda e: e.matmul(p[0:RC, :], lhsT=woutb[:, d, r * RC:(r + 1) * RC], rhs=a3[:], start=True, stop=True), [woutb, a3], [p])
            P.op("act", lambda e: e.activation(out=dec[j][:], in_=tb[:], func=AF.Exp, scale=nd[:, r:r + 1]), [tb, nd], [dec[j]])
            P.op("dve", lambda e: e.tensor_tensor(out=gt[j][:], in0=p[0:RC, :], in1=dec[j][:], op=ALU.mult), [p, dec[j]], [gt[j]])
            if it == nth:
                P.op("pool", lambda e: e.memset(gt[j][:, 0:1], 0.0), [], [gt[j]])
            return gt[j]

        for it in range(nt):
            a3, tb = mlp(it)
            for r in range(NCH):
                g = hr_chunk(a3, tb, r, it)
                P.op("act", lambda e, g=g, r=r, it=it: e.activation(out=junk[:], in_=g[:], func=AF.Abs, accum_out=acc[:, r, it:it + 1]), [g], [junk, acc])
        P.op("dve", lambda e: e.reduce_sum(out=accs[:], in_=acc[:], axis=AX.X), [acc], [accs])
        P.op("dve", lambda e: e.reciprocal(out=inv[:], in_=accs[:]), [accs], [inv])
        for it in range(nt):
            a3, tb = mlp(it)
            for r in range(NCH):
                g = hr_chunk(a3, tb, r, it)
                ob = gob[r % 4]
                P.op("act", lambda e, g=g, ob=ob, r=r: e.activation(out=ob[:], in_=g[:], func=AF.Identity, scale=inv[:, r:r + 1]), [g, inv], [ob])
                P.dma("act", FILT[r * RC:(r + 1) * RC, it * 512:(it + 1) * 512], ob[:], [ob], [FILT])
        P.barrier()


def interleave(gens):
    gens = list(gens)
    while gens:
        for g in list(gens):
            try:
                next(g)
            except StopIteration:
                gens.remove(g)


def hyena_conv(P, nc, L, s0, cd, FILT, GS, UH, skip_d, G2, own_off, Hown, YHo, CHN, tag):
    import contextlib
    with contextlib.ExitStack() as es:
        fft = FFT(P, nc, es, L, cd, tag, Hown=Hown)
        N1, H, Cb, nch = fft.N1, fft.H, fft.Cb, fft.nch
        ncg = CHN // Cb
        sb = lambda n, shp, dt=F32: es.enter_context(nc.sbuf_tensor(f"{n}{tag}", shp, dt))
        pa = [es.enter_context(nc.psum_tensor(f"pa{tag}{i}", [128, 1024], F32)) for i in range(2)]
        pb = [es.enter_context(nc.psum_tensor(f"pb{tag}{i}", [128, 1024], F32)) for i in range(2)]
        zf = [sb(f"zf{i}", [128, nch, Cb, 128], BF16) for i in range(2)]
        Gt = [sb(f"Gt{i}", [128, Cb, 2 * N1], BF16) for i in range(2)]
        skipbc = sb("skipbc", [128, 2, CHN])
        P.dma("sp", skipbc[:].rearrange("p o c -> p (o c)"), bcast_rows(skip_d, 128))
        def chain_f(n):
            o, cg = n // ncg, n % ncg
            k = n % 2
            z = zf[k]
            G = Gt[k]
            row0 = o * CHN + cg * Cb
            for kc in range(nch):
                P.dma("sp", z[:, kc], FILT[row0:row0 + Cb, kc * 16384:(kc + 1) * 16384].rearrange("c (a b) -> a c b", b=128), [FILT], [z])
            yield
            yield from fft.fwd([(kc, z[:, kc]) for kc in range(nch)], z, pa[k], pb[k], k)
            P.op("act", lambda e: e.copy(out=G[:, :, 0:N1], in_=pb[k][:, 0:512].rearrange("p (c k) -> p c k", c=Cb)), [pb[k]], [G])
            P.op("act", lambda e: e.copy(out=G[:, :, N1:2 * N1], in_=pb[k][:, 512:1024].rearrange("p (c k) -> p c k", c=Cb)), [pb[k]], [G])
            P.dma("act", GS[o][:, cg * Cb:(cg + 1) * Cb, :], G[:], [G], [GS[o]])
            yield
        for n in range(0, 2 * ncg, 2):
            interleave([chain_f(n), chain_f(n + 1)])
        vt = [sb(f"vt{i}", [H, Cb, 128], BF16) for i in range(2)]
        g1t = [sb(f"g1t{i}", [H, Cb, 128], BF16) for i in range(2)]
        g2t = [sb(f"g2t{i}", [Hown, Cb, 128], BF16) for i in range(2)]
        zt = [sb(f"zt{i}", [H, Cb, 128], BF16) for i in range(2)]
        zs = [sb(f"zs{i}", [H, Cb, 128], BF16) for i in range(2)]
        yt = [sb(f"yt{i}", [Hown, Cb, 128], BF16) for i in range(2)]
        tm = [sb(f"tm{i}", [H, Cb, 128]) for i in range(2)]
        G0 = [sb(f"G0{i}", [128, Cb, 2 * N1], BF16) for i in range(2)]
        G1 = [sb(f"G1{i}", [128, Cb, 2 * N1], BF16) for i in range(2)]
        def chain_d(cg):
            c0 = cg * Cb
            i = cg % 2
            g0, g1, v, ga, gb, z, zz, y, t = G0[i], G1[i], vt[i], g1t[i], g2t[i], zt[i], zs[i], yt[i], tm[i]
            P.dma("sp", g0[:], GS[0][:, c0:c0 + Cb, :], [GS[0]], [g0])
            P.dma("sp", g1[:], GS[1][:, c0:c0 + Cb, :], [GS[1]], [g1])
            P.dma("sp", v[:], UH[c0:c0 + Cb, 1 + s0:1 + s0 + L].rearrange("c (a b) -> a c b", b=128), [UH], [v])
            P.dma("sp", ga[:], UH[CHN + c0:CHN + c0 + Cb, 1 + s0:1 + s0 + L].rearrange("c (a b) -> a c b", b=128), [UH], [ga])
            P.dma("sp", gb[:], G2[c0:c0 + Cb, own_off:own_off + Hown * 128].rearrange("c (a b) -> a c b", b=128), [G2], [gb])
            yield
            k = i
            yield from fft.fwd([(0, v[:])], v, pa[k], pb[k], k)
            yield from fft.inv(g0, pa[k], pb[k], k)
            sk = bc_last(skipbc[0:H, 0, c0:c0 + Cb], 128)
            P.op("pool", lambda e, v=v, sk=sk, t=t: e.tensor_tensor(out=t[:], in0=v[:], in1=sk, op=ALU.mult), [v, skipbc], [t])
            P.op("dve", lambda e, t=t, k=k: e.tensor_tensor(out=t[:], in0=t[:], in1=pb[k][0:H, 0:Cb * 128].rearrange("p (c k) -> p c k", c=Cb), op=ALU.add),
                 [t, pb[k]], [t])
            P.op("dve", lambda e, t=t, ga=ga, z=z: e.tensor_tensor(out=z[:], in0=t[:], in1=ga[:], op=ALU.mult), [t, ga], [z])
            sk1 = bc_last(skipbc[0:H, 1, c0:c0 + Cb], 128)
            P.op("pool", lambda e, z=z, sk1=sk1, zz=zz: e.tensor_tensor(out=zz[:], in0=z[:], in1=sk1, op=ALU.mult), [z, skipbc], [zz])
            yield
            yield from fft.fwd([(0, z[:])], z, pa[k], pb[k], k)
            yield from fft.inv(g1, pa[k], pb[k], k, own=True, extra=(fft.SEL[:], zz[:], (fft.SEL, zz)))
            P.op("dve", lambda e, k=k, gb=gb, y=y: e.tensor_tensor(out=y[:], in0=pb[k][0:Hown, 0:Cb * 128].rearrange("p (c k) -> p c k", c=Cb), in1=gb[:], op=ALU.mult),
                 [pb[k], gb], [y])
            P.dma("act", YHo[c0:c0 + Cb, own_off:own_off + Hown * 128].rearrange("c (a b) -> a c b", b=128), y[:], [y], [YHo])
            yield
        for cg in range(0, ncg, 2):
            interleave([chain_d(cg), chain_d(cg + 1)])
        P.barrier()


NH = 12
HD = 64
DA = NH * HD
KB = 17
NEGV = -30000.0


def attn_sel_table():
    sel = np.zeros((32, 2432), np.float32)
    mult = {}
    for d in (1, 4, 16):
        for j in range(-64, 65):
            mult[j * d] = mult.get(j * d, 0) + 1
    for delta, m in mult.items():
        n = abs(delta)
        ret = 16 if delta > 0 else 0
        nf = np.float32(max(n, 1))
        large = 8 + int(np.float32(np.log(nf / np.float32(8)) / np.float32(math.log(1024 / 8)) * np.float32(8)))
        large = min(large, 15)
        b = ret + (n if n < 8 else large)
        sel[b, delta + 1151] = m
    return sel


def phase_q(P, nc, cfg, x_ext, wq_d, wk_d, wv_d, gmix, gqk_d, kvalid_d, ident_d, bd_d, KT, QT, VE):
    import contextlib
    NE = cfg["NE"]
    own_tiles = cfg["own_tiles"]
    with contextlib.ExitStack() as es:
        sb = lambda n, shp, dt=F32: es.enter_context(nc.sbuf_tensor(n, shp, dt))
        xt = XT(P, nc, es, "q")
        xt.setup(ident_d)
        Wq, Wk, Wv = sb("Wq", [128, DK, DA], BF16), sb("Wk", [128, DK, DA], BF16), sb("Wv", [128, DK, DA], BF16)
        g_sb, gqk, epsT = sb("gq_sb", [128, DK]), sb("gqk_sb", [128, 2]), sb("epsTq", [128, 1])
        ones, BD = sb("ones_q", [128, 128], BF16), sb("BDq", [128, 128], BF16)
        e2 = [sb(f"e2{i}", [128, 512]) for i in range(2)]
        e2c = [sb(f"e2c{i}", [128, 4]) for i in range(2)]
        e2bc = [sb(f"e2bc{i}", [128, 4, 128]) for i in range(2)]
        identf = sb("identf", [128, 128])
        ssq, rcol = [sb(f"ssq{i}", [128, 4]) for i in range(2)], [sb(f"rcol{i}", [128, 4]) for i in range(2)]
        junk = sb("junkq", [128, D], BF16)
        sqk = [sb(f"sqk{i}", [128, 512], BF16) for i in range(2)]
        den = [sb(f"den{i}", [128, 512]) for i in range(2)]
        kn = [sb(f"kn{i}", [128, 512], BF16) for i in range(2)]
        Vt = [sb(f"Vt{i}", [128, NH, HD + 1], BF16) for i in range(2)]
        pss = es.enter_context(nc.psum_tensor("pssq", [128, 512], F32))
        pk = [es.enter_context(nc.psum_tensor(f"pk{i}", [128, 512], F32)) for i in range(2)]
        pst = es.enter_context(nc.psum_tensor("pst", [128, 512], F32))
        pv = es.enter_context(nc.psum_tensor("pv", [128, 2, 512], F32))
        P.op("pool", lambda e: e.memset(ones[:], 1.0), [], [ones])
        P.op("pool", lambda e: e.memset(epsT[:], EPS), [], [epsT])
        for i in range(2):
            P.op("pool", lambda e, i=i: e.memset(Vt[i][:], 1.0), [], [Vt[i]])
            P.op("pool", lambda e, i=i: e.memset(ssq[i][:], 0.0), [], [ssq[i]])
        P.dma("sp", g_sb[:], gmix)
        P.dma("sp", gqk[:], gqk_d)
        P.op("dve", lambda e: e.tensor_scalar(out=gqk[:, 0:1], in0=gqk[:, 0:1], scalar1=0.125, scalar2=None, op0=ALU.mult), [gqk], [gqk])
        stage = xt.xin[0]
        P.dma("sp", stage[:, 0, 0:128], bd_d, None, [stage])
        P.op("dve", lambda e: e.tensor_copy(out=BD[:], in_=stage[:, 0, 0:128]), [stage], [BD])
        P.dma("sp", identf[:], ident_d)
        for h in range(NH):
            P.dma("act", KT[h, 64:65, :], kvalid_d, [], [("KTrow", h)])
            P.dma("act", QT[h, 64:65, :], cfg["ones_row"], [], [("QTrow", h)])
        for W, wd in ((Wq, wq_d), (Wk, wk_d), (Wv, wv_d)):
            for j in range(DK):
                P.dma("sp", stage[:, 0, 0:DA], wd[j * 128:(j + 1) * 128, :], None, [stage])
                P.op("dve", lambda e, j=j, W=W: e.tensor_scalar(out=W[:, j, :], in0=stage[:, 0, 0:DA], scalar1=g_sb[:, j:j + 1],
                                                               scalar2=None, op0=ALU.mult), [stage, g_sb], [W])
        for it in range(NE // 512):
            t0 = it * 512
            xin, xbf, xT = xt.load(x_ext[t0:t0 + 512, :])
            b = it % 2
            for s in range(4):
                P.op("act", lambda e, s=s: e.activation(out=junk[:], in_=xin[:, s, :], func=AF.Square, accum_out=ssq[b][:, s:s + 1]), [xin], [junk, ssq[b]])
            P.op("act", lambda e: e.activation(out=rcol[b][:], in_=ssq[b][:], func=AF.Sqrt, bias=epsT[:], scale=1.0 / D), [ssq[b], epsT], [rcol[b]])
            P.op("dve", lambda e: e.reciprocal(out=rcol[b][:], in_=rcol[b][:]), [rcol[b]], [rcol[b]])
            P.op("dve", lambda e: e.tensor_scalar(out=e2c[b][:], in0=ssq[b][:], scalar1=64.0 * EPS / D, scalar2=64.0 * EPS * EPS, op0=ALU.mult, op1=ALU.add),
                 [ssq[b]], [e2c[b]])
            P.op("dve", lambda e: e.tensor_copy(out=e2bc[b][:], in_=bc_last(e2c[b][:, 0:4], 128)), [e2c[b]], [e2bc[b]])
            for s4 in range(4):
                P.op("pe", lambda e, s4=s4: e.matmul(pss[:, s4 * 128:(s4 + 1) * 128], lhsT=e2bc[b][:, s4, :], rhs=identf[:], start=True, stop=True),
                     [e2bc[b], identf], [pss])
            P.op("act", lambda e: e.copy(out=e2[b][:], in_=pss[:]), [pss], [e2[b]])
            P.op("pool", lambda e: e.memset(ssq[b][:], 0.0), [], [ssq[b]])
            jobs = [(Wk, 1, KT, t0)]
            if it in own_tiles:
                jobs.append((Wq, 0, QT, own_tiles[it] * 512))
            n = 0
            for (W, gi, OUT, o0) in jobs:
                for m in range(DA // 128):
                    p = pk[n % 2]
                    i2 = n % 2
                    n += 1
                    for j in range(DK):
                        P.op("pe", lambda e, j=j, m=m, p=p, W=W: e.matmul(p[:], lhsT=W[:, j, m * 128:(m + 1) * 128], rhs=xT[:, j, :],
                                                                          start=(j == 0), stop=(j == DK - 1)), [W, (xT.name, j // 2)], [p])
                    P.op("act", lambda e, p=p, i2=i2: e.activation(out=sqk[i2][:], in_=p[:], func=AF.Square), [p], [sqk[i2]])
                    P.op("pe", lambda e, i2=i2: e.matmul(pst[:], lhsT=BD[:], rhs=sqk[i2][:], start=True, stop=True), [BD, sqk[i2]], [pst])
                    P.op("dve", lambda e, i2=i2: e.tensor_tensor(out=den[i2][:], in0=pst[:], in1=e2[b][:], op=ALU.add), [pst, e2[b]], [den[i2]])
                    P.op("act", lambda e, i2=i2: e.activation(out=den[i2][:], in_=den[i2][:], func=AF.Sqrt, scale=1.0 / 64), [den[i2]], [den[i2]])
                    P.op("dve", lambda e, i2=i2: e.reciprocal(out=den[i2][:], in_=den[i2][:]), [den[i2]], [den[i2]])
                    P.op("dve", lambda e, i2=i2, p=p, gi=gi: e.scalar_tensor_tensor(out=kn[i2][:], in0=p[:], scalar=gqk[:, gi:gi + 1], in1=den[i2][:],
                                                                                    op0=ALU.mult, op1=ALU.mult), [p, gqk, den[i2]], [kn[i2]])
                    for hh in range(2):
                        h = 2 * m + hh
                        P.dma("act", OUT[h, 0:64, o0:o0 + 512], kn[i2][hh * 64:(hh + 1) * 64, :], [kn[i2]], [(OUT.tensor.name, h)])
            for s in range(4):
                for (c0, cn, bk) in ((0, 512, 0), (512, 256, 1)):
                    for j in range(DK):
                        P.op("pe", lambda e, j=j, s=s, c0=c0, cn=cn, bk=bk: e.matmul(pv[:, bk, 0:cn], lhsT=xT[:, j, s * 128:(s + 1) * 128], rhs=Wv[:, j, c0:c0 + cn],
                                                                                    start=(j == 0), stop=(j == DK - 1)), [Wv, (xT.name, j // 2)], [pv])
                vt = Vt[s % 2]
                P.op("act", lambda e, s=s, vt=vt: e.activation(out=vt[:, 0:8, 0:HD], in_=pv[:, 0, :].rearrange("p (h c) -> p h c", c=HD), func=AF.Identity,
                                                               scale=rcol[b][:, s:s + 1]), [pv, rcol[b]], [vt])
                P.op("act", lambda e, s=s, vt=vt: e.activation(out=vt[:, 8:12, 0:HD], in_=pv[:, 1, 0:256].rearrange("p (h c) -> p h c", c=HD), func=AF.Identity,
                                                               scale=rcol[b][:, s:s + 1]), [pv, rcol[b]], [vt])
                P.dma("act", VE[t0 + s * 128:t0 + (s + 1) * 128, :], vt[:].rearrange("p h c -> p (h c)"), [vt], [VE])
        P.barrier()


def phase_attn(P, nc, cfg, relb_d, sel_d, jmat_d, KT, QT, VE, EV, EBD, YA):
    import contextlib
    with contextlib.ExitStack() as es:
        sb = lambda n, shp, dt=F32: es.enter_context(nc.sbuf_tensor(n, shp, dt))
        relb, expb, sel = sb("relb_sb", [32, NH]), sb("expb", [32, NH]), sb("sel_sb", [32, 2432])
        ev = sb("ev", [NH, 2432], BF16)
        J, stage = sb("Jm", [128, 128], BF16), sb("stg_a", [128, 128])
        Gall = [sb(f"Gall{i}", [128, 2304], BF16) for i in range(2)]
        EBt = [sb(f"EBt{i}", [128, KB * 128], BF16) for i in range(2)]
        psA = es.enter_context(nc.psum_tensor("psA", [128, 3, 512], F32))
        psB = es.enter_context(nc.psum_tensor("psB", [128, 2, 512], F32))
        po = [es.enter_context(nc.psum_tensor(f"po{i}", [128, 512], F32)) for i in range(2)]
        P.dma("sp", relb[:], relb_d)
        P.dma("sp", sel[:], sel_d)
        P.dma("sp", stage[:], jmat_d)
        P.op("dve", lambda e: e.tensor_copy(out=J[:], in_=stage[:]), [stage], [J])
        P.op("act", lambda e: e.activation(out=expb[:], in_=relb[:], func=AF.Exp), [relb], [expb])
        for c in range(5):
            w = 512 if c < 4 else 2432 - 2048
            pp = psA[0:NH, 0, 0:w]
            P.op("pe", lambda e, c=c, w=w, pp=pp: e.matmul(pp, lhsT=expb[:], rhs=sel[:, c * 512:c * 512 + w], start=True, stop=True), [expb, sel], [psA])
            P.op("dve", lambda e, c=c, w=w, pp=pp: e.tensor_copy(out=ev[:, c * 512:c * 512 + w], in_=pp), [psA], [ev])
        P.dma("sp", EV, ev[:], [ev], [EV])
        for h in range(NH):
            G = Gall[h % 2]
            E = EBt[h % 2]
            src = bass.AP(tensor=EV.tensor, offset=EV[h, 0:1].offset, ap=[[1, 128], [1, 2304]])
            P.dma("sp", G[:], src, [EV], [G])
            for i in range(KB):
                tgt = psA if i < 12 else psB
                ii = i if i < 12 else i - 12
                P.op("pe", lambda e, i=i, tgt=tgt, ii=ii, G=G: e.matmul(tgt[:, ii // 4, (ii % 4) * 128:(ii % 4 + 1) * 128], lhsT=G[:, i * 128:(i + 1) * 128], rhs=J[:],
                                                                        start=True, stop=True), [G, J], [tgt])
            P.op("dve", lambda e, E=E: e.tensor_copy(out=E[:, 0:1536], in_=psA[:].rearrange("p a b -> p (a b)")), [psA], [E])
            P.op("dve", lambda e, E=E: e.tensor_copy(out=E[:, 1536:KB * 128], in_=psB[:].rearrange("p a b -> p (a b)")[:, 0:KB * 128 - 1536]), [psB], [E])
            P.dma("sp", EBD[h], E[:], [E], [(EBD.tensor.name, h)])
        Lx = max(p[1] for p in cfg["pieces"])
        Lo = max(p[3] for p in cfg["pieces"])
        KTh = [sb(f"KTh{i}", [HD + 1, Lx], BF16) for i in range(2)]
        Vh = [sb(f"Vh{i}", [128, Lx // 128, HD + 1], BF16) for i in range(2)]
        QTh = [sb(f"QTh{i}", [HD + 1, Lo], BF16) for i in range(2)]
        EBh = [sb(f"EBh{i}", [128, KB * 128], BF16) for i in range(2)]
        Et = [sb(f"Et{i}", [128, KB * 128], BF16) for i in range(2)]
        Pt = [sb(f"Pt{i}", [128, KB * 128], BF16) for i in range(2)]
        yat = [sb(f"yat{i}", [128, Lo // 128, HD]) for i in range(2)]
        rs = sb("rs_a", [128, 2])
        n = 0
        nq = 0
        for (ext0, Lext, own0, Lown) in cfg["pieces"]:
            for h in range(NH):
                i = n % 2
                n += 1
                kt, vh, qt_, eb, ya = KTh[i], Vh[i], QTh[i], EBh[i], yat[i]
                P.dma("sp", kt[:, 0:Lext], KT[h, :, ext0:ext0 + Lext], [(KT.tensor.name, h), ("KTrow", h)], [kt])
                P.dma("sp", vh[:, 0:Lext // 128, :], VE[ext0:ext0 + Lext, h * (HD + 1):(h + 1) * (HD + 1)].rearrange("(t p) c -> p t c", p=128), [VE], [vh])
                P.dma("sp", qt_[:, 0:Lown], QT[h, :, own0:own0 + Lown], [(QT.tensor.name, h), ("QTrow", h)], [qt_])
                P.dma("sp", eb[:], EBD[h], [(EBD.tensor.name, h)], [eb])
                for q in range(Lown // 128):
                    j = nq % 2
                    nq += 1
                    et, pt, pq = Et[j], Pt[j], po[j]
                    qs = qt_[:, q * 128:(q + 1) * 128]
                    for i2 in range(KB):
                        if i2 < 9:
                            dst = psA[:, i2 // 4, (i2 % 4) * 128:(i2 % 4 + 1) * 128]
                            key = psA
                        else:
                            dst = psB[:, (i2 - 9) // 4, ((i2 - 9) % 4) * 128:((i2 - 9) % 4 + 1) * 128]
                            key = psB
                        P.op("pe", lambda e, dst=dst, i2=i2, q=q, qs=qs: e.matmul(dst, lhsT=kt[:, (q + i2) * 128:(q + i2 + 1) * 128], rhs=qs, start=True, stop=True),
                             [kt, qt_], [key])
                    for (ps_, c0, w, o0) in ((psA, 0, 512, 0), (psA, 1, 512, 512), (psA, 2, 128, 1024), (psB, 0, 512, 1152), (psB, 1, 512, 1664)):
                        P.op("act", lambda e, ps_=ps_, c0=c0, w=w, o0=o0: e.activation(out=et[:, o0:o0 + w], in_=ps_[:, c0, 0:w], func=AF.Exp), [ps_], [(et.name, o0 >= 1152)])
                    P.op("dve", lambda e: e.tensor_tensor(out=pt[:, 0:1152], in0=et[:, 0:1152], in1=eb[:, 0:1152], op=ALU.mult), [(et.name, False), eb], [(pt.name, False)])
                    P.op("dve", lambda e: e.tensor_tensor(out=pt[:, 1152:], in0=et[:, 1152:], in1=eb[:, 1152:], op=ALU.mult), [(et.name, True), eb], [(pt.name, True)])
                    for i2 in range(KB):
                        P.op("pe", lambda e, i2=i2, q=q: e.matmul(pq[:, 0:HD + 1], lhsT=pt[:, i2 * 128:(i2 + 1) * 128], rhs=vh[:, q + i2, :], start=(i2 == 0), stop=(i2 == KB - 1)),
                             [(pt.name, i2 >= 9), vh], [pq])
                    P.op("dve", lambda e, j=j: e.reciprocal(out=rs[:, j:j + 1], in_=pq[:, HD:HD + 1]), [pq], [(rs.name, j)])
                    P.op("dve", lambda e, j=j, q=q: e.tensor_scalar(out=ya[:, q, :], in0=pq[:, 0:HD], scalar1=rs[:, j:j + 1], scalar2=None, op0=ALU.mult),
                         [pq, (rs.name, j)], [ya])
                P.dma("act", YA[own0:own0 + Lown, h * HD:(h + 1) * HD].rearrange("(t p) c -> p t c", p=128), ya[:, 0:Lown // 128, :], [ya], [YA])
        P.barrier()


NEXP = 32
DE = 1024
BLK = 256
NBLK = 80
NSLOT = NBLK * BLK


def indirect_gather(P, out, in_, idx_ap, reads, writes):
    P.dma("pool", out, in_, reads, writes, fn=lambda e: e.indirect_dma_start(
        out=out, out_offset=None, in_=in_, in_offset=bass.IndirectOffsetOnAxis(ap=idx_ap, axis=0)))


def indirect_scatter(P, out, in_, idx_ap, reads, writes):
    P.dma("pool", out, in_, reads, writes, fn=lambda e: e.indirect_dma_start(
        out=out, out_offset=bass.IndirectOffsetOnAxis(ap=idx_ap, axis=0), in_=in_, in_offset=None))


def phase3(P, nc, cfg, d, YHo, YA, X1, H2B, AAd, GTd):
    import contextlib
    NOWN = cfg["NOWN"]
    NT = NOWN // 128
    CH = 1280 // 128
    CA = DA // 128
    with contextlib.ExitStack() as es:
        sb = lambda n, shp, dt=F32: es.enter_context(nc.sbuf_tensor(n, shp, dt))
        Wo = sb("Wo", [128, DK, D], BF16)
        gh, ga, gfb, Wr, rbb = sb("gh3", [128, CH]), sb("ga3", [128, CA]), sb("gfb", [128, D]), sb("Wr", [128, DK, 36]), sb("rbb", [128, 36])
        identf, identb, ones, epsT = sb("identf3", [128, 128]), sb("identb3", [128, 128], BF16), sb("ones3", [128, 128], BF16), sb("epsT3", [128, 1])
        stage = sb("stage3", [128, D])
        yh = [sb(f"yh{i}", [128, CH, 512], BF16) for i in range(2)]
        sq = sb("sq3", [128, CH, 512], BF16)
        rh = sb("rh3", [128, 512])
        yhn = sb("yhn", [128, CH, 512], BF16)
        yat = [sb(f"yat3{i}", [128, DA]) for i in range(2)]
        yan = sb("yan", [128, DA], BF16)
        yaT = sb("yaT", [128, CA, 512], BF16)
        xo = [sb(f"xo{i}", [128, D]) for i in range(2)]
        x1 = sb("x1t", [128, D])
        h2 = sb("h2t", [128, D])
        h2b = [sb(f"h2b{i}", [128, D], BF16) for i in range(2)]
        h2T = sb("h2T", [128, DK, 128])
        junk = sb("junk3", [128, D], BF16)
        sm = sb("sm3", [128, 16])
        lg = sb("lg3", [128, 36])
        ohg, eg, esel, oh1, e2, oh2 = sb("ohg", [128, 4]), sb("eg", [128, 4]), sb("esel", [128, 8]), sb("oh1", [128, 8]), sb("e2r", [128, 8]), sb("oh2", [128, 8])
        AA = sb("AA3", [128, NT, 64])
        GT = sb("GT3", [128, NT, 2])
        pss = es.enter_context(nc.psum_tensor("pss3", [128, 512], F32))
        ptr = es.enter_context(nc.psum_tensor("ptr3", [128, CA, 128], BF16))
        po = [es.enter_context(nc.psum_tensor(f"po3{i}", [128, 512], F32)) for i in range(4)]
        pt2 = es.enter_context(nc.psum_tensor("pt23", [128, 4, 128], F32))
        pr = es.enter_context(nc.psum_tensor("pr3", [128, 64], F32))
        P.op("pool", lambda e: e.memset(ones[:], 1.0), [], [ones])
        P.op("pool", lambda e: e.memset(epsT[:], EPS), [], [epsT])
        P.op("pool", lambda e: e.memset(sm[:], 0.0), [], [(sm.name, c) for c in range(16)])
        P.dma("sp", gh[:], d["gh"])
        P.dma("sp", ga[:], d["ga"])
        P.dma("sp", gfb[:], bcast_rows(d["gf"], 128))
        P.dma("sp", rbb[:], bcast_rows(d["rb"], 128))
        P.dma("sp", Wr[:], d["wr"])
        P.dma("sp", identf[:], d["ident"])
        P.op("dve", lambda e: e.tensor_copy(out=identb[:], in_=identf[:]), [identf], [identb])
        for j in range(DK):
            P.dma("sp", stage[:], d["w_out"][j * 128:(j + 1) * 128, :], None, [stage])
            P.op("dve" if j % 2 else "act", (lambda e, j=j: e.tensor_copy(out=Wo[:, j, :], in_=stage[:])) if j % 2 else (lambda e, j=j: e.copy(out=Wo[:, j, :], in_=stage[:])),
                 [stage], [Wo])
        for st in range(NOWN // 512):
            t0 = st * 512
            y = yh[st % 2]
            P.dma("sp", y[:], YHo[:, t0:t0 + 512].rearrange("(k p) t -> p k t", p=128), [YHo], [y])
            P.op("pool", lambda e, y=y: e.tensor_tensor(out=sq[:], in0=y[:], in1=y[:], op=ALU.mult), [y], [sq])
            for k in range(CH):
                P.op("pe", lambda e, k=k: e.matmul(pss[:], lhsT=ones[:], rhs=sq[:, k, :], start=(k == 0), stop=(k == CH - 1)), [ones, sq], [pss])
            P.op("act", lambda e: e.activation(out=rh[:], in_=pss[:], func=AF.Sqrt, bias=epsT[:], scale=1.0 / 1280), [pss, epsT], [rh])
            P.op("dve", lambda e: e.reciprocal(out=rh[:], in_=rh[:]), [rh], [rh])
            for k in range(CH):
                P.op("dve", lambda e, k=k, y=y: e.scalar_tensor_tensor(out=yhn[:, k, :], in0=y[:, k, :], scalar=gh[:, k:k + 1], in1=rh[:], op0=ALU.mult, op1=ALU.mult),
                     [y, gh, rh], [(yhn.name, k)])
            for s in range(4):
                tt = st * 4 + s
                r0 = t0 + s * 128
                ya, xin, hb = yat[tt % 2], xo[tt % 2], h2b[tt % 2]
                P.dma("sp", ya[:], YA[r0:r0 + 128, :], [YA], [ya])
                P.dma("sp", xin[:], d["x_own_fn"](r0), None, [xin])
                P.op("act", lambda e, ya=ya: e.activation(out=junk[:, 0:DA], in_=ya[:], func=AF.Square, accum_out=sm[:, 0:1]), [ya], [junk, (sm.name, 0)])
                P.op("act", lambda e: e.activation(out=sm[:, 1:2], in_=sm[:, 0:1], func=AF.Sqrt, bias=epsT[:], scale=1.0 / DA), [(sm.name, 0), epsT], [(sm.name, 1)])
                P.op("dve", lambda e: e.reciprocal(out=sm[:, 1:2], in_=sm[:, 1:2]), [(sm.name, 1)], [(sm.name, 1)])
                P.op("pool", lambda e: e.memset(sm[:, 0:1], 0.0), [], [(sm.name, 0)])
                P.op("act", lambda e, ya=ya: e.activation(out=yan[:], in_=ya[:], func=AF.Identity, scale=sm[:, 1:2]), [ya, (sm.name, 1)], [yan])
                for k in range(CA):
                    P.op("pe", lambda e, k=k: e.transpose(out=ptr[:, k, :], in_=yan[:, k * 128:(k + 1) * 128], identity=identb[:]), [yan, identb], [ptr])
                for k in range(CA):
                    P.op("dve", lambda e, k=k, s=s: e.tensor_scalar(out=yaT[:, k, s * 128:(s + 1) * 128], in0=ptr[:, k, :], scalar1=ga[:, k:k + 1], scalar2=None, op0=ALU.mult),
                         [ptr, ga], [(yaT.name, s)])
                for n in range(4):
                    for k in range(DK):
                        if k < CH:
                            lhs, key = yhn[:, k, s * 128:(s + 1) * 128], (yhn.name, k)
                        else:
                            lhs, key = yaT[:, k - CH, s * 128:(s + 1) * 128], (yaT.name, s)
                        P.op("pe", lambda e, n=n, k=k, lhs=lhs: e.matmul(po[n][:], lhsT=lhs, rhs=Wo[:, k, n * 512:(n + 1) * 512], start=(k == 0), stop=(k == DK - 1)),
                             [key, Wo], [po[n]])
                    P.op("dve", lambda e, n=n, xin=xin: e.tensor_tensor(out=x1[:, n * 512:(n + 1) * 512], in0=po[n][:], in1=xin[:, n * 512:(n + 1) * 512], op=ALU.add),
                         [po[n], xin], [x1])
                P.dma("act", X1[r0:r0 + 128, :], x1[:], [x1], [X1])
                P.op("act", lambda e: e.activation(out=junk[:], in_=x1[:], func=AF.Square, accum_out=sm[:, 2:3]), [x1], [junk, (sm.name, 2)])
                P.op("act", lambda e: e.activation(out=sm[:, 3:4], in_=sm[:, 2:3], func=AF.Sqrt, bias=epsT[:], scale=1.0 / D), [(sm.name, 2), epsT], [(sm.name, 3)])
                P.op("dve", lambda e: e.reciprocal(out=sm[:, 3:4], in_=sm[:, 3:4]), [(sm.name, 3)], [(sm.name, 3)])
                P.op("pool", lambda e: e.memset(sm[:, 2:3], 0.0), [], [(sm.name, 2)])
                P.op("dve", lambda e: e.scalar_tensor_tensor(out=h2[:], in0=x1[:], scalar=sm[:, 3:4], in1=gfb[:], op0=ALU.mult, op1=ALU.mult), [x1, (sm.name, 3), gfb], [h2])
                P.op("act", lambda e, hb=hb: e.copy(out=hb[:], in_=h2[:]), [h2], [hb])
                P.dma("act", H2B[r0:r0 + 128, :], hb[:], [hb], [H2B])
                for jj in range(4):
                    for j2 in range(4):
                        j = jj * 4 + j2
                        P.op("pe", lambda e, j=j, j2=j2: e.transpose(out=pt2[:, j2, :], in_=h2[:, j * 128:(j + 1) * 128], identity=identf[:]), [h2, identf], [pt2])
                    P.op("act" if jj % 2 else "dve", (lambda e, jj=jj: e.copy(out=h2T[:, jj * 4:(jj + 1) * 4, :], in_=pt2[:])) if jj % 2 else
                         (lambda e, jj=jj: e.tensor_copy(out=h2T[:, jj * 4:(jj + 1) * 4, :], in_=pt2[:])), [pt2], [h2T])
                for j in range(DK):
                    P.op("pe", lambda e, j=j: e.matmul(pr[:, 0:36], lhsT=h2T[:, j, :], rhs=Wr[:, j, :], start=(j == 0), stop=(j == DK - 1)), [h2T, Wr], [pr])
                P.op("dve", lambda e: e.tensor_tensor(out=lg[:], in0=pr[:, 0:36], in1=rbb[:], op=ALU.add), [pr, rbb], [lg])
                R = lambda c: (sm.name, c)
                lgg = lg[:, 0:4]
                lge = lg[:, 4:36].rearrange("p (g j) -> p g j", g=4)
                P.op("dve", lambda e: e.tensor_reduce(out=sm[:, 4:5], in_=lgg, axis=AX.X, op=ALU.max), [lg], [R(4)])
                P.op("dve", lambda e: e.tensor_scalar(out=ohg[:], in0=lgg, scalar1=sm[:, 4:5], scalar2=None, op0=ALU.is_equal), [lg, R(4)], [ohg])
                P.op("dve", lambda e: e.tensor_scalar(out=sm[:, 5:6], in0=sm[:, 4:5], scalar1=-1.0, scalar2=None, op0=ALU.mult), [R(4)], [R(5)])
                P.op("act", lambda e: e.activation(out=eg[:], in_=lgg, func=AF.Exp, bias=sm[:, 5:6], scale=1.0, accum_out=sm[:, 6:7]), [lg, R(5)], [eg, R(6)])
                P.op("dve", lambda e: e.reciprocal(out=sm[:, 7:8], in_=sm[:, 6:7]), [R(6)], [R(7)])
                P.op("pool", lambda e: e.memset(sm[:, 6:7], 0.0), [], [R(6)])
                P.op("dve", lambda e: e.tensor_scalar(out=esel[:], in0=lge[:, 0, :], scalar1=ohg[:, 0:1], scalar2=None, op0=ALU.mult), [lg, ohg], [esel])
                for g in range(1, 4):
                    P.op("dve", lambda e, g=g: e.scalar_tensor_tensor(out=esel[:], in0=lge[:, g, :], scalar=ohg[:, g:g + 1], in1=esel[:], op0=ALU.mult, op1=ALU.add),
                         [lg, ohg, esel], [esel])
                P.op("dve", lambda e: e.tensor_reduce(out=sm[:, 8:9], in_=esel[:], axis=AX.X, op=ALU.max), [esel], [R(8)])
                P.op("dve", lambda e: e.tensor_scalar(out=oh1[:], in0=esel[:], scalar1=sm[:, 8:9], scalar2=None, op0=ALU.is_equal), [esel, R(8)], [oh1])
                P.op("dve", lambda e: e.scalar_tensor_tensor(out=e2[:], in0=oh1[:], scalar=-1.0e9, in1=esel[:], op0=ALU.mult, op1=ALU.add), [oh1, esel], [e2])
                P.op("dve", lambda e: e.tensor_reduce(out=sm[:, 9:10], in_=e2[:], axis=AX.X, op=ALU.max), [e2], [R(9)])
                P.op("dve", lambda e: e.tensor_scalar(out=oh2[:], in0=e2[:], scalar1=sm[:, 9:10], scalar2=None, op0=ALU.is_equal), [e2, R(9)], [oh2])
                P.op("dve", lambda e: e.tensor_tensor(out=sm[:, 10:11], in0=sm[:, 9:10], in1=sm[:, 8:9], op=ALU.subtract), [R(9), R(8)], [R(10)])
                P.op("act", lambda e: e.activation(out=sm[:, 11:12], in_=sm[:, 10:11], func=AF.Exp), [R(10)], [R(11)])
                P.op("dve", lambda e: e.tensor_scalar(out=sm[:, 12:13], in0=sm[:, 11:12], scalar1=1.0, scalar2=None, op0=ALU.add), [R(11)], [R(12)])
                P.op("dve", lambda e: e.reciprocal(out=sm[:, 12:13], in_=sm[:, 12:13]), [R(12)], [R(12)])
                P.op("dve", lambda e, tt=tt: e.tensor_tensor(out=GT[:, tt, 0:1], in0=sm[:, 12:13], in1=sm[:, 7:8], op=ALU.mult), [R(12), R(7)], [GT])
                P.op("dve", lambda e, tt=tt: e.tensor_tensor(out=GT[:, tt, 1:2], in0=GT[:, tt, 0:1], in1=sm[:, 11:12], op=ALU.mult), [GT, R(11)], [GT])
                P.op("dve", lambda e, tt=tt: e.tensor_tensor(out=AA[:, tt, 0:32].rearrange("p (g j) -> p g j", g=4), in0=bc_last(ohg[:], 8), in1=bc_mid(oh1[:], 4), op=ALU.mult),
                     [ohg, oh1], [AA])
                P.op("dve", lambda e, tt=tt: e.tensor_tensor(out=AA[:, tt, 32:64].rearrange("p (g j) -> p g j", g=4), in0=bc_last(ohg[:], 8), in1=bc_mid(oh2[:], 4), op=ALU.mult),
                     [ohg, oh2], [AA])
        P.dma("sp", AAd, AA[:], [AA], [AAd])
        P.dma("sp", GTd, GT[:], [GT], [GTd])
        P.barrier()


def phase_moe(P, nc, cfg, d, X1, H2B, AAd, GTd, XB, YB, y_out):
    import contextlib
    NOWN = cfg["NOWN"]
    NT = NOWN // 128
    NBLK = cfg.get("NBLK", 128)
    with contextlib.ExitStack() as es:
        sb = lambda n, shp, dt=F32: es.enter_context(nc.sbuf_tensor(n, shp, dt))
        es0 = es.enter_context(contextlib.ExitStack())
        sb0 = lambda n, shp, dt=F32: es0.enter_context(nc.sbuf_tensor(n, shp, dt))
        GT = sb("GTm", [128, NT, 2])
        identb = sb("identbm", [128, 128], BF16)
        dst = sb("dsti", [128, NT, 2], I32)
        idxg, idxd = sb("idxg", [128, NBLK, DK], I32), sb("idxd", [128, NBLK, DK // 2], I32)
        AA = sb0("AAm", [128, NT, 64])
        identf, onesf, utri = sb0("identfm", [128, 128]), sb0("onesfm", [128, 128]), sb0("utrim", [128, 128])
        ltm, bpos, offg = sb0("ltm_sb", [128, 32, 32]), sb0("bposm", [128, 1]), sb0("offgm", [128, DK])
        cnt, padi, pad, pend, pstart, base = sb0("cntm", [128, 32]), sb0("padi", [128, 32], I32), sb0("padm", [128, 32]), sb0("pendm", [128, 32]), sb0("pstm", [128, 32]), sb0("basem", [128, 32])
        tmp3 = sb0("tmp3m", [128, 32, 32])
        off, prod = sb0("offm", [128, 32]), sb0("prodm", [128, 32])
        dstf = sb0("dstf", [128, NT, 2])
        cmp_, bef, be2 = sb0("cmpm", [128, 32]), sb0("befm", [128, 1]), sb0("be2m", [128, 128])
        bebc = sb0("bebc", [128, 128])
        idxf = sb0("idxfm", [128, NBLK, DK])
        pm = es.enter_context(nc.psum_tensor("pmm", [128, 128], F32))
        P.dma("sp", AA[:], AAd)
        P.dma("sp", GT[:], GTd)
        P.dma("sp", identf[:], d["ident"])
        P.dma("sp", utri[:], d["utri"])
        P.dma("sp", ltm[:].rearrange("p a b -> p (a b)"), bcast_rows(d["ltm"], 128))
        P.dma("sp", bpos[:], d["bpos"])
        P.dma("sp", offg[:], d["offg"])
        P.op("dve", lambda e: e.tensor_copy(out=identb[:], in_=identf[:]), [identf], [identb])
        P.op("pool", lambda e: e.memset(onesf[:], 1.0), [], [onesf])
        P.op("pool", lambda e: e.memset(base[:], 0.0), [], [base])
        for i in range(NT):
            P.op("pe", lambda e, i=i: e.matmul(pm[:, 0:64], lhsT=onesf[:], rhs=AA[:, i, :], start=(i == 0), stop=(i == NT - 1)), [onesf, AA], [pm])
        P.op("dve", lambda e: e.tensor_copy(out=cnt[:], in_=pm[:, 0:32]), [pm], [cnt])
        P.op("dve", lambda e: e.tensor_tensor(out=cnt[:], in0=cnt[:], in1=pm[:, 32:64], op=ALU.add), [cnt, pm], [cnt])
        P.op("dve", lambda e: e.tensor_scalar(out=padi[:], in0=cnt[:], scalar1=float(BLK - 1), scalar2=None, op0=ALU.add), [cnt], [padi])
        P.op("dve", lambda e: e.tensor_scalar(out=padi[:], in0=padi[:], scalar1=8, scalar2=8, op0=ALU.arith_shift_right, op1=ALU.logical_shift_left), [padi], [padi])
        P.op("dve", lambda e: e.tensor_copy(out=pad[:], in_=padi[:]), [padi], [pad])
        P.op("dve", lambda e: e.tensor_tensor(out=tmp3[:], in0=bc_mid(pad[:], 32), in1=ltm[:], op=ALU.mult), [pad, ltm], [tmp3])
        P.op("dve", lambda e: e.reduce_sum(out=pend[:], in_=tmp3[:], axis=AX.X), [tmp3], [pend])
        P.op("dve", lambda e: e.tensor_tensor(out=pstart[:], in0=pend[:], in1=pad[:], op=ALU.subtract), [pend, pad], [pstart])
        for i in range(NT):
            P.op("pe", lambda e, i=i: e.matmul(pm[:, 0:64], lhsT=utri[:], rhs=AA[:, i, :], start=True, stop=True), [utri, AA], [pm])
            P.op("pe", lambda e, i=i: e.matmul(pm[:, 64:128], lhsT=onesf[:], rhs=AA[:, i, :], start=True, stop=True), [onesf, AA], [pm])
            P.op("dve", lambda e: e.tensor_tensor(out=off[:], in0=pstart[:], in1=base[:], op=ALU.add), [pstart, base], [off])
            P.op("dve", lambda e: e.tensor_tensor(out=prod[:], in0=off[:], in1=pm[:, 0:32], op=ALU.add), [off, pm], [prod])
            P.op("dve", lambda e, i=i: e.tensor_tensor(out=prod[:], in0=prod[:], in1=AA[:, i, 0:32], op=ALU.mult), [prod, AA], [prod])
            P.op("dve", lambda e, i=i: e.reduce_sum(out=dstf[:, i, 0:1], in_=prod[:], axis=AX.X), [prod], [dstf])
            P.op("dve", lambda e: e.tensor_tensor(out=off[:], in0=off[:], in1=pm[:, 64:96], op=ALU.add), [off, pm], [off])
            P.op("dve", lambda e: e.tensor_tensor(out=prod[:], in0=off[:], in1=pm[:, 32:64], op=ALU.add), [off, pm], [prod])
            P.op("dve", lambda e, i=i: e.tensor_tensor(out=prod[:], in0=prod[:], in1=AA[:, i, 32:64], op=ALU.mult), [prod, AA], [prod])
            P.op("dve", lambda e, i=i: e.reduce_sum(out=dstf[:, i, 1:2], in_=prod[:], axis=AX.X), [prod], [dstf])
            P.op("dve", lambda e: e.tensor_tensor(out=base[:], in0=base[:], in1=pm[:, 64:96], op=ALU.add), [base, pm], [base])
            P.op("dve", lambda e: e.tensor_tensor(out=base[:], in0=base[:], in1=pm[:, 96:128], op=ALU.add), [base, pm], [base])
        P.op("dve", lambda e: e.tensor_copy(out=dst[:], in_=dstf[:]), [dstf], [dst])
        P.op("dve", lambda e: e.tensor_scalar(out=cmp_[:], in0=pend[:], scalar1=bpos[:, 0:1], scalar2=None, op0=ALU.is_le), [pend, bpos], [cmp_])
        P.op("dve", lambda e: e.reduce_sum(out=bef[:], in_=cmp_[:], axis=AX.X), [cmp_], [bef])
        P.op("dve", lambda e: e.tensor_scalar(out=bef[:], in0=bef[:], scalar1=float(NEXP - 1), scalar2=None, op0=ALU.min), [bef], [bef])
        P.op("dve", lambda e: e.tensor_copy(out=bebc[:], in_=bc_col(bef[:, 0:1], 128)), [bef], [bebc])
        P.op("pe", lambda e: e.matmul(pm[:], lhsT=bebc[:], rhs=identf[:], start=True, stop=True), [bebc, identf], [pm])
        P.op("dve", lambda e: e.tensor_copy(out=be2[:], in_=pm[:]), [pm], [be2])
        for (scale, tgt, nj) in ((float(D), idxg, DK), (float(DE), idxd, DK // 2)):
            P.op("dve", lambda e, scale=scale, nj=nj: e.scalar_tensor_tensor(out=idxf[:, :, 0:nj], in0=bc_last(be2[:, 0:NBLK], nj), scalar=scale, in1=bc_mid(offg[:, 0:nj], NBLK),
                                                                              op0=ALU.mult, op1=ALU.add), [be2, offg], [idxf])
            P.op("dve", lambda e, tgt=tgt, nj=nj: e.tensor_copy(out=tgt[:], in_=idxf[:, :, 0:nj]), [idxf], [tgt])
        es1 = es.enter_context(contextlib.ExitStack())
        hb = [es1.enter_context(nc.sbuf_tensor(f"hbm{i}", [128, D], BF16)) for i in range(2)]
        for i in range(NT):
            h = hb[i % 2]
            P.dma("sp", h[:], H2B[i * 128:(i + 1) * 128, :], [H2B], [h])
            for k in range(2):
                indirect_scatter(P, XB, h[:], dst[:, i, k:k + 1], [h, dst], [XB])
        P.barrier()
        es1.close()
        es0.close()
        es2 = es.enter_context(contextlib.ExitStack())
        sb2 = lambda n, shp, dt=F32: es2.enter_context(nc.sbuf_tensor(n, shp, dt))
        stg = [sb2(f"wst{i}", [128, 4, DE]) for i in range(2)]
        Wg, Wu, Wd = sb2("Wgb", [128, DK, DE], BF16), sb2("Wub", [128, DK, DE], BF16), sb2("Wdb", [128, DK // 2, D], BF16)
        xb = [sb2(f"xbm{i}", [128, D], BF16) for i in range(2)]
        xbT = [sb2(f"xbT{i}", [128, DK, 128], BF16) for i in range(2)]
        sg = [sb2(f"sgm{i}", [128, 128]) for i in range(2)]
        aT = sb2("aTm", [128, DK // 2, 128], BF16)
        yb = [sb2(f"ybm{i}", [128, D]) for i in range(2)]
        ptx = es2.enter_context(nc.psum_tensor("ptxm", [128, 8, 128], BF16))
        pg = [es2.enter_context(nc.psum_tensor(f"pgm{i}", [128, 128], F32)) for i in range(2)]
        pu = [es2.enter_context(nc.psum_tensor(f"pum{i}", [128, 128], F32)) for i in range(2)]
        pd = [es2.enter_context(nc.psum_tensor(f"pdm{i}", [128, 512], F32)) for i in range(2)]
        ns = 0
        ce = 0
        cast_engs = ("act", "dve")
        nsub = 0
        for b in range(NBLK):
            for (Wsrc, Wdst, idx, nchunk, width) in ((d["w_gate"], Wg, idxg, DK, DE), (d["w_up"], Wu, idxg, DK, DE), (d["w_down"], Wd, idxd, DK // 2, D)):
                per = 4 * DE // width
                for c0 in range(0, nchunk, per):
                    st = stg[ns % 2]
                    ns += 1
                    stv = st[:].rearrange("p a b -> p (a b)").rearrange("p (a b) -> p a b", b=width)
                    for c in range(per):
                        indirect_gather(P, stv[:, c, :], Wsrc, idx[:, b, c0 + c:c0 + c + 1], [idx], [st])
                    eng = cast_engs[ce % 2]
                    ce += 1
                    dstv = Wdst[:, c0:c0 + per, :]
                    if eng == "act":
                        P.op("act", lambda e, dstv=dstv, stv=stv: e.copy(out=dstv, in_=stv), [st], [Wdst])
                    else:
                        P.op(eng, lambda e, dstv=dstv, stv=stv: e.tensor_copy(out=dstv, in_=stv), [st], [Wdst])
            for sub in range(BLK // 128):
                r0 = b * BLK + sub * 128
                x, xT, y = xb[nsub % 2], xbT[nsub % 2], yb[nsub % 2]
                nsub += 1
                P.dma("sp", x[:], XB[r0:r0 + 128, :], [XB], [x])
                for hh in range(2):
                    for j2 in range(8):
                        j = hh * 8 + j2
                        P.op("pe", lambda e, j=j, j2=j2: e.transpose(out=ptx[:, j2, :], in_=x[:, j * 128:(j + 1) * 128], identity=identb[:]), [x, identb], [ptx])
                    P.op("dve", lambda e, hh=hh: e.tensor_copy(out=xT[:, hh * 8:(hh + 1) * 8, :], in_=ptx[:]), [ptx], [xT])
                for fc in range(DE // 128):
                    pgg, puu, sgg = pg[fc % 2], pu[fc % 2], sg[fc % 2]
                    for j in range(DK):
                        P.op("pe", lambda e, j=j, fc=fc, pgg=pgg: e.matmul(pgg[:], lhsT=Wg[:, j, fc * 128:(fc + 1) * 128], rhs=xT[:, j, :], start=(j == 0), stop=(j == DK - 1)),
                             [Wg, xT], [pgg])
                    for j in range(DK):
                        P.op("pe", lambda e, j=j, fc=fc, puu=puu: e.matmul(puu[:], lhsT=Wu[:, j, fc * 128:(fc + 1) * 128], rhs=xT[:, j, :], start=(j == 0), stop=(j == DK - 1)),
                             [Wu, xT], [puu])
                    P.op("act", lambda e, pgg=pgg, sgg=sgg: e.activation(out=sgg[:], in_=pgg[:], func=AF.Silu), [pgg], [sgg])
                    P.op("dve", lambda e, fc=fc, puu=puu, sgg=sgg: e.tensor_tensor(out=aT[:, fc, :], in0=puu[:], in1=sgg[:], op=ALU.mult), [puu, sgg], [aT])
                for n in range(4):
                    pdd = pd[n % 2]
                    for fc in range(DE // 128):
                        P.op("pe", lambda e, n=n, fc=fc, pdd=pdd: e.matmul(pdd[:], lhsT=aT[:, fc, :], rhs=Wd[:, fc, n * 512:(n + 1) * 512], start=(fc == 0), stop=(fc == DE // 128 - 1)),
                             [aT, Wd], [pdd])
                    if n % 2 == 0:
                        P.op("act", lambda e, n=n, pdd=pdd: e.copy(out=y[:, n * 512:(n + 1) * 512], in_=pdd[:]), [pdd], [y])
                    else:
                        P.op("dve", lambda e, n=n, pdd=pdd: e.tensor_copy(out=y[:, n * 512:(n + 1) * 512], in_=pdd[:]), [pdd], [y])
                P.dma("sp", YB[r0:r0 + 128, :], y[:], [y], [YB])
        P.barrier()
        es2.close()
        y1 = [sb(f"y1m{i}", [128, D]) for i in range(2)]
        y2 = [sb(f"y2m{i}", [128, D]) for i in range(2)]
        xr = [sb(f"xrm{i}", [128, D]) for i in range(2)]
        for i in range(NT):
            a, bb, x = y1[i % 2], y2[i % 2], xr[i % 2]
            P.dma("sp", x[:], X1[i * 128:(i + 1) * 128, :], [X1], [x])
            indirect_gather(P, a[:], YB, dst[:, i, 0:1], [YB, dst], [a])
            indirect_gather(P, bb[:], YB, dst[:, i, 1:2], [YB, dst], [bb])
            P.op("dve", lambda e, i=i, a=a, x=x: e.scalar_tensor_tensor(out=x[:], in0=a[:], scalar=GT[:, i, 0:1], in1=x[:], op0=ALU.mult, op1=ALU.add), [a, GT, x], [x])
            P.op("dve", lambda e, i=i, bb=bb, x=x: e.scalar_tensor_tensor(out=x[:], in0=bb[:], scalar=GT[:, i, 1:2], in1=x[:], op0=ALU.mult, op1=ALU.add), [bb, GT, x], [x])
            P.dma("sp", y_out[i * 128:(i + 1) * 128, :], x[:], [x], [y_out])
        P.barrier()


LP, LS_ = 8192, 16384
CHN_ALL = 1280
NOWN_ = 6144
NE_ = 10240
MIN_DECAY_ = math.log(1e-2) / 1.5
MAX_DECAY_ = math.log(1e-2) / 0.3


def _circle_tables(L):
    pos = np.arange(L, dtype=np.float64)
    t = np.linspace(0.0, 1.0, L)
    bands = np.linspace(1e-4, 15, 16)
    ang = (2 * np.pi / L) * pos[:, None] * bands[None, :]
    feats = np.concatenate([t[:, None], np.cos(ang), -np.sin(ang)], -1)
    idx2 = np.concatenate([[0], np.arange(L - 1, 0, -1)])
    f2 = np.concatenate([feats, feats[idx2]], 0)
    t2 = np.concatenate([t, t[idx2]])
    return np.ascontiguousarray(f2.T, dtype=np.float32), np.ascontiguousarray(t2[None], dtype=np.float32)


def build_program():
    nc = bass.Bass("TRN2", target_bir_lowering=False)
    di = lambda n, shp, dt=F32: nc.dram_tensor(n, list(shp), dt, kind="ExternalInput").ap()
    dn = lambda n, shp, dt=F32: nc.dram_tensor(n, list(shp), dt, kind="Internal").ap()
    T = LP + LS_
    a = {}
    a["x_seq"] = di("x_seq", [T, D])
    a["x_ext"] = di("x_ext", [NE_, D])
    a["kvalid"] = di("kvalid", [1, NE_], BF16)
    a["ones_row"] = di("ones_row", [1, NOWN_], BF16)
    a["w_h"] = di("w_h", [D, 2 * CHN_ALL])
    a["w_g2"] = di("w_g2", [D, CHN_ALL])
    a["wq"], a["wk"], a["wv"] = di("wq", [D, DA]), di("wk", [D, DA]), di("wv", [D, DA])
    a["gmix"] = di("gmix", [128, DK])
    a["scw_h"] = di("scw_h", [128, 20, 4])
    a["scw_g2"] = di("scw_g2", [128, 10, 4])
    a["ident"] = di("ident", [128, 128])
    a["gqk"] = di("gqk", [128, 2])
    a["bd"] = di("bd", [128, 128])
    a["relb"] = di("relb", [32, NH])
    a["sel"] = di("sel", [32, 2432])
    a["jm"] = di("jm", [128, 128])
    fdc = dict(w1=di("f_w1", [33, 64]), w2=di("f_w2", [64, 64]), w3=di("f_w3", [64, 64]), fvec=di("f_vec", [64, 4]),
               woutd=di("f_woutd", [64, 2, 2 * CHN_ALL]), ndelta=di("f_ndelta", [128, 20]))
    a["skip"] = di("skip", [1, 2 * CHN_ALL])
    fl = {}
    for L, tg in ((LP, "p"), (LS_, "s")):
        N1 = 2 * L // 128
        Hown = 32 if L == LP else 16
        cc = {k: di(f"c{tg}_{k}", v.shape) for k, v in fft_consts(L).items()}
        cc["F4own"] = di(f"c{tg}_F4own", [128, N1 // 128, 2, Hown])
        cc["SEL"] = di(f"c{tg}_SEL", [N1 // 2, Hown])
        fd = dict(fdc)
        fd["featsT2"] = di(f"f{tg}_feats", [33, 2 * L])
        fd["trow2"] = di(f"f{tg}_trow", [1, 2 * L])
        fl[L] = (cc, fd, Hown, N1)
    d3 = dict(w_out=di("w_out", [D, D]), gh=di("gh", [128, 10]), ga=di("ga", [128, 6]), gf=di("gf", [1, D]), wr=di("wr", [128, DK, 36]),
              rb=di("rb", [1, 36]), ident=a["ident"], w_gate=di("w_gate", [NEXP * D, DE]), w_up=di("w_up", [NEXP * D, DE]),
              w_down=di("w_down", [NEXP * DE, D]), utri=di("utri", [128, 128]), ltm=di("ltm", [1, 1024]), bpos=di("bpos", [128, 1]),
              offg=di("offg", [128, DK]))
    x_ext = a["x_ext"]
    d3["x_own_fn"] = lambda r0: x_ext[(1024 + r0 if r0 < 4096 else r0 + 3072):(1024 + r0 if r0 < 4096 else r0 + 3072) + 128, :]
    y_out = nc.dram_tensor("y_own", [NOWN_, D], F32, kind="ExternalOutput").ap()
    UH = dn("UH", [2 * CHN_ALL, 1 + T], BF16)
    G2 = dn("G2", [CHN_ALL, NOWN_], BF16)
    YHo = dn("YHo", [CHN_ALL, NOWN_], BF16)
    KT, QT, VE = dn("KT", [NH, HD + 1, NE_], BF16), dn("QT", [NH, HD + 1, NOWN_], BF16), dn("VE", [NE_, NH * (HD + 1)], BF16)
    EV, EBD, YA = dn("EV", [NH, 2432], BF16), dn("EBD", [NH, 128, KB * 128], BF16), dn("YA", [NOWN_, DA])
    X1, H2B = dn("X1", [NOWN_, D]), dn("H2B", [NOWN_, D], BF16)
    AAd, GTd = dn("AAd", [128, NOWN_ // 128, 64]), dn("GTd", [128, NOWN_ // 128, 2])
    XB, YB = dn("XB", [NSLOT, D], BF16), dn("YB", [NSLOT, D])
    P = Prog(nc)
    cfg = dict(NE=NE_, NOWN=NOWN_, NBLK=NBLK, ones_row=a["ones_row"],
               own_tiles={**{2 + i: i for i in range(8)}, **{14 + i: 8 + i for i in range(4)}},
               pieces=[(0, 6144, 0, 4096), (6144, 4096, 4096, 2048)])
    P.mark("start")
    phase_h(P, nc, [(0, LP, 0, LP, 1), (LP, LS_, LP, T, 1 + LP)], 2 * CHN_ALL, a["x_seq"], a["w_h"], a["gmix"], a["scw_h"], a["ident"], UH, tag="h")
    P.mark("phase_h")
    phase_h(P, nc, [(512, 5120, 1024, 5120, 0), (6656, 3072, 7168, 9216, 4096)], CHN_ALL, a["x_ext"], a["w_g2"], a["gmix"], a["scw_g2"], a["ident"], G2, tag="g")
    P.mark("g2")
    for L, tg, s0, own_off in ((LP, "p", 0, 0), (LS_, "s", LP, 4096)):
        cc, fd, Hown, N1 = fl[L]
        FILT = dn(f"FILT{tg}", [2 * CHN_ALL, 2 * L], BF16)
        GS = [dn(f"GS{tg}{o}", [128, CHN_ALL, 2 * N1], BF16) for o in range(2)]
        hyena_filters(P, nc, L, fd, FILT, CHN_ALL, tg)
        P.mark("filt" + tg)
        hyena_conv(P, nc, L, s0, cc, FILT, GS, UH, a["skip"], G2, own_off, Hown, YHo, CHN_ALL, tg)
        P.mark("conv" + tg)
    phase_q(P, nc, cfg, a["x_ext"], a["wq"], a["wk"], a["wv"], a["gmix"], a["gqk"], a["kvalid"], a["ident"], a["bd"], KT, QT, VE)
    P.mark("phase_q")
    phase_attn(P, nc, cfg, a["relb"], a["sel"], a["jm"], KT, QT, VE, EV, EBD, YA)
    P.mark("attn")
    phase3(P, nc, cfg, d3, YHo, YA, X1, H2B, AAd, GTd)
    P.mark("phase3")
    phase_moe(P, nc, cfg, d3, X1, H2B, AAd, GTd, XB, YB, y_out)
    P.mark("moe")
    P.finish()
    return nc, P


def host_inputs(inp):
    import ml_dtypes
    bf = ml_dtypes.bfloat16
    f32 = np.float32
    g = lambda k: np.asarray(inp[k])
    xp, xs = g("x_prompt"), g("x_sample")
    w_in = g("w_in")[0]
    scwv = np.concatenate([g("sconv_w")[0], g("sconv_b")[0][None]], 0).T
    arr = lambda v, n: np.ascontiguousarray(v.reshape(n, 128, -1).transpose(1, 0, 2))
    col = lambda v: np.ascontiguousarray(v.reshape(-1, 128).T)
    shared = {}
    shared["w_h"] = np.ascontiguousarray(w_in[:, 0:2560])
    shared["w_g2"] = np.ascontiguousarray(w_in[:, 2560:3840])
    shared["wq"] = np.ascontiguousarray(w_in[:, 3840:4608])
    shared["wk"] = np.ascontiguousarray(w_in[:, 4608:5376])
    shared["wv"] = np.ascontiguousarray(w_in[:, 5376:6144])
    shared["gmix"] = col(g("mix_norm_g")[0])
    shared["scw_h"] = arr(scwv[0:2560], 20)
    shared["scw_g2"] = arr(scwv[2560:3840], 10)
    shared["ident"] = np.eye(128, dtype=f32)
    shared["gqk"] = np.ascontiguousarray(np.stack([np.tile(g("q_norm_g")[0], 2), np.tile(g("k_norm_g")[0], 2)], 1))
    shared["bd"] = np.kron(np.eye(2), np.ones((64, 64))).astype(f32)
    shared["relb"] = g("rel_bias")
    shared["sel"] = attn_sel_table()
    shared["jm"] = np.ascontiguousarray(np.eye(128, dtype=f32)[::-1])
    shared["ones_row"] = np.ones((1, NOWN_), f32).astype(bf)
    shared["f_w1"], shared["f_w2"], shared["f_w3"] = g("filt_w1")[0], g("filt_w2")[0], g("filt_w3")[0]
    shared["f_vec"] = np.ascontiguousarray(np.stack([g("filt_freq")[0], g("filt_b1")[0], g("filt_b2")[0], g("filt_b3")[0]], 1))
    shared["f_woutd"] = np.ascontiguousarray(g("filt_w_out")[0].reshape(64, 2, 2, CHN_ALL).transpose(0, 2, 1, 3).reshape(64, 2, 2 * CHN_ALL))
    delta = np.abs(np.linspace(MIN_DECAY_, MAX_DECAY_, CHN_ALL)).astype(f32)
    shared["f_ndelta"] = col(-np.tile(delta, 2))
    shared["skip"] = np.ascontiguousarray(g("hyena_skip")[0].reshape(1, -1))
    fcon = {}
    for L, tg in ((LP, "p"), (LS_, "s")):
        fc = fft_consts(L)
        fcon[L] = fc
        for k, v in fc.items():
            shared[f"c{tg}_{k}"] = v
        shared[f"f{tg}_feats"], shared[f"f{tg}_trow"] = _circle_tables(L)
    shared["w_out"] = g("w_out")[0]
    shared["gh"] = col(g("out_norm_h")[0])
    shared["ga"] = col(g("out_norm_a")[0])
    shared["gf"] = np.ascontiguousarray(g("ffn_norm_g")[0][None])
    wcat = np.concatenate([g("group_router_w")[0], g("expert_router_w")[0].transpose(1, 0, 2).reshape(D, 32)], 1)
    shared["wr"] = arr(wcat, DK)
    shared["rb"] = np.ascontiguousarray(np.concatenate([g("group_router_b")[0], g("expert_router_b")[0].reshape(-1)])[None])
    shared["w_gate"] = g("w_gate")[0].reshape(NEXP * D, DE)
    shared["w_up"] = g("w_up")[0].reshape(NEXP * D, DE)
    shared["w_down"] = g("w_down")[0].reshape(NEXP * DE, D)
    shared["utri"] = np.triu(np.ones((128, 128), f32), 1)
    shared["ltm"] = np.tril(np.ones((32, 32), f32)).reshape(1, -1)
    shared["bpos"] = (np.arange(128) * float(BLK)).astype(f32)[:, None]
    shared["offg"] = (np.arange(DK)[None, :] * 128 + np.arange(128)[:, None]).astype(f32)
    maps = []
    for c in range(8):
        b, hf = c // 2, c % 2
        m = dict(shared)
        m["x_seq"] = np.concatenate([xp[b], xs[0]], 0)
        xe = np.zeros((NE_, D), f32)
        kv = np.full((1, NE_), NEGV, f32)
        for (src, Lq, lo, n, e0) in ((xp[b], LP, 4096 * hf - 1024, 6144, 0), (xs[0], LS_, 2048 * c - 1024, 4096, 6144)):
            a0, a1 = max(lo, 0), min(lo + n, Lq)
            xe[e0 + a0 - lo:e0 + a1 - lo] = src[a0:a1]
            kv[0, e0 + a0 - lo:e0 + a1 - lo] = 0.0
        m["x_ext"] = xe
        m["kvalid"] = kv.astype(bf)
        for L, tg, Hown, r0 in ((LP, "p", 32, 32 * hf), (LS_, "s", 16, 16 * c)):
            H = L // 128
            sel = np.zeros((H, Hown), f32)
            sel[r0 + np.arange(Hown), np.arange(Hown)] = 1.0
            m[f"c{tg}_SEL"] = sel
            m[f"c{tg}_F4own"] = np.ascontiguousarray(fcon[L]["F4"][:, :, :, r0:r0 + Hown])
        maps.append(m)
    return maps


_PROG = None


def kernel(**inputs):
    global _PROG
    if _PROG is None:
        _PROG = build_program()[0]
    maps = host_inputs(inputs)
    res = run_bass_kernel_spmd(_PROG, maps, core_ids=list(range(8)))
    yp = np.zeros((4, LP, D), np.float32)
    ys = np.zeros((1, LS_, D), np.float32)
    for c in range(8):
        yo = np.asarray(res.results[c]["y_own"])
        yp[c // 2, 4096 * (c % 2):4096 * (c % 2) + 4096] = yo[0:4096]
        ys[0, 2048 * c:2048 * c + 2048] = yo[4096:6144]
    return yp, ys
```
